# Optimizing a Trainium2 kernel written in Bass

```python
import math
import jax
import jax.numpy as jnp
from jax import lax
import numpy as np

D_MODEL = 1024
BATCH = 8
SEQ = 8192
DEPTH = 2

GRID_W = 64
CTX_LEN = 256
HEAD_DIM = 64
D_MIX = D_MODEL
D_NA = D_MIX // 2
D_ML = D_MIX // 4
D_HY = D_MIX // 4
H_NA = D_NA // HEAD_DIM
H_ML = D_ML // HEAD_DIM
NA_ROWS = 8
NA_COLS = 16
NA_QBLK = 16
NA_KBLK = 2 * NA_COLS
ML_CHUNK = 64
HY_ORDER = 2
HY_EMB = 33
HY_FFN = 64
HY_TARGET = 1e-2
HY_FAST = 0.3
HY_SLOW = 1.5
ROPE_BASE = 10000.0
EPS = 1e-6
SPLIT_SIZES = (D_NA,) * 4 + (D_ML,) * 5 + (4 * H_ML,) + (D_HY,) * 4
N_IN = 4 * D_NA + 5 * D_ML + 4 * H_ML + 4 * D_HY

kernel_name = 'hybrid_natten_mlstm_hyena_block'


def _rmsnorm(x, g):
    xf = x.astype(jnp.float32)
    y = xf * lax.rsqrt(jnp.mean(xf * xf, axis=-1, keepdims=True) + EPS)
    return (y * g.astype(jnp.float32)).astype(x.dtype)


def _split_cols(p):
    idx = np.cumsum(SPLIT_SIZES)[:-1].tolist()
    return jnp.split(p, idx, axis=-1)


def _heads(a, n_heads):
    return a.reshape(a.shape[:-1] + (n_heads, a.shape[-1] // n_heads))


def _dwconv3(u, w):
    up = jnp.pad(u, ((0, 0), (1, 1), (0, 0)))
    return up[:, :-2] * w[0] + up[:, 1:-1] * w[1] + up[:, 2:] * w[2]


def _axial_rope(x):
    L, d = x.shape[1], x.shape[-1]
    n = d // 4
    t = jnp.arange(L)
    inv = ROPE_BASE ** (-jnp.arange(n, dtype=jnp.float32) / n)
    pos = jnp.stack([t // GRID_W, t % GRID_W], axis=-1).astype(jnp.float32)
    ang = pos[:, :, None] * inv
    cos = jnp.cos(ang)[None, :, None]
    sin = jnp.sin(ang)[None, :, None]
    xr = x.astype(jnp.float32).reshape(x.shape[:-1] + (2, 2, n))
    x1, x2 = xr[..., 0, :], xr[..., 1, :]
    out = jnp.stack([x1 * cos - x2 * sin, x1 * sin + x2 * cos], axis=-2).reshape(x.shape)
    return out.astype(x.dtype)


def _neighbourhood_attention(q, k, v, kc, vc, rpb, rows):
    nb, L, nh, d = q.shape
    kr = min(NA_ROWS, rows)
    scale = d ** -0.5
    qg = q.reshape(nb, rows, GRID_W, nh, d)
    kg = k.reshape(nb, rows, GRID_W, nh, d)
    vg = v.reshape(nb, rows, GRID_W, nh, d)
    n_cb = GRID_W // NA_QBLK
    cb_start = np.clip(np.arange(n_cb) * NA_QBLK - NA_COLS // 2, 0, GRID_W - NA_KBLK)
    col_idx = cb_start[:, None] + np.arange(NA_KBLK)
    qcol = np.arange(GRID_W).reshape(n_cb, NA_QBLK)
    qstart = np.clip(qcol - NA_COLS // 2, 0, GRID_W - NA_COLS)
    kcol = col_idx[:, None, :]
    col_mask = (kcol >= qstart[..., None]) & (kcol < qstart[..., None] + NA_COLS)
    col_off = np.clip(kcol - qcol[..., None], -(NA_COLS - 1), NA_COLS - 1) + NA_COLS - 1
    rpb_col = rpb[:, :, col_off]
    row_start = jnp.clip(jnp.arange(rows) - NA_ROWS // 2, 0, rows - kr)
    mask = jnp.asarray(col_mask)[None, None, :, :, None, :]

    def row_block(r):
        rs = row_start[r]
        qr = qg[:, r].reshape(nb, n_cb, NA_QBLK, nh, d)
        kw = lax.dynamic_slice_in_dim(kg, rs, kr, axis=1)[:, :, col_idx]
        vw = lax.dynamic_slice_in_dim(vg, rs, kr, axis=1)[:, :, col_idx]
        s_lat = jnp.einsum('bjqhd,bijchd->bhjqic', qr, kw).astype(jnp.float32) * scale
        roff = rs + jnp.arange(kr) - r + (NA_ROWS - 1)
        bias = jnp.transpose(rpb_col[:, roff], (0, 2, 3, 1, 4)).astype(jnp.float32)
        s_lat = jnp.where(mask, s_lat + bias[None], -jnp.inf)
        s_ctx = jnp.einsum('bjqhd,bnhd->bhjqn', qr, kc).astype(jnp.float32) * scale
        s = jnp.concatenate([s_lat.reshape(s_lat.shape[:4] + (kr * NA_KBLK,)), s_ctx], axis=-1)
        p = jax.nn.softmax(s, axis=-1).astype(v.dtype)
        p_lat = p[..., :kr * NA_KBLK].reshape(s_lat.shape)
        p_ctx = p[..., kr * NA_KBLK:]
        o = jnp.einsum('bhjqic,bijchd->bjqhd', p_lat, vw) + jnp.einsum('bhjqn,bnhd->bjqhd', p_ctx, vc)
        return o.reshape(nb, GRID_W, nh * d)

    out = lax.map(row_block, jnp.arange(rows))
    return jnp.moveaxis(out, 0, 1).reshape(nb, L, nh * d)


def _ctx_attention(q, k, v):
    nb, n, nh, d = q.shape
    s = jnp.einsum('bnhd,bmhd->bhnm', q, k).astype(jnp.float32) * d ** -0.5
    p = jax.nn.softmax(s, axis=-1).astype(v.dtype)
    return jnp.einsum('bhnm,bmhd->bnhd', p, v).reshape(nb, n, nh * d)


def _mlstm_scan(q, k, v, li, lf, state, emit):
    nb, nh, L, d = q.shape
    nc = L // ML_CHUNK

    def chunks(a):
        a = a.reshape(a.shape[:2] + (nc, ML_CHUNK) + a.shape[3:])
        return jnp.moveaxis(a, 2, 0)

    tril = jnp.tril(jnp.ones((ML_CHUNK, ML_CHUNK), dtype=bool))

    def step(carry, inp):
        C, n, m = carry
        qc, kc, vc, ic, fc = inp
        b = jnp.cumsum(fc, axis=-1)
        dlog = jnp.where(tril, b[..., :, None] - b[..., None, :] + ic[..., None, :], -jnp.inf)
        inter = b + m[..., None]
        m_t = jnp.maximum(inter, jnp.max(dlog, axis=-1))
        s = jnp.einsum('bhtk,bhsk->bhts', qc, kc) * jnp.exp(dlog - m_t[..., None])
        a = jnp.exp(inter - m_t)
        num = a[..., None] * jnp.einsum('bhvk,bhtk->bhtv', C, qc) + jnp.einsum('bhts,bhsv->bhtv', s, vc)
        den = a * jnp.einsum('bhk,bhtk->bht', n, qc) + jnp.sum(s, axis=-1)
        h = num / jnp.maximum(jnp.abs(den), jnp.exp(-m_t))[..., None]
        b_end = b[..., -1]
        g = b_end[..., None] - b + ic
        m_new = jnp.maximum(b_end + m, jnp.max(g, axis=-1))
        decay = jnp.exp(b_end + m - m_new)
        w = jnp.exp(g - m_new[..., None])
        C = decay[..., None, None] * C + jnp.einsum('bhsv,bhsk->bhvk', w[..., None] * vc, kc)
        n = decay[..., None] * n + jnp.einsum('bhs,bhsk->bhk', w, kc)
        return (C, n, m_new), (h if emit else None)

    state, h = lax.scan(step, state, tuple(chunks(a) for a in (q, k, v, li, lf)))
    if emit:
        h = jnp.moveaxis(h, 0, 2).reshape(nb, nh, L, d)
    return h, state


def _mlstm_bidirectional(qc, kc, vc, gc, ql, kl, vl, gl, emit_ctx):
    nb, nh, _, d = ql.shape
    zero = (jnp.zeros((nb, nh, d, d), jnp.float32), jnp.zeros((nb, nh, d), jnp.float32),
            jnp.zeros((nb, nh), jnp.float32))
    outs_lat, outs_ctx = [], []
    for direction in range(2):
        flip = (lambda a: jnp.flip(a, axis=2)) if direction else (lambda a: a)
        hc, st = _mlstm_scan(flip(qc), flip(kc), flip(vc), flip(gc[:, direction, 0]),
                             flip(jax.nn.log_sigmoid(gc[:, direction, 1])), zero, emit_ctx)
        hl, _ = _mlstm_scan(flip(ql), flip(kl), flip(vl), flip(gl[:, direction, 0]),
                            flip(jax.nn.log_sigmoid(gl[:, direction, 1])), st, True)
        outs_lat.append(flip(hl))
        if emit_ctx:
            outs_ctx.append(flip(hc))
    h_ctx = (outs_ctx[0] + outs_ctx[1]) if emit_ctx else None
    return outs_lat[0] + outs_lat[1], h_ctx


def _mlstm_inputs(qm, km, vm, gm, conv_ml, b_if, rope):
    qk = jax.nn.silu(_dwconv3(jnp.concatenate([qm, km], axis=-1), conv_ml))
    q, k = jnp.split(qk, 2, axis=-1)
    q, k, v = _heads(q, H_ML), _heads(k, H_ML), _heads(vm, H_ML)
    if rope:
        q, k = _axial_rope(q), _axial_rope(k)
    to = lambda a: jnp.swapaxes(a, 1, 2).astype(jnp.float32)
    g = (gm.reshape(gm.shape[:-1] + (2, 2, H_ML)) + b_if).astype(jnp.float32)
    g = jnp.transpose(g, (0, 2, 3, 4, 1))
    return to(q), to(k) * HEAD_DIM ** -0.5, to(v), g


def _merge_heads(h):
    nb, nh, L, d = h.shape
    return jnp.swapaxes(h, 1, 2).reshape(nb, L, nh * d)


def _hyena_filters(L, w1, b1, w2, b2, w3, freq):
    f32 = jnp.float32
    t = jnp.arange(L, dtype=f32)
    tn = t / (L - 1)
    bands = (HY_EMB - 1) // 2
    fr = jnp.linspace(1e-4, bands - 1, bands, dtype=f32)
    ang = (2.0 * math.pi / L) * t[:, None] * fr[None, :]
    feat = jnp.concatenate([tn[:, None], jnp.cos(ang), -jnp.sin(ang)], axis=-1)
    a = jnp.sin(freq[0].astype(f32) * (feat @ w1.astype(f32) + b1.astype(f32)))
    a = jnp.sin(freq[1].astype(f32) * (a @ w2.astype(f32) + b2.astype(f32)))
    filt = (a @ w3.astype(f32)).reshape(L, HY_ORDER, 2, D_HY)
    deltas = jnp.abs(jnp.linspace(math.log(HY_TARGET) / HY_SLOW, math.log(HY_TARGET) / HY_FAST, D_HY, dtype=f32))
    filt = filt * jnp.exp(-tn[:, None] * deltas)[:, None, None, :]
    fwd, bwd = filt[:, :, 0], filt[:, :, 1]
    k2 = jnp.concatenate([fwd, jnp.zeros((1, HY_ORDER, D_HY), f32), jnp.flip(bwd[1:], axis=0)], axis=0)
    k2 = k2 * lax.rsqrt(jnp.sum(k2 * k2, axis=0, keepdims=True) + EPS)
    return jnp.fft.rfft(k2, axis=0)


def _fftconv(z, kf, bias):
    L = z.shape[1]
    y = jnp.fft.irfft(jnp.fft.rfft(z, n=2 * L, axis=1) * kf, n=2 * L, axis=1)[:, :L]
    return y + z * bias


def _hyena(vh, x1h, x2h, conv_hy, hf_w1, hf_b1, hf_w2, hf_b2, hf_w3, hf_freq, hy_bias):
    u = _dwconv3(jnp.concatenate([vh, x1h, x2h], axis=-1), conv_hy).astype(jnp.float32)
    v, x1, x2 = jnp.split(u, 3, axis=-1)
    kf = _hyena_filters(u.shape[1], hf_w1, hf_b1, hf_w2, hf_b2, hf_w3, hf_freq)
    bias = hy_bias.astype(jnp.float32)
    z = x1 * _fftconv(v, kf[:, 0], bias[0])
    return x2 * _fftconv(z, kf[:, 1], bias[1])


def _layer(x, ctx, c, c_ctx, w_ada, b_ada, g_pre, g_post, w_in, b_if, conv_ml, conv_hy, rpb,
           hf_w1, hf_b1, hf_w2, hf_b2, hf_w3, hf_freq, hy_bias, w_out, update_ctx):
    rows = x.shape[1] // GRID_W
    mod = jax.nn.silu(c) @ w_ada + b_ada
    mod_c = jax.nn.silu(c_ctx) @ w_ada + b_ada
    shift, scale, gate = jnp.split(mod, 3, axis=-1)
    shift_c, scale_c, gate_c = jnp.split(mod_c, 3, axis=-1)
    h = _rmsnorm(x, g_pre) * (1.0 + scale[:, None]) + shift[:, None]
    hc = _rmsnorm(ctx, g_pre) * (1.0 + scale_c) + shift_c
    qa, ka, va, za, qm, km, vm, om, zm, gm, vh, x1h, x2h, zh = _split_cols(h @ w_in)
    qac, kac, vac, zac, qmc, kmc, vmc, omc, zmc, gmc, vhc, x1hc, x2hc, zhc = _split_cols(hc @ w_in)

    kac_h, vac_h = _heads(kac, H_NA), _heads(vac, H_NA)
    o_na = _neighbourhood_attention(_heads(qa, H_NA), _heads(ka, H_NA), _heads(va, H_NA), kac_h, vac_h, rpb, rows)

    ql, kl, vl, gl = _mlstm_inputs(qm, km, vm, gm, conv_ml, b_if, True)
    qc, kc, vc, gc = _mlstm_inputs(qmc, kmc, vmc, gmc, conv_ml, b_if, False)
    h_lat, h_ctx = _mlstm_bidirectional(qc, kc, vc, gc, ql, kl, vl, gl, update_ctx)
    o_ml = jax.nn.sigmoid(om) * _merge_heads(h_lat).astype(om.dtype)

    o_hy = _hyena(vh, x1h, x2h, conv_hy, hf_w1, hf_b1, hf_w2, hf_b2, hf_w3, hf_freq, hy_bias).astype(zh.dtype)

    y = jnp.concatenate([o_na * jax.nn.silu(za), o_ml * jax.nn.silu(zm), o_hy * jax.nn.silu(zh)], axis=-1) @ w_out
    x = x + gate[:, None] * _rmsnorm(y, g_post)

    if update_ctx:
        o_na_c = _ctx_attention(_heads(qac, H_NA), kac_h, vac_h)
        o_ml_c = jax.nn.sigmoid(omc) * _merge_heads(h_ctx).astype(omc.dtype)
        o_hy_c = _hyena(vhc, x1hc, x2hc, conv_hy, hf_w1, hf_b1, hf_w2, hf_b2, hf_w3, hf_freq, hy_bias).astype(zhc.dtype)
        yc = jnp.concatenate([o_na_c * jax.nn.silu(zac), o_ml_c * jax.nn.silu(zmc), o_hy_c * jax.nn.silu(zhc)], axis=-1) @ w_out
        ctx = ctx + gate_c * _rmsnorm(yc, g_post)
    return x, ctx


def setup_inputs(seed: int = 0) -> dict:
    key = jax.random.key(seed)
    ks = jax.random.split(key, 24)
    f32 = jnp.float32
    nrm = lambda k, shape, s: s * jax.random.normal(k, shape, f32)
    x = nrm(ks[0], (BATCH, SEQ, D_MODEL), 1.0)
    c = nrm(ks[1], (BATCH, D_MODEL), 1.0)
    ctx = nrm(ks[2], (BATCH, CTX_LEN, D_MODEL), 1.0)
    c_ctx = nrm(ks[3], (D_MODEL,), 0.5)
    w_ada = nrm(ks[4], (DEPTH, D_MODEL, 3 * D_MODEL), 0.5 * D_MODEL ** -0.5)
    b_ada = nrm(ks[5], (DEPTH, 3 * D_MODEL), 0.02)
    g_pre = 1.0 + nrm(ks[6], (DEPTH, D_MODEL), 0.02)
    g_post = 1.0 + nrm(ks[7], (DEPTH, D_MODEL), 0.02)
    w_in = nrm(ks[8], (DEPTH, D_MODEL, N_IN), D_MODEL ** -0.5)
    f_bias = jnp.linspace(3.0, 6.0, H_ML, dtype=f32) + nrm(ks[9], (DEPTH, 2, H_ML), 0.1)
    i_bias = nrm(ks[10], (DEPTH, 2, H_ML), 0.1)
    b_if = jnp.stack([i_bias, f_bias], axis=2)
    conv_ml = nrm(ks[11], (DEPTH, 3, 2 * D_ML), 0.5)
    conv_hy = nrm(ks[12], (DEPTH, 3, 3 * D_HY), 0.5)
    rpb = nrm(ks[13], (DEPTH, H_NA, 2 * NA_ROWS - 1, 2 * NA_COLS - 1), 0.1)
    hf_w1 = nrm(ks[14], (DEPTH, HY_EMB, HY_FFN), HY_EMB ** -0.5)
    hf_b1 = nrm(ks[15], (DEPTH, HY_FFN), 0.1)
    hf_w2 = nrm(ks[16], (DEPTH, HY_FFN, HY_FFN), HY_FFN ** -0.5)
    hf_b2 = nrm(ks[17], (DEPTH, HY_FFN), 0.1)
    hf_w3 = nrm(ks[18], (DEPTH, HY_FFN, HY_ORDER * 2 * D_HY), HY_FFN ** -0.5)
    hf_freq = 1.0 + nrm(ks[19], (DEPTH, 2, HY_FFN), 0.1)
    hy_bias = nrm(ks[20], (DEPTH, HY_ORDER, D_HY), 0.5)
    w_out = nrm(ks[21], (DEPTH, D_MIX, D_MODEL), D_MIX ** -0.5)
    return {'x': x, 'c': c, 'ctx': ctx, 'c_ctx': c_ctx, 'w_ada': w_ada, 'b_ada': b_ada,
            'g_pre': g_pre, 'g_post': g_post, 'w_in': w_in, 'b_if': b_if, 'conv_ml': conv_ml,
            'conv_hy': conv_hy, 'rpb': rpb, 'hf_w1': hf_w1, 'hf_b1': hf_b1, 'hf_w2': hf_w2,
            'hf_b2': hf_b2, 'hf_w3': hf_w3, 'hf_freq': hf_freq, 'hy_bias': hy_bias, 'w_out': w_out}


def reference(x, c, ctx, c_ctx, w_ada, b_ada, g_pre, g_post, w_in, b_if, conv_ml, conv_hy, rpb,
              hf_w1, hf_b1, hf_w2, hf_b2, hf_w3, hf_freq, hy_bias, w_out):
    for l in range(DEPTH):
        x, ctx = _layer(x, ctx, c, c_ctx, w_ada[l], b_ada[l], g_pre[l], g_post[l], w_in[l], b_if[l],
                        conv_ml[l], conv_hy[l], rpb[l], hf_w1[l], hf_b1[l], hf_w2[l], hf_b2[l], hf_w3[l],
                        hf_freq[l], hy_bias[l], w_out[l], l < DEPTH - 1)
    return x
```

```python
import contextlib
import numpy as np
import concourse.bass as bass
import concourse.mybir as mybir
from concourse.bass_utils import run_bass_kernel_spmd

F32 = mybir.dt.float32
BF16 = mybir.dt.bfloat16
ACT = mybir.ActivationFunctionType
ALU = mybir.AluOpType
AX = mybir.AxisListType

NDMA_SEMS = 6


class Prog:
    ENGS = ('pe', 'act', 'dve', 'pool', 'sp')

    def __init__(self, nc):
        self.nc = nc
        self.stack = contextlib.ExitStack()
        self.streams = {e: [] for e in self.ENGS}
        self.new_semset()
        self.waited = {e: {} for e in self.ENGS}
        self.lastw = {}
        self.readers = {}
        self.n_inst = 0

    def new_semset(self):
        self._setid = getattr(self, "_setid", -1) + 1
        t = "_%d" % self._setid
        self.sem = {}
        self.cnt = {}
        for e in self.ENGS:
            self.sem[e] = self.stack.enter_context(self.nc.semaphore("s_" + e + t))
            self.cnt[e] = 0
        self.dsem = {}
        self.dcnt = {}
        self.dnext = {}
        for q in ('sp', 'pool', 'act'):
            self.dsem[q] = [self.stack.enter_context(self.nc.semaphore("d_%s%d%s" % (q, i, t))) for i in range(NDMA_SEMS)]
            self.dcnt[q] = [0] * NDMA_SEMS
            self.dnext[q] = 0
        self.waited = {e: {} for e in self.ENGS}

    def begin(self):
        self.pstack = contextlib.ExitStack()

    def sb(self, name, shape, dtype):
        self._uid = getattr(self, "_uid", 0) + 1
        return self.pstack.enter_context(self.nc.sbuf_tensor("%s_%d" % (name, self._uid), list(shape), dtype))

    def ps(self, name, shape, dtype):
        return self.stack.enter_context(self.nc.psum_tensor(name, list(shape), dtype))

    def _need(self, eng, ev):
        sem, val, name = ev
        w = self.waited[eng]
        if w.get(name, 0) >= val:
            return
        w[name] = val
        self.streams[eng].append(lambda e, sem=sem, val=val: e.wait_ge(sem, val))

    def _deps(self, eng, r, w, skip_same=False):
        evs = []
        for k in r:
            if k in self.lastw:
                evs.append(self.lastw[k])
        for k in w:
            if k in self.lastw:
                evs.append(self.lastw[k])
            evs.extend(self.readers.get(k, ()))
        for ev in evs:
            if skip_same and ev[2] == eng:
                continue
            self._need(eng, ev)

    def _record(self, ev, r, w):
        for k in w:
            self.lastw[k] = ev
            self.readers[k] = []
        for k in r:
            if k in w:
                continue
            self.readers.setdefault(k, []).append(ev)

    def op(self, eng, fn, r=(), w=()):
        self._deps(eng, r, w, skip_same=(eng == 'pe'))
        self.cnt[eng] += 1
        sem, val = self.sem[eng], self.cnt[eng]
        self.streams[eng].append(lambda e, fn=fn, sem=sem: fn(e).then_inc(sem, 1))
        self._record((sem, val, eng), r, w)
        self.n_inst += 1

    def dma(self, q, out, in_, r=(), w=(), **kw):
        self._deps(q, r, w)
        i = self.dnext[q]
        self.dnext[q] = (i + 1) % NDMA_SEMS
        sem = self.dsem[q][i]
        name = "d_%s%d" % (q, i)
        if self.dcnt[q][i] > 0:
            self._need(q, (sem, self.dcnt[q][i], name))
        self.dcnt[q][i] += 16
        val = self.dcnt[q][i]
        self.streams[q].append(lambda e, out=out, in_=in_, sem=sem, kw=kw: e.dma_start(out=out, in_=in_, **kw).then_inc(sem, 16))
        self._record((sem, val, name), r, w)
        self.n_inst += 1

    def barrier(self):
        evs = []
        for q in self.dsem:
            for i in range(NDMA_SEMS):
                if self.dcnt[q][i]:
                    evs.append((self.dsem[q][i], self.dcnt[q][i], "d_%s%d" % (q, i)))
        for e in self.ENGS:
            if self.cnt[e]:
                evs.append((self.sem[e], self.cnt[e], e))
        for e in self.ENGS:
            for ev in evs:
                if ev[2] != e:
                    self._need(e, ev)
        self.lastw = {}
        self.readers = {}

    def end(self):
        self.barrier()
        streams = self.streams
        with self.nc.Block() as block:
            @block.tensor
            def _(eng):
                for f in streams['pe']:
                    f(eng)

            @block.scalar
            def _(eng):
                for f in streams['act']:
                    f(eng)

            @block.vector
            def _(eng):
                for f in streams['dve']:
                    f(eng)

            @block.gpsimd
            def _(eng):
                for f in streams['pool']:
                    f(eng)

            @block.sync
            def _(eng):
                for f in streams['sp']:
                    f(eng)
        self.streams = {e: [] for e in self.ENGS}
        self.pstack.close()

    def finish(self):
        self.stack.close()

    def mm(self, out, lhsT, rhs, start=True, stop=True, r=(), w=()):
        self.op('pe', lambda e: e.matmul(out, lhsT, rhs, start=start, stop=stop), r, w)

    def tr(self, out, in_, ident, r=(), w=()):
        self.op('pe', lambda e: e.transpose(out, in_, ident), r, w)

    def act(self, out, in_, func, r=(), w=(), **kw):
        self.op('act', lambda e: e.activation(out, in_, func, **kw), r, w)

    def ts(self, eng, out, in0, s1, s2, op0, op1=None, r=(), w=()):
        if op1 is None:
            self.op(eng, lambda e: e.tensor_scalar(out, in0, s1, None, op0), r, w)
        else:
            self.op(eng, lambda e: e.tensor_scalar(out, in0, s1, s2, op0, op1), r, w)

    def tt(self, eng, out, in0, in1, op, r=(), w=()):
        self.op(eng, lambda e: e.tensor_tensor(out, in0, in1, op), r, w)

    def stt(self, eng, out, in0, sc, in1, op0, op1, r=(), w=()):
        self.op(eng, lambda e: e.scalar_tensor_tensor(out, in0, sc, in1, op0, op1), r, w)

    def cp(self, eng, out, in_, r=(), w=()):
        if eng == 'act':
            self.op(eng, lambda e: e.copy(out, in_), r, w)
        else:
            self.op(eng, lambda e: e.tensor_copy(out, in_), r, w)

    def recip(self, out, in_, r=(), w=()):
        self.op('dve', lambda e: e.reciprocal(out, in_), r, w)

    def memset(self, eng, ap, val, w=()):
        self.op(eng, lambda e: e.memset(ap, val), (), w)


L = 8192
LC = 256
T = L + LC
NT = T // 128
D = 1024
NIN = 4368
DEPTH = 2
NTYPES = 21
NEG = -30000.0
PI = float(np.pi)


def na_tile_list(j):
    if j == 0:
        return [(jp, 5 + jp) for jp in range(4)]
    if j == 1:
        return [(jp, 9 + jp) for jp in range(4)]
    if j == 62:
        return [(60 + i, 13 + i) for i in range(4)]
    if j == 63:
        return [(60 + i, 17 + i) for i in range(4)]
    return [(j + d, d + 2) for d in range(-2, 3)]


def na_type_pairs():
    reps = {}
    for j in [10, 0, 1, 62, 63]:
        for jp, ty in na_tile_list(j):
            reps[ty] = (j, jp)
    return [reps[t] for t in range(NTYPES)]


class Ctx:
    pass


def build_program(n_layers=DEPTH, stop_after=None, debug=()):
    nc = bass.Bass("TRN2", target_bir_lowering=False)
    P = Prog(nc)
    C = Ctx()
    C.nc, C.P = nc, P

    def din(name, shape, dt=F32):
        return nc.dram_tensor(name, list(shape), dt, kind="ExternalInput").ap()

    def scratch(name, shape, dt):
        kind = "ExternalOutput" if name in debug else "Internal"
        return nc.dram_tensor(name, list(shape), dt, kind=kind).ap()

    C.x = din("x", [L, D]); C.ctx = din("ctx", [LC, D]); C.cc = din("cc", [128, 16])
    C.w_ada = din("w_ada", [DEPTH, D, 3 * D]); C.b_ada = din("b_ada", [DEPTH, 1, 3 * D])
    C.g_pre = din("g_pre", [DEPTH, 1, D]); C.g_post = din("g_post", [DEPTH, 1, D])
    C.w_in = din("w_in", [DEPTH, D, NIN]); C.b_if = din("b_if", [DEPTH, 16, 1])
    C.conv_ml = din("conv_ml", [DEPTH, 128, 12]); C.conv_hy = din("conv_hy", [DEPTH, 128, 18])
    C.rpb_g = din("rpb_g", [DEPTH, 8, 128, NTYPES * 128])
    C.hf_w1 = din("hf_w1", [DEPTH, 33, 64]); C.hf_b1 = din("hf_b1", [DEPTH, 64, 1])
    C.hf_w2 = din("hf_w2", [DEPTH, 64, 64]); C.hf_b2 = din("hf_b2", [DEPTH, 64, 1])
    C.hf_w3 = din("hf_w3", [DEPTH, 64, 1024]); C.hf_freq = din("hf_freq", [DEPTH, 64, 2])
    C.hy_bias = din("hy_bias", [DEPTH, 128, 4]); C.w_out = din("w_out", [DEPTH, D, D])
    C.ident_f = din("ident_f", [128, 128]); C.ident_b = din("ident_b", [128, 128], BF16)
    C.na_mask = din("na_mask", [128, NTYPES * 128])
    C.rope_cos = din("rope_cos", [128, L]); C.rope_sin = din("rope_sin", [128, L])
    C.perm_b = din("perm_b", [128, 128], BF16)
    C.ml_mask = din("ml_mask", [128, 2 * 512], BF16)
    C.selC = din("selC", [8, 8 * 128]); C.selcols = din("selcols", [16, 4]); C.selC2 = din("selC2", [40, 8 * 128], BF16)
    C.featT = din("featT", [33, L]); C.decayT = din("decayT", [256, L])
    C.featTc = din("featTc", [33, LC]); C.decayTc = din("decayTc", [256, LC])
    C.dft = din("dft", [128, 12 * 128], BF16)
    C.tw = din("tw", [128, 4 * 1024], BF16)
    C.out = nc.dram_tensor("out", [L, D], F32, kind="ExternalOutput").ap()
    C.x1 = scratch("x1", [T, D], F32)
    C.modrows = scratch("modrows", [2, 3 * D], F32)
    C.qkT = scratch("qkT", [1024, T], BF16)
    C.qkmT = scratch("qkmT", [512, T], BF16)
    C.qkm2 = scratch("qkm2", [512, T], BF16)
    C.hyT = scratch("hyT", [1024, T], BF16)
    C.gmT = scratch("gmT", [16, T], F32)
    C.pT = scratch("pT", [T, 1792], BF16)
    C.gT = scratch("gT", [D, T], BF16)
    C.filtT = scratch("filtT", [1024, L], F32)
    C.filtN = scratch("filtN", [1024, L], BF16)
    C.kspec = scratch("kspec", [2, 128, 256 * 256], BF16)
    C.hyV = scratch("hyV", [256, L], BF16)
    C.hyX = scratch("hyX", [512, L], BF16)
    C.hyY = scratch("hyY", [256, L], BF16)
    C.hyZ = scratch("hyZ", [256, L], BF16)
    C.filtC = scratch("filtC", [1024, LC], F32)
    C.bank = [P.ps("bank%d" % i, [128, 512], F32) for i in range(7)]
    C.psb = P.ps("psb", [128, 1024], BF16)

    phases = []
    for l in range(n_layers):
        phases += [("mods", l), ("inproj", l), ("na", l), ("mlstm", l), ("hyena", l), ("outproj", l)]
    for name, l in phases:
        if name == "mods" and l > 0:
            P.new_semset()
        fn = globals().get("phase_" + name)
        if fn is not None:
            fn(C, l)
        if stop_after == (name, l):
            break
    P.finish()
    C.n_inst = P.n_inst
    return nc, C


def xrows(C, l, tile):
    if l == 0:
        if tile < 64:
            return C.x[tile * 128:(tile + 1) * 128, :]
        return C.ctx[(tile - 64) * 128:(tile - 63) * 128, :]
    return C.x1[tile * 128:(tile + 1) * 128, :]


def phase_mods(C, l):
    P = C.P
    P.begin()
    cc = P.sb("cc", [128, 16], F32)
    sc = P.sb("sc", [128, 16], F32)
    P.dma('sp', cc[:], C.cc, w=['cc'])
    P.act(sc[:], cc[:], ACT.Silu, r=['cc'], w=['sc'])
    wa = [P.sb("wa%d" % i, [128, 3 * D], F32) for i in range(2)]
    for k in range(8):
        t = wa[k % 2]
        P.dma('sp' if k % 2 == 0 else 'act', t[:], C.w_ada[l, k * 128:(k + 1) * 128, :], w=['wa%d' % (k % 2)])
        for nb in range(6):
            P.mm(C.bank[nb][0:2, :], sc[:, 2 * k:2 * k + 2], t[:, nb * 512:(nb + 1) * 512], start=(k == 0), stop=(k == 7),
                 r=['sc', 'wa%d' % (k % 2)], w=['bank%d' % nb])
    mod = P.sb("mod", [2, 3 * D], F32)
    bb = P.sb("bb", [2, 3 * D], F32)
    gp = P.sb("gp", [2, 2 * D], F32)
    res = P.sb("res", [2, 3 * D], F32)
    P.dma('sp', bb[:], C.b_ada[l].partition_broadcast(2), w=['bb'])
    P.dma('sp', gp[:, 0:D], C.g_pre[l].partition_broadcast(2), w=['gp0'])
    P.dma('sp', gp[:, D:2 * D], C.g_post[l].partition_broadcast(2), w=['gp1'])
    for nb in range(6):
        P.tt('dve', mod[:, nb * 512:(nb + 1) * 512], C.bank[nb][0:2, :], bb[:, nb * 512:(nb + 1) * 512], ALU.add,
             r=['bank%d' % nb, 'bb'], w=['mod'])
    P.stt('dve', res[:, 0:D], mod[:, D:2 * D], 1.0, gp[:, 0:D], ALU.add, ALU.mult, r=['mod', 'gp0'], w=['res'])
    P.cp('dve', res[:, D:2 * D], mod[:, 0:D], r=['mod'], w=['res'])
    P.tt('dve', res[:, 2 * D:3 * D], mod[:, 2 * D:3 * D], gp[:, D:2 * D], ALU.mult, r=['mod', 'gp1'], w=['res'])
    P.dma('sp', C.modrows, res[:], r=['res'], w=['modrows'])
    P.end()


def phase_inproj(C, l):
    P = C.P
    P.begin()
    W = P.sb("W", [128, 8, NIN], BF16)
    wst = [P.sb("wst%d" % i, [128, NIN], F32) for i in range(2)]
    for k in range(8):
        P.dma('sp' if k % 2 == 0 else 'act', wst[k % 2][:], C.w_in[l, k * 128:(k + 1) * 128, :], w=['wst%d' % (k % 2)])
        P.cp('dve' if k % 2 == 0 else 'pool', W[:, k, :], wst[k % 2][:], r=['wst%d' % (k % 2)], w=['W'])
    gmod = P.sb("gmod", [128, 2, D], F32)
    shift = P.sb("shift", [128, 2, D], F32)
    for i in range(2):
        P.dma('sp', gmod[:, i, :], C.modrows[i:i + 1, 0:D].partition_broadcast(128), w=['gmod'])
        P.dma('act', shift[:, i, :], C.modrows[i:i + 1, D:2 * D].partition_broadcast(128), w=['shift'])
    idb = P.sb("idb", [128, 128], BF16)
    P.dma('sp', idb[:], C.ident_b, w=['idb'])
    eps = P.sb("eps", [128, 1], F32)
    P.memset('pool', eps[:], 1e-6, w=['eps'])
    xt = [P.sb("xt%d" % i, [128, D], F32) for i in range(3)]
    junk = P.sb("junk", [128, D], BF16)
    hf = [P.sb("hf%d" % i, [128, D], F32) for i in range(2)]
    hb = [P.sb("hb%d" % i, [128, D], BF16) for i in range(2)]
    st3 = [P.sb("st%d" % i, [128, 8], F32) for i in range(3)]
    hT = [P.sb("hT%d" % i, [128, 8, 512], BF16) for i in range(2)]
    stF = [P.sb("stF%d" % i, [128, 512], BF16) for i in range(3)]
    stG = P.sb("stG", [16, 512], F32)
    stT = [P.sb("stT%d" % i, [128, 1792], BF16) for i in range(2)]
    fchunks = []
    for i in range(4):
        fchunks.append((0 + i * 128, C.qkT, i * 128, 0.125))
    for i in range(4):
        fchunks.append((512 + i * 128, C.qkT, 512 + i * 128, 1.0))
    for i in range(4):
        fchunks.append((2048 + i * 128, C.qkmT, i * 128, 1.0))
    for i in range(8):
        fchunks.append((3344 + i * 128, C.hyT, i * 128, 1.0))
    tcols = [(1024, 0, 512), (1536, 512, 512), (2560, 1024, 512), (3072, 1536, 256)]
    blocks = [(b * 4, 4, 0) for b in range(16)] + [(64, 2, 1)]
    nbank = 0
    tcount = 0
    for bi, (t0, ntl, ci) in enumerate(blocks):
        h_ = hT[bi % 2]
        hk = 'hT%d' % (bi % 2)
        ncol = ntl * 128
        for i in range(ntl):
            tile = t0 + i
            xi = tcount % 3
            st = st3[xi]
            sk = 'st%d_' % xi
            tcount += 1
            P.dma('sp' if tile % 2 == 0 else 'act', xt[xi][:], xrows(C, l, tile), w=['xt%d' % xi])
            P.act(junk[:], xt[xi][:], ACT.Square, accum_out=st[:, 0:1], r=['xt%d' % xi], w=[sk + '0'])
            P.act(st[:, 1:2], st[:, 0:1], ACT.Sqrt, scale=1.0 / D, bias=eps[:, 0:1], r=[sk + '0', 'eps'], w=[sk + '1'])
            P.recip(st[:, 2:3], st[:, 1:2], r=[sk + '1'], w=[sk + '2'])
            hi = tile % 2
            P.stt('dve', hf[hi][:], xt[xi][:], st[:, 2:3], gmod[:, ci, :], ALU.mult, ALU.mult,
                  r=['xt%d' % xi, sk + '2', 'gmod'], w=['hf%d' % hi])
            P.tt('pool', hb[hi][:], hf[hi][:], shift[:, ci, :], ALU.add, r=['hf%d' % hi, 'shift'], w=['hb%d' % hi])
            for k in range(8):
                P.tr(C.psb[:, k * 128:(k + 1) * 128], hb[hi][:, k * 128:(k + 1) * 128], idb[:], r=['hb%d' % hi, 'idb'], w=['psb'])
            P.cp('act', h_[:, :, i * 128:(i + 1) * 128], C.psb[:].rearrange("p (k t) -> p k t", k=8), r=['psb'], w=[hk])
        for fi, (wc, dst, drow, scl) in enumerate(fchunks):
            bk = nbank % 4
            nbank += 1
            for k in range(8):
                P.mm(C.bank[bk][:, 0:ncol], W[:, k, wc:wc + 128], h_[:, k, 0:ncol], start=(k == 0), stop=(k == 7),
                     r=['W', hk], w=['bank%d' % bk])
            si = fi % 3
            if fi % 2 == 0:
                P.act(stF[si][:, 0:ncol], C.bank[bk][:, 0:ncol], ACT.Copy, scale=scl, r=['bank%d' % bk], w=['stF%d' % si])
            else:
                P.ts('dve', stF[si][:, 0:ncol], C.bank[bk][:, 0:ncol], scl, None, ALU.mult, r=['bank%d' % bk], w=['stF%d' % si])
            P.dma('pool', dst[drow:drow + 128, t0 * 128:t0 * 128 + ncol], stF[si][:, 0:ncol], r=['stF%d' % si], w=[])
        bk = nbank % 4
        nbank += 1
        for k in range(8):
            P.mm(C.bank[bk][0:16, 0:ncol], W[:, k, 3328:3344], h_[:, k, 0:ncol], start=(k == 0), stop=(k == 7),
                 r=['W', hk], w=['bank%d' % bk])
        P.cp('dve', stG[:, 0:ncol], C.bank[bk][0:16, 0:ncol], r=['bank%d' % bk], w=['stG'])
        P.dma('act', C.gmT[:, t0 * 128:t0 * 128 + ncol], stG[:, 0:ncol], r=['stG'], w=[])
        for i in range(ntl):
            tile = t0 + i
            so = stT[tile % 2]
            sk = 'stT%d' % (tile % 2)
            for ti, (wc, pc, wd) in enumerate(tcols):
                bk = nbank % 4
                nbank += 1
                for k in range(8):
                    P.mm(C.bank[bk][:, 0:wd], h_[:, k, i * 128:(i + 1) * 128], W[:, k, wc:wc + wd], start=(k == 0), stop=(k == 7),
                         r=['W', hk], w=['bank%d' % bk])
                if ti % 2 == 0:
                    P.cp('act', so[:, pc:pc + wd], C.bank[bk][:, 0:wd], r=['bank%d' % bk], w=[sk])
                else:
                    P.cp('dve', so[:, pc:pc + wd], C.bank[bk][:, 0:wd], r=['bank%d' % bk], w=[sk])
            P.dma('pool', C.pT[tile * 128:(tile + 1) * 128, :], so[:], r=[sk], w=[])
    P.end()


def phase_na(C, l):
    P = C.P
    P.begin()
    upd = (l < DEPTH - 1)
    nq = NT if upd else 64
    idb = P.sb("idb", [128, 128], BF16)
    P.dma('sp', idb[:], C.ident_b, w=['idb'])
    maskt = P.sb("maskt", [128, NTYPES * 128], F32)
    P.dma('act', maskt[:], C.na_mask, w=['maskt'])
    ona = P.sb("ona", [128, NT, 512], BF16)
    qT = P.sb("qT", [64, T], BF16)
    kT = P.sb("kT", [64, T], BF16)
    V1 = P.sb("V1", [128, NT, 65], BF16)
    P.memset('pool', V1[:, :, 64:65], 1.0, w=['V1'])
    biasf = P.sb("biasf", [128, NTYPES * 128], F32)
    biasb = P.sb("biasb", [128, NTYPES * 128], BF16)
    E = [P.sb("E%d" % i, [128, 7 * 128], BF16) for i in range(2)]
    rr = P.sb("rr", [128, 4], F32)
    pv = C.pT.rearrange("(t p) c -> p t c", p=128)
    def tiles_of(j):
        if j < 64:
            return na_tile_list(j) + [(64, None), (65, None)]
        return [(64, None), (65, None)]

    def head_loads(h):
        P.dma('sp', qT[:], C.qkT[h * 64:(h + 1) * 64, :], w=['qT'])
        P.dma('act', kT[:], C.qkT[512 + h * 64:512 + (h + 1) * 64, :], w=['kT'])
        P.dma('sp', V1[:, 0:33, 0:64], pv[:, 0:33, h * 64:(h + 1) * 64], w=['V1'])
        P.dma('act', V1[:, 33:NT, 0:64], pv[:, 33:NT, h * 64:(h + 1) * 64], w=['V1'])
        P.dma('sp', biasf[:], C.rpb_g[l, h], w=['biasf'])
        P.tt('dve', biasf[:], biasf[:], maskt[:], ALU.add, r=['biasf', 'maskt'], w=['biasf'])
        P.act(biasb[:], biasf[:], ACT.Exp, r=['biasf'], w=['biasb'])

    def stS(n):
        h, j = n // nq, n % nq
        if j == 0:
            head_loads(h)
        tiles = tiles_of(j)
        par = n % 2
        bA, bB = C.bank[par * 2], C.bank[par * 2 + 1]
        kA, kB = 'bank%d' % (par * 2), 'bank%d' % (par * 2 + 1)
        e_ = E[par]
        ek = 'E%d' % par
        for i, (jp, ty) in enumerate(tiles):
            bk, kk = (bA, kA) if i < 4 else (bB, kB)
            col = (i % 4) * 128
            P.mm(bk[:, col:col + 128], kT[:, jp * 128:(jp + 1) * 128], qT[:, j * 128:(j + 1) * 128],
                 start=True, stop=True, r=['kT', 'qT'], w=[kk])
        nt_ = len(tiles)
        na_ = min(nt_, 4)
        P.act(e_[:, 0:na_ * 128], bA[:, 0:na_ * 128], ACT.Exp, r=[kA], w=[ek])
        if nt_ > 4:
            P.act(e_[:, 512:nt_ * 128], bB[:, 0:(nt_ - 4) * 128], ACT.Exp, r=[kB], w=[ek])
        nl = nt_ - 2
        if nl > 0:
            ty0 = tiles[0][1]
            P.tt('dve', e_[:, 0:nl * 128], e_[:, 0:nl * 128], biasb[:, ty0 * 128:(ty0 + nl) * 128], ALU.mult, r=[ek, 'biasb'], w=[ek])

    def stPV(n):
        h, j = n // nq, n % nq
        tiles = tiles_of(j)
        par = n % 2
        bO, kO = C.bank[4 + par], 'bank%d' % (4 + par)
        e_ = E[par]
        ek = 'E%d' % par
        nt_ = len(tiles)
        for i, (jp, ty) in enumerate(tiles):
            P.mm(bO[:, 0:65], e_[:, i * 128:(i + 1) * 128], V1[:, jp, :], start=(i == 0), stop=(i == nt_ - 1),
                 r=[ek, 'V1'], w=[kO])
        P.recip(rr[:, par:par + 1], bO[:, 64:65], r=[kO], w=['rr%d' % par])
        P.ts('dve', ona[:, j, h * 64:(h + 1) * 64], bO[:, 0:64], rr[:, par:par + 1], None, ALU.mult,
             r=[kO, 'rr%d' % par], w=['ona'])

    for h_ in range(8):
        _run_pipeline([(lambda j, h_=h_: stS(h_ * nq + j), 0), (lambda j, h_=h_: stPV(h_ * nq + j), 1)], nq)
    za = [P.sb("za%d" % i, [128, 512], BF16) for i in range(2)]
    sil = [P.sb("sil%d" % i, [128, 512], F32) for i in range(2)]
    gg = [P.sb("gg%d" % i, [128, 512], BF16) for i in range(2)]
    gst = [P.sb("gst%d" % i, [128, 512], BF16) for i in range(2)]
    for j in range(nq):
        p2 = j % 2
        P.dma('sp', za[p2][:], C.pT[j * 128:(j + 1) * 128, 512:1024], w=['za%d' % p2])
        P.act(sil[p2][:], za[p2][:], ACT.Silu, r=['za%d' % p2], w=['sil%d' % p2])
        P.tt('dve', gg[p2][:], ona[:, j, :], sil[p2][:], ALU.mult, r=['ona', 'sil%d' % p2], w=['gg%d' % p2])
        for c4 in range(4):
            P.tr(C.psb[:, c4 * 128:(c4 + 1) * 128], gg[p2][:, c4 * 128:(c4 + 1) * 128], idb[:], r=['gg%d' % p2, 'idb'], w=['psb'])
        P.cp('act', gst[p2][:], C.psb[:, 0:512], r=['psb'], w=['gst%d' % p2])
        P.dma('act', C.gT[0:512, j * 128:(j + 1) * 128].rearrange("(c p) t -> p c t", p=128),
              gst[p2][:].rearrange("p (c t) -> p c t", c=4), r=['gst%d' % p2], w=[])
    P.end()


def phase_mlstm(C, l):
    P = C.P
    upd = (l < DEPTH - 1)
    P.begin()
    cw = P.sb("cw", [128, 12], F32)
    P.dma('sp', cw[:], C.conv_ml[l], w=['cw'])
    perm = P.sb("perm", [128, 128], BF16)
    P.dma('act', perm[:], C.perm_b, w=['perm'])
    cosb = [P.sb("cosb%d" % i, [128, 512], F32) for i in range(2)]
    sinb = [P.sb("sinb%d" % i, [128, 512], F32) for i in range(2)]
    raw = [P.sb("raw%d" % i, [128, 514], BF16) for i in range(3)]
    a_ = [P.sb("a%d" % i, [128, 512], F32) for i in range(2)]
    s_ = [P.sb("s%d" % i, [128, 512], F32) for i in range(2)]
    sb16 = [P.sb("sb16%d" % i, [128, 512], BF16) for i in range(2)]
    o1 = [P.sb("o1%d" % i, [128, 512], F32) for i in range(2)]
    o2 = [P.sb("o2%d" % i, [128, 512], F32) for i in range(2)]
    ob = [P.sb("ob%d" % i, [128, 512], BF16) for i in range(2)]
    blocks = [(b * 512, 512, 0, L) for b in range(16)] + [(L, 256, L, T)]
    it = 0
    for bi, (t0, n, s0, s1) in enumerate(blocks):
        lat = t0 < L
        cb, sn = cosb[bi % 2], sinb[bi % 2]
        if lat:
            P.dma('sp', cb[:], C.rope_cos[:, t0:t0 + 512], w=['cos%d' % (bi % 2)])
            P.dma('act', sn[:], C.rope_sin[:, t0:t0 + 512], w=['sin%d' % (bi % 2)])
        for c in range(4):
            ri = it % 3
            p2 = it % 2
            it += 1
            rw = raw[ri]
            rk = 'raw%d' % ri
            lo = max(t0 - 1, s0)
            hi = min(t0 + n + 1, s1)
            if lo > t0 - 1:
                P.memset('pool', rw[:, 0:1], 0.0, w=[rk])
            if hi < t0 + n + 1:
                P.memset('pool', rw[:, n + 1:n + 2], 0.0, w=[rk])
            P.dma('sp' if it % 2 == 0 else 'act', rw[:, lo - (t0 - 1):hi - (t0 - 1)], C.qkmT[c * 128:(c + 1) * 128, lo:hi], r=[rk], w=[rk])
            a, sv = a_[p2], s_[p2]
            P.ts('dve', a[:, 0:n], rw[:, 0:n], cw[:, c * 3:c * 3 + 1], None, ALU.mult, r=[rk, 'cw'], w=['a%d' % p2])
            P.stt('dve', a[:, 0:n], rw[:, 1:n + 1], cw[:, c * 3 + 1:c * 3 + 2], a[:, 0:n], ALU.mult, ALU.add, r=[rk, 'cw', 'a%d' % p2], w=['a%d' % p2])
            P.stt('dve', a[:, 0:n], rw[:, 2:n + 2], cw[:, c * 3 + 2:c * 3 + 3], a[:, 0:n], ALU.mult, ALU.add, r=[rk, 'cw', 'a%d' % p2], w=['a%d' % p2])
            P.act(sv[:, 0:n], a[:, 0:n], ACT.Silu, r=['a%d' % p2], w=['s%d' % p2])
            if c >= 2:
                P.act(sv[:, 0:n], sv[:, 0:n], ACT.Copy, scale=0.125, r=['s%d' % p2], w=['s%d' % p2])
            if lat:
                P.cp('act', sb16[p2][:, 0:n], sv[:, 0:n], r=['s%d' % p2], w=['sb16%d' % p2])
                bk = C.bank[p2]
                P.mm(bk[:, 0:n], perm[:], sb16[p2][:, 0:n], r=['perm', 'sb16%d' % p2], w=['bank%d' % p2])
                P.tt('pool', o1[p2][:, 0:n], sv[:, 0:n], cb[:, 0:n], ALU.mult, r=['s%d' % p2, 'cos%d' % (bi % 2)], w=['o1%d' % p2])
                P.tt('dve', o2[p2][:, 0:n], bk[:, 0:n], sn[:, 0:n], ALU.mult, r=['bank%d' % p2, 'sin%d' % (bi % 2)], w=['o2%d' % p2])
                P.tt('pool', ob[p2][:, 0:n], o1[p2][:, 0:n], o2[p2][:, 0:n], ALU.add, r=['o1%d' % p2, 'o2%d' % p2], w=['ob%d' % p2])
            else:
                P.cp('act', ob[p2][:, 0:n], sv[:, 0:n], r=['s%d' % p2], w=['ob%d' % p2])
            P.dma('sp', C.qkm2[c * 128:(c + 1) * 128, t0:t0 + n], ob[p2][:, 0:n], r=['ob%d' % p2], w=[])
    P.end()

    P.begin()
    cum = P.sb("cum", [8, T], F32)
    ict = P.sb("ict", [128, NT, 16], F32)
    wt = P.sb("wt", [128, NT, 16], F32)
    idf = P.sb("idf", [128, 128], F32)
    P.dma('sp', idf[:], C.ident_f, w=['idf'])
    idb = P.sb("idb", [128, 128], BF16)
    P.dma('sp', idb[:], C.ident_b, w=['idb'])
    bif = P.sb("bif", [16, 1], F32)
    P.dma('act', bif[:], C.b_if[l], w=['bif'])
    selc = P.sb("selc", [16, 4], F32)
    P.dma('act', selc[:], C.selcols, w=['selc'])
    selC = P.sb("selC", [8, 8 * 128], F32)
    P.dma('sp', selC[:], C.selC, w=['selC'])
    mlm = P.sb("mlm", [128, 2 * 512], BF16)
    P.dma('sp', mlm[:], C.ml_mask, w=['mlm'])
    NB = 1024
    gmb = P.sb("gmb", [16, NB], F32)
    e1 = P.sb("e1", [16, NB], F32)
    xa = P.sb("xa", [8, NB], F32)
    xb = P.sb("xb", [8, NB], F32)
    x0 = P.sb("x0", [8, NB], F32)
    cF = P.sb("cF", [8, NB], F32)
    A16 = P.sb("A16", [16, NB], F32)
    B16 = P.sb("B16", [16, NB], F32)
    G1 = P.sb("G1", [16, NB], F32)
    gblocks = [(b * NB, NB) for b in range(8)] + [(L, 256)]
    for (t0, n) in gblocks:
        nch = n // 128
        P.dma('sp', gmb[:, 0:n], C.gmT[:, t0:t0 + n], w=['gmb'])
        P.ts('dve', gmb[:, 0:n], gmb[:, 0:n], bif[:, 0:1], None, ALU.add, r=['gmb', 'bif'], w=['gmb'])
        P.act(e1[:, 0:n], gmb[:, 0:n], ACT.Exp, scale=-1.0, r=['gmb'], w=['e1'])
        P.act(e1[:, 0:n], e1[:, 0:n], ACT.Ln, bias=1.0, r=['e1'], w=['e1'])
        P.ts('dve', x0[:, 0:n], e1[0:8, 0:n], -1.0, None, ALU.mult, r=['e1'], w=['x0'])
        src, sk = x0, 'x0'
        pp = [(xa, 'xa'), (xb, 'xb')]
        step = 1
        i = 0
        while step < 128:
            dst, dk = pp[i % 2]
            sv = src[:, 0:n].rearrange("p (c t) -> p c t", t=128)
            dv = dst[:, 0:n].rearrange("p (c t) -> p c t", t=128)
            P.tt('dve', dv[:, :, step:], sv[:, :, step:], sv[:, :, :128 - step], ALU.add, r=[sk], w=[dk])
            P.cp('pool', dv[:, :, :step], sv[:, :, :step], r=[sk], w=[dk])
            src, sk = dst, dk
            step *= 2
            i += 1
        pv_ = src[:, 0:n].rearrange("p (c t) -> p c t", t=128)
        cfv = cF[:, 0:n].rearrange("p (c t) -> p c t", t=128)
        P.tt('dve', cfv, pv_[:, :, 127:128].broadcast_to([8, nch, 128]), pv_, ALU.subtract, r=[sk], w=['cF'])
        P.tt('dve', cF[:, 0:n], cF[:, 0:n], x0[:, 0:n], ALU.add, r=['cF', 'x0'], w=['cF'])
        P.ts('dve', cF[:, 0:n], cF[:, 0:n], selc[0:8, 3:4], None, ALU.mult, r=['cF', 'selc'], w=['cF'])
        P.stt('dve', cum[:, t0:t0 + n], src[:, 0:n], selc[0:8, 2:3], cF[:, 0:n], ALU.mult, ALU.add, r=[sk, 'selc', 'cF'], w=['cum'])
        P.memset('pool', B16[:, 0:n], 0.0, w=['B16'])
        P.dma('sp', B16[8:16, 0:n], cum[:, t0:t0 + n], r=['cum', 'B16'], w=['B16'])
        P.ts('dve', A16[:, 0:n], gmb[:, 0:n], selc[:, 0:1], selc[:, 1:2], ALU.mult, ALU.add, r=['gmb', 'selc'], w=['A16'])
        P.tt('dve', G1[:, 0:n], A16[:, 0:n], B16[:, 0:n], ALU.subtract, r=['A16', 'B16'], w=['G1'])
        for ci in range(nch):
            ch = t0 // 128 + ci
            bk = C.bank[ci % 2]
            P.tr(bk[:, 0:16], G1[:, ci * 128:(ci + 1) * 128], idf[0:16, 0:16], r=['G1', 'idf'], w=['bank%d' % (ci % 2)])
            P.cp('dve', ict[:, ch, :], bk[:, 0:16], r=['bank%d' % (ci % 2)], w=['ict'])
            P.act(wt[:, ch, :], bk[:, 0:16], ACT.Exp, r=['bank%d' % (ci % 2)], w=['wt'])

    hfwd = P.sb("hfwd", [128, NT, 256], BF16)
    Z = P.sb("Z", [64, 4, 65], F32)
    Zb = P.sb("Zb", [64, 4, 65], BF16)
    qc = [P.sb("qc%d" % i, [64, 4, 128], BF16) for i in range(3)]
    kc = [P.sb("kc%d" % i, [64, 4, 128], BF16) for i in range(3)]
    vc = [P.sb("vc%d" % i, [128, 4, 65], BF16) for i in range(3)]
    for i in range(3):
        P.memset('pool', vc[i][:, :, 64:65], 1.0, w=['vc%d' % i])
    oz = [P.sb("oz%d" % i, [128, 512], BF16) for i in range(2)]
    Dt = [P.sb("Dt%d" % i, [128, 512], BF16) for i in range(2)]
    U = [P.sb("U%d" % i, [64, 512], F32) for i in range(2)]
    St = [P.sb("St%d" % i, [128, 512], BF16) for i in range(2)]
    qs = [P.sb("qs%d" % i, [64, 4, 128], BF16) for i in range(2)]
    ks = [P.sb("ks%d" % i, [128, 4, 64], BF16) for i in range(2)]
    dd = P.sb("dd", [128, 16], F32)
    hh = [P.sb("hh%d" % i, [128, 256], F32) for i in range(2)]
    sg = [P.sb("sg%d" % i, [128, 512], F32) for i in range(2)]
    go = [P.sb("go%d" % i, [128, 256], BF16) for i in range(2)]
    gst = [P.sb("gst%d" % i, [128, 256], BF16) for i in range(2)]
    qv = C.qkm2[0:256, :].rearrange("(h d) t -> d h t", d=64)
    kv = C.qkm2[256:512, :].rearrange("(h d) t -> d h t", d=64)
    it = 0
    for dpass in range(2):
        order = ([64, 65] + list(range(64))) if dpass == 0 else ([65, 64] + list(range(63, -1, -1)))
        P.memset('pool', Z[:], 0.0, w=['Z'])
        P.memset('pool', Zb[:], 0.0, w=['Zb'])
        for c in order:
            emit = (c < 64) or upd
            b3 = it % 3
            p2 = it % 2
            it += 1
            q_, k_, v_ = qc[b3], kc[b3], vc[b3]
            qk_, kk_, vk_ = 'qc%d' % b3, 'kc%d' % b3, 'vc%d' % b3
            cs = slice(c * 128, (c + 1) * 128)
            P.dma('sp', q_[:], qv[:, :, cs], w=[qk_])
            P.dma('act', k_[:], kv[:, :, cs], w=[kk_])
            P.dma('sp', v_[:, :, 0:64], C.pT[cs, 1024:1280].rearrange("p (h d) -> p h d", d=64), w=[vk_])
            bD, kD = C.bank[p2], 'bank%d' % p2
            bS, kS = C.bank[2 + p2], 'bank%d' % (2 + p2)
            bU, kU = C.bank[4], 'bank4'
            bN, kN = C.bank[5], 'bank5'
            bZ, kZ = C.bank[6], 'bank6'
            P.mm(bD[:, :], idb[:], mlm[:, dpass * 512:(dpass + 1) * 512], start=True, stop=False, r=['idb', 'mlm'], w=[kD])
            for h in range(4):
                dh = dpass * 4 + h
                P.mm(bD[:, h * 128:(h + 1) * 128], selC[:, dh * 128:(dh + 1) * 128], cum[:, cs], start=False, stop=True,
                     r=['selC', 'cum'], w=[kD])
            for h in range(4):
                dh = dpass * 4 + h
                P.mm(bU[0:64, h * 128:(h + 1) * 128], selC[:, dh * 128:dh * 128 + 64], cum[:, cs], start=True, stop=True,
                     r=['selC', 'cum'], w=[kU])
            for h in range(4):
                P.mm(bS[:, h * 128:(h + 1) * 128], k_[:, h, :], q_[:, h, :], start=True, stop=True, r=[kk_, qk_], w=[kS])
            for h in range(4):
                P.tr(C.psb[:, h * 64:(h + 1) * 64], k_[:, h, :], idb[0:64, 0:64], r=[kk_, 'idb'], w=['psb'])
            d_, u_, s2, q2, k2 = Dt[p2], U[p2], St[p2], qs[p2], ks[p2]
            for h in range(4):
                dh = dpass * 4 + h
                P.act(d_[:, h * 128:(h + 1) * 128], bD[:, h * 128:(h + 1) * 128], ACT.Exp, bias=ict[:, c, 8 + dh:9 + dh],
                      r=[kD, 'ict'], w=['Dt%d' % p2])
            P.act(u_[:], bU[0:64, :], ACT.Exp, r=[kU], w=['U%d' % p2])
            P.tt('dve', s2[:], bS[:, :], d_[:], ALU.mult, r=[kS, 'Dt%d' % p2], w=['St%d' % p2])
            P.tt('pool', q2[:], q_[:], u_[:].rearrange("p (h t) -> p h t", h=4), ALU.mult, r=[qk_, 'U%d' % p2], w=['qs%d' % p2])
            P.tt('dve', k2[:], C.psb[:, 0:256].rearrange("p (h d) -> p h d", d=64),
                 wt[:, c, 8 + dpass * 4:12 + dpass * 4].unsqueeze(2).broadcast_to([128, 4, 64]), ALU.mult,
                 r=['psb', 'wt'], w=['ks%d' % p2])
            if emit:
                for h in range(4):
                    P.mm(bN[:, h * 65:(h + 1) * 65], s2[:, h * 128:(h + 1) * 128], v_[:, h, :], start=True, stop=False,
                         r=['St%d' % p2, vk_], w=[kN])
                    P.mm(bN[:, h * 65:(h + 1) * 65], q2[:, h, :], Zb[:, h, :], start=False, stop=True,
                         r=['qs%d' % p2, 'Zb'], w=[kN])
                nv = bN[:, 0:260].rearrange("p (h e) -> p h e", e=65)
                P.cp('dve', dd[:, 0:4], nv[:, :, 64], r=[kN], w=['dd'])
                P.stt('dve', dd[:, 4:8], dd[:, 0:4], -1.0, dd[:, 0:4], ALU.mult, ALU.max, r=['dd'], w=['dd'])
                P.ts('dve', dd[:, 4:8], dd[:, 4:8], 1.0, None, ALU.max, r=['dd'], w=['dd'])
                P.op('dve', lambda e: e.reciprocal(dd[:, 8:12], dd[:, 4:8]), r=['dd'], w=['dd'])
                hv = hh[p2][:].rearrange("p (h d) -> p h d", d=64)
                P.tt('dve', hv, nv[:, :, 0:64], dd[:, 8:12].unsqueeze(2).broadcast_to([128, 4, 64]), ALU.mult, r=[kN, 'dd'], w=['hh%d' % p2])
                if dpass == 0:
                    P.cp('pool', hfwd[:, c, :], hh[p2][:], r=['hh%d' % p2], w=['hfwd'])
                else:
                    P.dma('act', oz[p2][:], C.pT[cs, 1280:1792], w=['oz%d' % p2])
                    P.act(sg[p2][:, 0:256], oz[p2][:, 0:256], ACT.Sigmoid, r=['oz%d' % p2], w=['sg%d' % p2])
                    P.act(sg[p2][:, 256:512], oz[p2][:, 256:512], ACT.Silu, r=['oz%d' % p2], w=['sg%d' % p2])
                    P.tt('pool', hh[p2][:], hh[p2][:], hfwd[:, c, :], ALU.add, r=['hh%d' % p2, 'hfwd'], w=['hh%d' % p2])
                    P.tt('pool', hh[p2][:], hh[p2][:], sg[p2][:, 0:256], ALU.mult, r=['hh%d' % p2, 'sg%d' % p2], w=['hh%d' % p2])
                    P.tt('pool', go[p2][:], hh[p2][:], sg[p2][:, 256:512], ALU.mult, r=['hh%d' % p2, 'sg%d' % p2], w=['go%d' % p2])
                    for c2 in range(2):
                        P.tr(C.psb[:, 512 + c2 * 128:512 + (c2 + 1) * 128], go[p2][:, c2 * 128:(c2 + 1) * 128], idb[:], r=['go%d' % p2, 'idb'], w=['psb2'])
                    P.cp('act', gst[p2][:], C.psb[:, 512:768], r=['psb2'], w=['gst%d' % p2])
                    P.dma('sp', C.gT[512:768, cs].rearrange("(c p) t -> p c t", p=128), gst[p2][:].rearrange("p (c t) -> p c t", c=2),
                          r=['gst%d' % p2], w=[])
            for h in range(4):
                P.mm(bZ[0:64, h * 65:(h + 1) * 65], k2[:, h, :], v_[:, h, :], start=True, stop=True, r=['ks%d' % p2, vk_], w=[kZ])
            P.tt('dve', Z[:], Z[:], bZ[0:64, 0:260].rearrange("p (h e) -> p h e", e=65), ALU.add, r=['Z', kZ], w=['Z'])
            ecol = 127 if dpass == 0 else 0
            ev = u_[:].rearrange("p (h t) -> p h t", h=4)[:, :, ecol:ecol + 1].broadcast_to([64, 4, 65])
            P.tt('dve', Z[:], Z[:], ev, ALU.mult, r=['Z', 'U%d' % p2], w=['Z'])
            P.cp('act', Zb[:], Z[:], r=['Z'], w=['Zb'])
    P.end()


def _sin_mlp_block(P, C, ps, bcol, fcol, out, n, tag, tmp, pskey):
    pre, t2 = tmp
    P.ts('dve', pre[:, 0:n], ps, bcol, fcol, ALU.add, ALU.mult, r=[pskey, 'hfc'], w=[tag + 'pre'])
    P.ts('pool', t2[:, 0:n], pre[:, 0:n], -1.0, PI, ALU.mult, ALU.add, r=[tag + 'pre'], w=[tag + 't2'])
    P.tt('dve', t2[:, 0:n], pre[:, 0:n], t2[:, 0:n], ALU.min, r=[tag + 'pre', tag + 't2'], w=[tag + 't2'])
    P.ts('dve', pre[:, 0:n], pre[:, 0:n], -1.0, -PI, ALU.mult, ALU.add, r=[tag + 'pre'], w=[tag + 'pre'])
    P.tt('dve', t2[:, 0:n], t2[:, 0:n], pre[:, 0:n], ALU.max, r=[tag + 'pre', tag + 't2'], w=[tag + 't2'])
    P.act(out[:, 0:n], t2[:, 0:n], ACT.Sin, r=[tag + 't2'], w=[tag + 'out'])


def _hy_filters(C, l, Ln, featT, decayT, dstF):
    P = C.P
    w1 = P.sb("w1", [33, 64], F32); w2 = P.sb("w2", [64, 64], F32); w3 = P.sb("w3", [64, 1024], F32)
    hfc = P.sb("hfc", [64, 4], F32)
    P.dma('sp', w1[:], C.hf_w1[l], w=['hfw'])
    P.dma('sp', w2[:], C.hf_w2[l], w=['hfw'])
    P.dma('act', w3[:], C.hf_w3[l], w=['hfw'])
    P.dma('sp', hfc[:, 0:1], C.hf_b1[l], w=['hfc'])
    P.dma('sp', hfc[:, 1:2], C.hf_b2[l], w=['hfc'])
    P.dma('sp', hfc[:, 2:4], C.hf_freq[l], w=['hfc'])
    nb = max(Ln // 512, 1)
    bw = min(Ln, 512)
    ssq = P.sb("ssq", [128, 8, 16], F32)
    P.memset('pool', ssq[:], 0.0, w=['ssq'])
    ft = [P.sb("ft%d" % i, [33, 512], F32) for i in range(3)]
    dec = [P.sb("dec%d" % i, [128, 2, 512], F32) for i in range(3)]
    pre1 = [P.sb("pre1%d" % i, [64, 512], F32) for i in range(2)]
    t21 = [P.sb("t21%d" % i, [64, 512], F32) for i in range(2)]
    a1 = [P.sb("a1%d" % i, [64, 512], F32) for i in range(2)]
    pre2 = [P.sb("pre2%d" % i, [64, 512], F32) for i in range(2)]
    t22 = [P.sb("t22%d" % i, [64, 512], F32) for i in range(2)]
    a2 = [P.sb("a2%d" % i, [64, 512], F32) for i in range(2)]
    fl = [P.sb("fl%d" % i, [128, 512], F32) for i in range(4)]
    junk = P.sb("junkf", [128, 512], BF16)

    def st1(b):
        p3, p2 = b % 3, b % 2
        cs = slice(b * bw, (b + 1) * bw)
        P.dma('sp', ft[p3][:, 0:bw], featT[:, cs], w=['ft%d' % p3])
        P.dma('act', dec[p3][:, :, 0:bw], decayT[:, cs].rearrange("(h p) t -> p h t", p=128), w=['dec%d' % p3])
        P.mm(C.bank[0][0:64, 0:bw], w1[:], ft[p3][:, 0:bw], r=['hfw', 'ft%d' % p3], w=['bank0'])
        _sin_mlp_block(P, C, C.bank[0][0:64, 0:bw], hfc[:, 0:1], hfc[:, 2:3], a1[p2], bw, 'm1%d' % p2, (pre1[p2], t21[p2]), 'bank0')

    def st2(b):
        p2 = b % 2
        P.mm(C.bank[1][0:64, 0:bw], w2[:], a1[p2][:, 0:bw], r=['hfw', 'm1%dout' % p2], w=['bank1'])
        _sin_mlp_block(P, C, C.bank[1][0:64, 0:bw], hfc[:, 1:2], hfc[:, 3:4], a2[p2], bw, 'm2%d' % p2, (pre2[p2], t22[p2]), 'bank1')

    def st3(b):
        p3, p2 = b % 3, b % 2
        cs = slice(b * bw, (b + 1) * bw)
        for cc in range(8):
            bk = C.bank[2 + cc % 4]
            bkk = 'bank%d' % (2 + cc % 4)
            f3 = (b * 8 + cc) % 4
            P.mm(bk[:, 0:bw], w3[:, cc * 128:(cc + 1) * 128], a2[p2][:, 0:bw], r=['hfw', 'm2%dout' % p2], w=[bkk])
            P.tt('dve', fl[f3][:, 0:bw], bk[:, 0:bw], dec[p3][:, cc % 2, 0:bw], ALU.mult, r=[bkk, 'dec%d' % p3], w=['fl%d' % f3])
            if b == 0 and (cc // 2) % 2 == 1:
                P.memset('pool', fl[f3][:, 0:1], 0.0, w=['fl%d' % f3])
            P.act(junk[:, 0:bw], fl[f3][:, 0:bw], ACT.Square, accum_out=ssq[:, cc, b:b + 1], r=['fl%d' % f3], w=['ssq'])
            P.dma('sp', dstF[cc * 128:(cc + 1) * 128, cs], fl[f3][:, 0:bw], r=['fl%d' % f3], w=[])

    _run_pipeline([(st1, 0), (st2, 1), (st3, 2)], nb)
    tot = P.sb("tot", [128, 8], F32)
    nrm = P.sb("nrm", [128, 8], F32)
    P.op('dve', lambda e: e.tensor_reduce(tot[:], ssq[:], AX.X, ALU.add), r=['ssq'], w=['tot'])
    tv = tot[:].rearrange("p (o d h) -> p o d h", o=2, d=2)
    nv = nrm[:, 0:4].rearrange("p (o h) -> p o h", o=2)
    P.tt('dve', nv, tv[:, :, 0, :], tv[:, :, 1, :], ALU.add, r=['tot'], w=['nrm'])
    P.ts('dve', nrm[:, 0:4], nrm[:, 0:4], 1e-6, None, ALU.add, r=['nrm'], w=['nrm'])
    P.act(nrm[:, 0:4], nrm[:, 0:4], ACT.Sqrt, r=['nrm'], w=['nrm'])
    P.op('dve', lambda e: e.reciprocal(nrm[:, 4:8], nrm[:, 0:4]), r=['nrm'], w=['nrm'])
    return nrm


def _fft_S1(P, C, xg, xk, dft, banks):
    for c in range(4):
        bi = banks[c // 2]
        P.mm(C.bank[bi][:, (c % 2) * 256:(c % 2) * 256 + 256], xg[0:64, c, :], dft[0:64, 0:256], r=[xk, 'dft'], w=['bank%d' % bi])


def _fft_S3(P, C, dft, Bt, bkey, banks):
    Bv = Bt[:].rearrange("p (c r k) -> p c r k", c=4, r=2)
    Br, Bi = Bv[:, :, 0, :], Bv[:, :, 1, :]
    r_, i_ = banks
    P.mm(C.bank[r_][:, :], dft[:, 2 * 128:3 * 128], Br, start=True, stop=False, r=['dft', bkey], w=['bank%d' % r_])
    P.mm(C.bank[r_][:, :], dft[:, 3 * 128:4 * 128], Bi, start=False, stop=True, r=['dft', bkey], w=['bank%d' % r_])
    P.mm(C.bank[i_][:, :], dft[:, 4 * 128:5 * 128], Br, start=True, stop=False, r=['dft', bkey], w=['bank%d' % i_])
    P.mm(C.bank[i_][:, :], dft[:, 5 * 128:6 * 128], Bi, start=False, stop=True, r=['dft', bkey], w=['bank%d' % i_])


def _twiddle(P, C, banks, tw, which, Bt, bkey, tmp, tkey):
    TA = tw[:, (2 * which) * 1024:(2 * which) * 1024 + 512].rearrange("p (c r k) -> p c r k", c=2, r=2)
    TB = tw[:, (2 * which + 1) * 1024:(2 * which + 1) * 1024 + 512].rearrange("p (c r k) -> p c r k", c=2, r=2)
    for hb in range(2):
        bi = banks[hb]
        bkk = 'bank%d' % bi
        t1, t2 = tmp[hb]
        k1, k2 = tkey + 'a%d' % hb, tkey + 'b%d' % hb
        A = C.bank[bi][:, :].rearrange("p (c r k) -> p c r k", c=2, r=2)
        T1 = t1[:].rearrange("p (c r k) -> p c r k", c=2, r=2)
        T2 = t2[:].rearrange("p (c r k) -> p c r k", c=2, r=2)
        P.tt('dve', T1, A, TA, ALU.mult, r=[bkk, 'tw'], w=[k1])
        P.tt('dve', T2[:, :, 0, :], A[:, :, 1, :], TB[:, :, 0, :], ALU.mult, r=[bkk, 'tw'], w=[k2])
        P.tt('dve', T2[:, :, 1, :], A[:, :, 0, :], TB[:, :, 1, :], ALU.mult, r=[bkk, 'tw'], w=[k2])
        P.tt('pool', Bt[:, hb * 512:(hb + 1) * 512], t1[:], t2[:], ALU.add, r=[k1, k2], w=[bkey])


def _fft_tables(C):
    P = C.P
    dft = P.sb("dft", [128, 12 * 128], BF16)
    tw = P.sb("tw", [128, 4 * 1024], BF16)
    P.dma('sp', dft[:], C.dft, w=['dft'])
    P.dma('act', tw[:], C.tw, w=['tw'])
    return dft, tw


def _tw_tmp(P, name):
    return [[(P.sb("%st1_%d%d" % (name, p, h), [128, 512], F32), P.sb("%st2_%d%d" % (name, p, h), [128, 512], F32)) for h in range(2)] for p in range(2)]


def _run_pipeline(stages, ng):
    maxlag = max(l for _, l in stages)
    for t in range(ng + maxlag):
        for fn, lag in stages:
            g = t - lag
            if 0 <= g < ng:
                fn(g)


def _fftconv(C, src, order, dst, ng=64, stage=9):
    P = C.P
    P.begin()
    dft, tw = _fft_tables(C)
    xin = [P.sb("xin%d" % i, [64, 16, 128], BF16) for i in range(2)]
    kin = [P.sb("kin%d" % i, [128, 1024], BF16) for i in range(2)]
    Bt = [P.sb("Bt%d" % i, [128, 1024], BF16) for i in range(2)]
    Ht = [P.sb("Ht%d" % i, [128, 1024], BF16) for i in range(2)]
    Yt = [P.sb("Yt%d" % i, [128, 1024], BF16) for i in range(2)]
    pp = [[P.sb("pp%d%d" % (p, i), [128, 512], F32) for i in range(4)] for p in range(2)]
    yst = [P.sb("yst%d" % i, [64, 16, 128], BF16) for i in range(2)]
    tmpB = _tw_tmp(P, "tB")
    tmpF = _tw_tmp(P, "tF")
    sv = src.rearrange("c (a b) -> a c b", b=128)
    dv = dst.rearrange("c (a b) -> a c b", b=128)

    def stA(g):
        ch0, x16, p2 = g * 4, (g // 4) % 2, g % 2
        if g % 4 == 0:
            P.dma('sp', xin[x16][:], sv[:, ch0:ch0 + 16, :], w=['xin%d' % x16])
        P.dma('act', kin[p2][:], C.kspec[order, :, ch0 * 256:(ch0 + 4) * 256], w=['kin%d' % p2])
        xg = xin[x16][:, (g % 4) * 4:(g % 4) * 4 + 4, :]
        _fft_S1(P, C, xg, 'xin%d' % x16, dft, (0, 1))

    def stB(g):
        p2 = g % 2
        _twiddle(P, C, (0, 1), tw, 0, Bt[p2], 'B%d' % p2, tmpB[p2], 'tB%d' % p2)

    def stC(g):
        p2 = g % 2
        _fft_S3(P, C, dft, Bt[p2], 'B%d' % p2, (2, 3))

    def stD(g):
        p2 = g % 2
        Kv = kin[p2][:].rearrange("p (c r k) -> p c r k", c=4, r=2)
        Kr, Ki = Kv[:, :, 0, :], Kv[:, :, 1, :]
        Xr = C.bank[2][:, :].rearrange("p (c k) -> p c k", c=4)
        Xi = C.bank[3][:, :].rearrange("p (c k) -> p c k", c=4)
        ppv = [t[:].rearrange("p (c k) -> p c k", c=4) for t in pp[p2]]
        pk = ['pp%d%d' % (p2, i) for i in range(4)]
        P.tt('dve', ppv[0], Xr, Kr, ALU.mult, r=['bank2', 'kin%d' % p2], w=[pk[0]])
        P.tt('dve', ppv[1], Xi, Ki, ALU.mult, r=['bank3', 'kin%d' % p2], w=[pk[1]])
        P.tt('dve', ppv[2], Xr, Ki, ALU.mult, r=['bank2', 'kin%d' % p2], w=[pk[2]])
        P.tt('dve', ppv[3], Xi, Kr, ALU.mult, r=['bank3', 'kin%d' % p2], w=[pk[3]])
        Y = Yt[p2]
        P.tt('pool', Y[:, 0:512], pp[p2][0][:], pp[p2][1][:], ALU.subtract, r=[pk[0], pk[1]], w=['Y%d' % p2])
        P.tt('pool', Y[:, 512:1024], pp[p2][2][:], pp[p2][3][:], ALU.add, r=[pk[2], pk[3]], w=['Y%d' % p2])

    def stE(g):
        p2 = g % 2
        Y = Yt[p2]
        for c in range(4):
            bi = 4 + c // 2
            o_ = C.bank[bi][:, (c % 2) * 256:(c % 2) * 256 + 256]
            P.mm(o_, Y[:, c * 128:(c + 1) * 128], dft[:, 6 * 128:8 * 128], start=True, stop=False, r=['Y%d' % p2, 'dft'], w=['bank%d' % bi])
            P.mm(o_, Y[:, 512 + c * 128:512 + (c + 1) * 128], dft[:, 8 * 128:10 * 128], start=False, stop=True, r=['Y%d' % p2, 'dft'], w=['bank%d' % bi])

    def stF(g):
        p2 = g % 2
        _twiddle(P, C, (4, 5), tw, 1, Ht[p2], 'H%d' % p2, tmpF[p2], 'tF%d' % p2)

    def stG(g):
        ch0, x16, p2 = g * 4, (g // 4) % 2, g % 2
        Hv = Ht[p2][:].rearrange("p (c r k) -> p c r k", c=4, r=2)
        P.mm(C.bank[6][0:64, :], dft[:, 10 * 128:10 * 128 + 64], Hv[:, :, 0, :], start=True, stop=False, r=['dft', 'H%d' % p2], w=['bank6'])
        P.mm(C.bank[6][0:64, :], dft[:, 11 * 128:11 * 128 + 64], Hv[:, :, 1, :], start=False, stop=True, r=['dft', 'H%d' % p2], w=['bank6'])
        P.cp('act', yst[x16][:, (g % 4) * 4:(g % 4) * 4 + 4, :], C.bank[6][0:64, :].rearrange("p (c k) -> p c k", c=4), r=['bank6'], w=['yst%d' % x16])
        if g % 4 == 3:
            P.dma('sp', dv[:, ch0 - 12:ch0 + 4, :], yst[x16][:], r=['yst%d' % x16], w=[])

    _run_pipeline([(stA, 0), (stC, 1), (stE, 2), (stG, 3), (stB, 0), (stD, 1), (stF, 2)], ng)
    P.end()


def _conv3_fm(P, rw, rk, cw, col, out, ok, n, eng='dve'):
    P.ts(eng, out[:, 0:n], rw[:, 0:n], cw[:, col:col + 1], None, ALU.mult, r=[rk, 'cwh'], w=[ok])
    P.stt(eng, out[:, 0:n], rw[:, 1:n + 1], cw[:, col + 1:col + 2], out[:, 0:n], ALU.mult, ALU.add, r=[rk, 'cwh', ok], w=[ok])
    P.stt(eng, out[:, 0:n], rw[:, 2:n + 2], cw[:, col + 2:col + 3], out[:, 0:n], ALU.mult, ALU.add, r=[rk, 'cwh', ok], w=[ok])


_DBG_STOP = None


def phase_hyena(C, l):
    P = C.P
    upd = (l < DEPTH - 1)
    P.begin()
    nrm = _hy_filters(C, l, L, C.featT, C.decayT, C.filtT)
    P.barrier_dram = True
    fin = [P.sb("fin%d" % i, [128, 2048], F32) for i in range(2)]
    fob = [P.sb("fob%d" % i, [128, 2048], BF16) for i in range(2)]
    P.barrier()
    it = 0
    for cc in range(8):
        o, half = cc // 4, cc % 2
        for b in range(4):
            p2 = it % 2
            it += 1
            cs = slice(b * 2048, (b + 1) * 2048)
            P.dma('sp', fin[p2][:], C.filtT[cc * 128:(cc + 1) * 128, cs], w=['fin%d' % p2])
            P.ts('dve' if it % 2 else 'pool', fob[p2][:], fin[p2][:], nrm[:, 4 + o * 2 + half:5 + o * 2 + half], None, ALU.mult,
                 r=['fin%d' % p2, 'nrm'], w=['fob%d' % p2])
            P.dma('act', C.filtN[cc * 128:(cc + 1) * 128, cs], fob[p2][:], r=['fob%d' % p2], w=[])
    P.end()
    if _DBG_STOP == 'E0':
        return
    P.begin()
    dft, tw = _fft_tables(C)
    xin = [P.sb("xin%d" % i, [64, 16, 128], BF16) for i in range(2)]
    Bt = [P.sb("Bt%d" % i, [128, 1024], BF16) for i in range(3)]
    tb = [[P.sb("tb%d%d" % (p, i), [128, 256], F32) for i in range(2)] for p in range(2)]
    kout = [P.sb("kout%d" % i, [128, 8, 256], BF16) for i in range(2)]
    tmpB = _tw_tmp(P, "tB")
    fv = C.filtN.rearrange("c (a b) -> a c b", b=128)
    for o in range(2):
        def stA(g, o=o):
            ch0, x16 = g * 2, (g // 4) % 2
            if g % 4 == 0:
                for q in range(4):
                    P.dma('sp', xin[x16][:, q * 4:q * 4 + 2, :], fv[:, o * 512 + ch0 + 2 * q:o * 512 + ch0 + 2 * q + 2, :], w=['xin%d' % x16])
                    P.dma('act', xin[x16][:, q * 4 + 2:q * 4 + 4, :], fv[:, o * 512 + 256 + ch0 + 2 * q:o * 512 + 256 + ch0 + 2 * q + 2, :], w=['xin%d' % x16])
            xg = xin[x16][:, (g % 4) * 4:(g % 4) * 4 + 4, :]
            sb = (0, 1) if g % 2 == 0 else (4, 5)
            _fft_S1(P, C, xg, 'xin%d' % x16, dft, sb)

        def stB(g):
            p2 = g % 2
            sb = (0, 1) if g % 2 == 0 else (4, 5)
            _twiddle(P, C, sb, tw, 0, Bt[g % 3], 'B%d' % (g % 3), tmpB[p2], 'tB%d' % p2)

        def stC(g):
            _fft_S3(P, C, dft, Bt[g % 3], 'B%d' % (g % 3), (2, 3))

        def stD(g, o=o):
            ch0, x16, p2 = g * 2, (g // 4) % 2, g % 2
            P.cp('act', tb[p2][0][:], C.bank[2][:, 256:512], r=['bank2'], w=['tb%d0' % p2])
            P.cp('act', tb[p2][1][:], C.bank[3][:, 256:512], r=['bank3'], w=['tb%d1' % p2])
            ko = kout[x16][:, (g % 4) * 2:(g % 4) * 2 + 2, :].rearrange("p c (r k) -> p c r k", r=2)
            P.tt('dve', ko[:, :, 0, :], C.bank[2][:, 0:256].rearrange("p (c k) -> p c k", c=2),
                 tb[p2][0][:].rearrange("p (c k) -> p c k", c=2), ALU.add, r=['bank2', 'tb%d0' % p2], w=['kout%d' % x16])
            P.tt('dve', ko[:, :, 1, :], C.bank[3][:, 0:256].rearrange("p (c k) -> p c k", c=2),
                 tb[p2][1][:].rearrange("p (c k) -> p c k", c=2), ALU.subtract, r=['bank3', 'tb%d1' % p2], w=['kout%d' % x16])
            if g % 4 == 3:
                P.dma('sp', C.kspec[o, :, (ch0 - 6) * 256:(ch0 + 2) * 256], kout[x16][:].rearrange("p c k -> p (c k)"), r=['kout%d' % x16], w=[])

        _run_pipeline([(stA, 0), (stC, 2), (stB, 0), (stD, 2)], 128)
    P.end()
    if _DBG_STOP == 'E0b':
        return
    P.begin()
    cwh = P.sb("cwh", [128, 18], F32)
    P.dma('sp', cwh[:], C.conv_hy[l], w=['cwh'])
    raw = [P.sb("raw%d" % i, [128, 2050], BF16) for i in range(3)]
    cf = [P.sb("cf%d" % i, [128, 2048], F32) for i in range(2)]
    cb = [P.sb("cb%d" % i, [128, 2048], BF16) for i in range(2)]
    it = 0
    for b in range(4):
        t0 = b * 2048
        for c in range(6):
            ri, p2 = it % 3, it % 2
            it += 1
            rw, rk = raw[ri], 'raw%d' % ri
            lo, hi = max(t0 - 1, 0), min(t0 + 2049, L)
            if lo > t0 - 1:
                P.memset('pool', rw[:, 0:1], 0.0, w=[rk])
            if hi < t0 + 2049:
                P.memset('pool', rw[:, 2049:2050], 0.0, w=[rk])
            P.dma('sp' if it % 2 else 'act', rw[:, lo - (t0 - 1):hi - (t0 - 1)], C.hyT[c * 128:(c + 1) * 128, lo:hi], r=[rk], w=[rk])
            _conv3_fm(P, rw, rk, cwh, c * 3, cf[p2], 'cf%d' % p2, 2048)
            P.cp('act', cb[p2][:], cf[p2][:], r=['cf%d' % p2], w=['cb%d' % p2])
            dst = C.hyV[c * 128:(c + 1) * 128, t0:t0 + 2048] if c < 2 else C.hyX[(c - 2) * 128:(c - 1) * 128, t0:t0 + 2048]
            P.dma('sp', dst, cb[p2][:], r=['cb%d' % p2], w=[])
    P.end()
    if _DBG_STOP == 'E1':
        return
    _fftconv(C, C.hyV, 0, C.hyY)
    if _DBG_STOP == 'E2':
        return
    P.begin()
    hbias = P.sb("hbias", [128, 4], F32)
    P.dma('sp', hbias[:], C.hy_bias[l], w=['hbias'])
    ty = [P.sb("ty%d" % i, [128, 2048], BF16) for i in range(2)]
    tv = [P.sb("tv%d" % i, [128, 2048], BF16) for i in range(2)]
    tx = [P.sb("tx%d" % i, [128, 2048], BF16) for i in range(2)]
    tf = [P.sb("tf%d" % i, [128, 2048], F32) for i in range(2)]
    tz = [P.sb("tz%d" % i, [128, 2048], BF16) for i in range(2)]
    it = 0
    for half in range(2):
        rs = slice(half * 128, (half + 1) * 128)
        for b in range(4):
            p2 = it % 2
            it += 1
            cs = slice(b * 2048, (b + 1) * 2048)
            P.dma('sp', ty[p2][:], C.hyY[rs, cs], w=['ty%d' % p2])
            P.dma('act', tv[p2][:], C.hyV[rs, cs], w=['tv%d' % p2])
            P.dma('sp', tx[p2][:], C.hyX[rs, cs], w=['tx%d' % p2])
            P.stt('dve', tf[p2][:], tv[p2][:], hbias[:, half:half + 1], ty[p2][:], ALU.mult, ALU.add, r=['tv%d' % p2, 'ty%d' % p2, 'hbias'], w=['tf%d' % p2])
            P.tt('pool', tz[p2][:], tf[p2][:], tx[p2][:], ALU.mult, r=['tf%d' % p2, 'tx%d' % p2], w=['tz%d' % p2])
            P.dma('act', C.hyZ[rs, cs], tz[p2][:], r=['tz%d' % p2], w=[])
    P.end()
    _fftconv(C, C.hyZ, 1, C.hyY)
    P.begin()
    hbias = P.sb("hbias", [128, 4], F32)
    P.dma('sp', hbias[:], C.hy_bias[l], w=['hbias'])
    ty = [P.sb("ty%d" % i, [128, 2048], BF16) for i in range(2)]
    tv = [P.sb("tv%d" % i, [128, 2048], BF16) for i in range(2)]
    tx = [P.sb("tx%d" % i, [128, 2048], BF16) for i in range(2)]
    tg = [P.sb("tg%d" % i, [128, 2048], BF16) for i in range(2)]
    tf = [P.sb("tf%d" % i, [128, 2048], F32) for i in range(2)]
    ts_ = [P.sb("tsl%d" % i, [128, 2048], F32) for i in range(2)]
    tz = [P.sb("tz%d" % i, [128, 2048], BF16) for i in range(2)]
    it = 0
    for half in range(2):
        rs = slice(half * 128, (half + 1) * 128)
        for b in range(4):
            p2 = it % 2
            it += 1
            cs = slice(b * 2048, (b + 1) * 2048)
            P.dma('sp', ty[p2][:], C.hyY[rs, cs], w=['ty%d' % p2])
            P.dma('act', tv[p2][:], C.hyZ[rs, cs], w=['tv%d' % p2])
            P.dma('sp', tx[p2][:], C.hyX[256 + half * 128:256 + (half + 1) * 128, cs], w=['tx%d' % p2])
            P.dma('act', tg[p2][:], C.hyT[768 + half * 128:768 + (half + 1) * 128, cs], w=['tg%d' % p2])
            P.act(ts_[p2][:], tg[p2][:], ACT.Silu, r=['tg%d' % p2], w=['tsl%d' % p2])
            P.stt('dve', tf[p2][:], tv[p2][:], hbias[:, 2 + half:3 + half], ty[p2][:], ALU.mult, ALU.add, r=['tv%d' % p2, 'ty%d' % p2, 'hbias'], w=['tf%d' % p2])
            P.tt('pool', tf[p2][:], tf[p2][:], tx[p2][:], ALU.mult, r=['tf%d' % p2, 'tx%d' % p2], w=['tf%d' % p2])
            P.tt('dve', tz[p2][:], tf[p2][:], ts_[p2][:], ALU.mult, r=['tf%d' % p2, 'tsl%d' % p2], w=['tz%d' % p2])
            P.dma('sp', C.gT[768 + half * 128:768 + (half + 1) * 128, cs], tz[p2][:], r=['tz%d' % p2], w=[])
    P.end()
    if upd:
        _hyena_ctx(C, l)


def _hyena_ctx(C, l):
    P = C.P
    P.begin()
    nrm = _hy_filters(C, l, LC, C.featTc, C.decayTc, C.filtC)
    P.barrier()
    cwh = P.sb("cwh", [128, 18], F32)
    P.dma('sp', cwh[:], C.conv_hy[l], w=['cwh'])
    hbias = P.sb("hbias", [128, 4], F32)
    P.dma('sp', hbias[:], C.hy_bias[l], w=['hbias'])
    for half in range(2):
        fk = []
        for o in range(2):
            for d in range(2):
                cc = o * 4 + d * 2 + half
                t = P.sb("fk%d%d%d" % (half, o, d), [128, LC], F32)
                P.dma('sp', t[:], C.filtC[cc * 128:(cc + 1) * 128, :], w=['fk%d%d' % (o, d)])
                P.ts('dve', t[:], t[:], nrm[:, 4 + o * 2 + half:5 + o * 2 + half], None, ALU.mult, r=['fk%d%d' % (o, d), 'nrm'], w=['fk%d%d' % (o, d)])
                fk.append(t)
        u = []
        for c3 in range(3):
            rw = P.sb("rwc%d%d" % (half, c3), [128, LC + 2], BF16)
            P.memset('pool', rw[:, 0:1], 0.0, w=['rwc%d' % c3])
            P.memset('pool', rw[:, LC + 1:LC + 2], 0.0, w=['rwc%d' % c3])
            chunk = c3 * 2 + half
            P.dma('sp', rw[:, 1:LC + 1], C.hyT[chunk * 128:(chunk + 1) * 128, L:T], r=['rwc%d' % c3], w=['rwc%d' % c3])
            t = P.sb("uc%d%d" % (half, c3), [128, LC], F32)
            _conv3_fm(P, rw, 'rwc%d' % c3, cwh, chunk * 3, t, 'uc%d' % c3, LC)
            u.append(t)
        zin = u[0]
        zk = 'uc0'
        for o in range(2):
            fw, bw_ = fk[o * 2], fk[o * 2 + 1]
            NA_ = 4
            accs = [P.sb("accd%d%d%d" % (half, o, i), [128, LC], F32) for i in range(2 * NA_)]
            accp = [P.sb("accp%d%d%d" % (half, o, i), [128, LC], F32) for i in range(2)]
            tmpp = [P.sb("tmpp%d%d%d" % (half, o, i), [128, LC], F32) for i in range(2)]
            accf = accs[0]
            P.ts('dve', accs[0][:], zin[:], fw[:, 0:1], None, ALU.mult, r=[zk, 'fk%d0' % o], w=['ad0'])
            for i in range(1, 2 * NA_):
                P.memset('pool', accs[i][:], 0.0, w=['ad%d' % i])
            for i in range(2):
                P.memset('pool', accp[i][:], 0.0, w=['ap%d' % i])
            npool = 0
            for m in range(1, LC):
                ia = m % NA_
                P.stt('dve', accs[ia][:, m:], zin[:, :LC - m], fw[:, m:m + 1], accs[ia][:, m:], ALU.mult, ALU.add, r=[zk, 'fk%d0' % o, 'ad%d' % ia], w=['ad%d' % ia])
                if m % 4 == 0:
                    ip = npool % 2
                    npool += 1
                    P.ts('pool', tmpp[ip][:, :LC - m], zin[:, m:], bw_[:, m:m + 1], None, ALU.mult, r=[zk, 'fk%d1' % o], w=['tp%d' % ip])
                    P.tt('pool', accp[ip][:, :LC - m], accp[ip][:, :LC - m], tmpp[ip][:, :LC - m], ALU.add, r=['tp%d' % ip, 'ap%d' % ip], w=['ap%d' % ip])
                else:
                    ib = NA_ + m % NA_
                    P.stt('dve', accs[ib][:, :LC - m], zin[:, m:], bw_[:, m:m + 1], accs[ib][:, :LC - m], ALU.mult, ALU.add, r=[zk, 'fk%d1' % o, 'ad%d' % ib], w=['ad%d' % ib])
            for i in range(1, 2 * NA_):
                P.tt('dve', accf[:], accf[:], accs[i][:], ALU.add, r=['ad0', 'ad%d' % i], w=['ad0'])
            for i in range(2):
                P.tt('dve', accf[:], accf[:], accp[i][:], ALU.add, r=['ad0', 'ap%d' % i], w=['ad0'])
            P.stt('dve', accf[:], zin[:], hbias[:, o * 2 + half:o * 2 + half + 1], accf[:], ALU.mult, ALU.add, r=[zk, 'hbias', 'ad0'], w=['ad0'])
            znew = P.sb("zn%d%d" % (half, o), [128, LC], F32)
            P.tt('dve', znew[:], accf[:], u[1 + o][:], ALU.mult, r=['ad0', 'uc%d' % (1 + o)], w=['zn%d' % o])
            zin, zk = znew, 'zn%d' % o
        zg = P.sb("zg%d" % half, [128, LC], BF16)
        sl = P.sb("slc%d" % half, [128, LC], F32)
        ob = P.sb("obc%d" % half, [128, LC], BF16)
        P.dma('act', zg[:], C.hyT[768 + half * 128:768 + (half + 1) * 128, L:T], w=['zg'])
        P.act(sl[:], zg[:], ACT.Silu, r=['zg'], w=['slc'])
        P.tt('dve', ob[:], zin[:], sl[:], ALU.mult, r=[zk, 'slc'], w=['obc'])
        P.dma('sp', C.gT[768 + half * 128:768 + (half + 1) * 128, L:T], ob[:], r=['obc'], w=[])
    P.end()


def phase_outproj(C, l):
    P = C.P
    P.begin()
    upd = (l < DEPTH - 1)
    ntl = NT if upd else 64
    W = P.sb("Wo", [128, 8, D], BF16)
    wst = [P.sb("wst%d" % i, [128, D], F32) for i in range(2)]
    for k in range(8):
        P.dma('sp' if k % 2 == 0 else 'act', wst[k % 2][:], C.w_out[l, k * 128:(k + 1) * 128, :], w=['wst%d' % (k % 2)])
        P.cp('dve' if k % 2 == 0 else 'pool', W[:, k, :], wst[k % 2][:], r=['wst%d' % (k % 2)], w=['Wo'])
    ggate = P.sb("ggate", [128, 2, D], F32)
    for i in range(2):
        P.dma('sp', ggate[:, i, :], C.modrows[i:i + 1, 2 * D:3 * D].partition_broadcast(128), w=['ggate'])
    eps = P.sb("eps", [128, 1], F32)
    P.memset('pool', eps[:], 1e-6, w=['eps'])
    gt = [P.sb("gt%d" % i, [128, 8, 128], BF16) for i in range(2)]
    xt = [P.sb("xt%d" % i, [128, D], F32) for i in range(2)]
    yt = [P.sb("yt%d" % i, [128, D], F32) for i in range(2)]
    junk = P.sb("junk", [128, D], BF16)
    st2 = [P.sb("st%d" % i, [128, 8], F32) for i in range(2)]
    gv = C.gT.rearrange("(k p) t -> p k t", p=128)
    for tile in range(ntl):
        p2 = tile % 2
        st = st2[p2]
        sk = 'st%d_' % p2
        ci = 0 if tile < 64 else 1
        P.dma('sp', gt[p2][:], gv[:, :, tile * 128:(tile + 1) * 128], w=['gt%d' % p2])
        P.dma('act', xt[p2][:], xrows(C, l, tile), w=['xt%d' % p2])
        b0, b1 = C.bank[p2 * 2], C.bank[p2 * 2 + 1]
        k0, k1 = 'bank%d' % (p2 * 2), 'bank%d' % (p2 * 2 + 1)
        for half, (bk, kk) in enumerate(((b0, k0), (b1, k1))):
            for k in range(8):
                P.mm(bk[:, :], gt[p2][:, k, :], W[:, k, half * 512:(half + 1) * 512], start=(k == 0), stop=(k == 7),
                     r=['gt%d' % p2, 'Wo'], w=[kk])
        P.act(junk[:, 0:512], b0[:, :], ACT.Square, accum_out=st[:, 0:1], r=[k0], w=[sk + '0'])
        P.act(junk[:, 512:1024], b1[:, :], ACT.Square, accum_out=st[:, 1:2], r=[k1], w=[sk + '1'])
        P.tt('dve', st[:, 2:3], st[:, 0:1], st[:, 1:2], ALU.add, r=[sk + '0', sk + '1'], w=[sk + '2'])
        P.act(st[:, 3:4], st[:, 2:3], ACT.Sqrt, scale=1.0 / D, bias=eps[:, 0:1], r=[sk + '2', 'eps'], w=[sk + '3'])
        P.recip(st[:, 4:5], st[:, 3:4], r=[sk + '3'], w=[sk + '4'])
        P.stt('dve', yt[p2][:, 0:512], b0[:, :], st[:, 4:5], ggate[:, ci, 0:512], ALU.mult, ALU.mult, r=[k0, sk + '4', 'ggate'], w=['yt%d' % p2])
        P.stt('dve', yt[p2][:, 512:1024], b1[:, :], st[:, 4:5], ggate[:, ci, 512:1024], ALU.mult, ALU.mult, r=[k1, sk + '4', 'ggate'], w=['yt%d' % p2])
        P.tt('pool', yt[p2][:], yt[p2][:], xt[p2][:], ALU.add, r=['yt%d' % p2, 'xt%d' % p2], w=['yt%d' % p2])
        if upd:
            dst = C.x1[tile * 128:(tile + 1) * 128, :]
        else:
            dst = C.out[tile * 128:(tile + 1) * 128, :]
        P.dma('pool', dst, yt[p2][:], r=['yt%d' % p2], w=[])
    P.end()


def _bf16(a):
    import ml_dtypes
    return np.ascontiguousarray(a).astype(ml_dtypes.bfloat16)


_CONSTS = None


def host_consts():
    global _CONSTS
    if _CONSTS is not None:
        return _CONSTS
    f32 = np.float32
    c = {}
    c["ident_f"] = np.eye(128, dtype=f32)
    c["ident_b"] = _bf16(np.eye(128, dtype=f32))
    pairs = na_type_pairs()
    idx_r = np.zeros((NTYPES, 128, 128), np.int64)
    idx_c = np.zeros((NTYPES, 128, 128), np.int64)
    mask = np.zeros((NTYPES, 128, 128), f32)
    a = np.arange(128)
    krow_in, kcol = a // 64, a % 64
    for ty, (j, jp) in enumerate(pairs):
        qr = 2 * j + krow_in
        kr = 2 * jp + krow_in
        rs = np.clip(qr - 4, 0, 120)
        cs = np.clip(kcol - 8, 0, 48)
        dr = kr[:, None] - qr[None, :]
        okr = (kr[:, None] >= rs[None, :]) & (kr[:, None] < rs[None, :] + 8)
        okc = (kcol[:, None] >= cs[None, :]) & (kcol[:, None] < cs[None, :] + 16)
        ok = okr & okc
        idx_r[ty] = np.clip(dr + 7, 0, 14)
        idx_c[ty] = np.clip(kcol[:, None] - kcol[None, :], -15, 15) + 15
        mask[ty] = np.where(ok, 0.0, NEG)
    c["_na_idx_r"], c["_na_idx_c"] = idx_r, idx_c
    c["na_mask"] = np.ascontiguousarray(mask.transpose(1, 0, 2).reshape(128, NTYPES * 128))
    f = np.arange(128)
    jj = f % 64
    ax, half, n = jj // 32, (jj % 32) // 16, jj % 16
    inv = (10000.0 ** (-(np.arange(16, dtype=f32)) / 16)).astype(f32)
    t = np.arange(L)
    pos = np.stack([t // 64, t % 64], 0).astype(f32)
    ang = pos[ax, :] * inv[n][:, None]
    c["rope_cos"] = np.cos(ang).astype(f32)
    c["rope_sin"] = (np.where(half == 0, -1.0, 1.0)[:, None] * np.sin(ang)).astype(f32)
    partner = np.where(half == 0, f + 16, f - 16)
    perm = np.zeros((128, 128), f32)
    perm[partner, f] = 1.0
    c["perm_b"] = _bf16(perm)
    s_, t_ = np.meshgrid(np.arange(128), np.arange(128), indexing="ij")
    mf = np.where(s_ <= t_, 0.0, NEG).astype(f32)
    mb = np.where(s_ >= t_, 0.0, NEG).astype(f32)
    c["ml_mask"] = _bf16(np.concatenate([np.tile(mf, (1, 4)), np.tile(mb, (1, 4))], axis=1))
    selC = np.zeros((8, 8, 128), f32)
    for dh in range(8):
        selC[dh, dh, :] = 1.0
    c["selC"] = selC.reshape(8, 8 * 128)
    selC2 = np.zeros((40, 8, 128), f32)
    for dh in range(8):
        selC2[dh, dh, :] = 1.0
        selC2[32 + dh, dh, :] = 1.0
    c["selC2"] = _bf16(selC2.reshape(40, 8 * 128))
    sc = np.zeros((16, 4), f32)
    sc[8:, 0] = 1.0
    sc[:8, 1] = 1.0
    sc[0:4, 2] = 1.0
    sc[4:8, 3] = 1.0
    c["selcols"] = sc
    def feats(Ln):
        tt = np.arange(Ln, dtype=f32)
        tn = tt / f32(Ln - 1)
        fr = np.linspace(1e-4, 15.0, 16, dtype=f32)
        an = (f32(2.0 * np.pi / Ln) * tt[:, None] * fr[None, :]).astype(f32)
        feat = np.concatenate([tn[:, None], np.cos(an), -np.sin(an)], axis=-1).astype(f32)
        deltas = np.abs(np.linspace(np.log(1e-2) / 1.5, np.log(1e-2) / 0.3, 256, dtype=f32))
        dec = np.exp(-tn[:, None] * deltas[None, :]).astype(f32)
        return np.ascontiguousarray(feat.T), np.ascontiguousarray(dec.T)
    c["featT"], c["decayT"] = feats(L)
    c["featTc"], c["decayTc"] = feats(LC)
    k = np.arange(128)
    th = 2.0 * np.pi * np.outer(k, k) / 128.0
    Cm, Sm = np.cos(th), np.sin(th)
    N2 = 2 * L
    dft = np.zeros((128, 12, 128), np.float64)
    dft[:, 0], dft[:, 1] = Cm, -Sm
    dft[:, 2], dft[:, 3] = Cm, Sm
    dft[:, 4], dft[:, 5] = -Sm, Cm
    dft[:, 6], dft[:, 7] = Cm, Sm
    dft[:, 8], dft[:, 9] = -Sm, Cm
    dft[:, 10], dft[:, 11] = Cm / N2, -Sm / N2
    c["dft"] = _bf16(dft.reshape(128, 12 * 128).astype(f32))
    tht = 2.0 * np.pi * np.outer(k, k) / N2
    twr, tws = np.cos(tht), np.sin(tht)
    def rep(a0, a1):
        return np.tile(np.stack([a0, a1], 1)[:, None], (1, 4, 1, 1)).reshape(128, 1024)
    tw = np.concatenate([rep(twr, twr), rep(tws, -tws), rep(twr, twr), rep(-tws, tws)], axis=1)
    c["tw"] = _bf16(tw.astype(f32))
    _CONSTS = c
    return c


CONST_KEYS = ["ident_f", "ident_b", "na_mask", "rope_cos", "rope_sin", "perm_b", "ml_mask", "selC", "selC2", "selcols",
              "featT", "decayT", "featTc", "decayTc", "dft", "tw"]


def layout_inputs(inp):
    c = host_consts()
    f32 = np.float32
    shared = {k: c[k] for k in CONST_KEYS}
    shared["w_ada"] = np.ascontiguousarray(inp["w_ada"], f32)
    shared["b_ada"] = np.ascontiguousarray(inp["b_ada"], f32).reshape(DEPTH, 1, 3 * D)
    shared["g_pre"] = np.ascontiguousarray(inp["g_pre"], f32).reshape(DEPTH, 1, D)
    shared["g_post"] = np.ascontiguousarray(inp["g_post"], f32).reshape(DEPTH, 1, D)
    w_in = np.array(inp["w_in"], f32, copy=True)
    gm = w_in[:, :, 3328:3344].reshape(DEPTH, D, 2, 2, 4).copy()
    w_in[:, :, 3328:3336] = gm[:, :, :, 1, :].reshape(DEPTH, D, 8)
    w_in[:, :, 3336:3344] = gm[:, :, :, 0, :].reshape(DEPTH, D, 8)
    shared["w_in"] = w_in
    b_if = np.asarray(inp["b_if"], f32)
    shared["b_if"] = np.concatenate([b_if[:, :, 1, :].reshape(DEPTH, 8), b_if[:, :, 0, :].reshape(DEPTH, 8)], 1).reshape(DEPTH, 16, 1).copy()
    cm = np.asarray(inp["conv_ml"], f32)
    shared["conv_ml"] = np.ascontiguousarray(cm.reshape(DEPTH, 3, 4, 128).transpose(0, 3, 2, 1)).reshape(DEPTH, 128, 12)
    ch = np.asarray(inp["conv_hy"], f32)
    shared["conv_hy"] = np.ascontiguousarray(ch.reshape(DEPTH, 3, 6, 128).transpose(0, 3, 2, 1)).reshape(DEPTH, 128, 18)
    rpb = np.asarray(inp["rpb"], f32)
    g = rpb[:, :, c["_na_idx_r"], c["_na_idx_c"]]
    shared["rpb_g"] = np.ascontiguousarray(g.transpose(0, 1, 3, 2, 4)).reshape(DEPTH, 8, 128, NTYPES * 128)
    shared["hf_w1"] = np.ascontiguousarray(inp["hf_w1"], f32)
    shared["hf_b1"] = np.ascontiguousarray(inp["hf_b1"], f32).reshape(DEPTH, 64, 1)
    shared["hf_w2"] = np.ascontiguousarray(inp["hf_w2"], f32)
    shared["hf_b2"] = np.ascontiguousarray(inp["hf_b2"], f32).reshape(DEPTH, 64, 1)
    shared["hf_w3"] = np.ascontiguousarray(inp["hf_w3"], f32)
    shared["hf_freq"] = np.ascontiguousarray(np.asarray(inp["hf_freq"], f32).transpose(0, 2, 1))
    hb = np.asarray(inp["hy_bias"], f32)
    shared["hy_bias"] = np.ascontiguousarray(hb.reshape(DEPTH, 2, 2, 128).transpose(0, 3, 1, 2)).reshape(DEPTH, 128, 4)
    shared["w_out"] = np.ascontiguousarray(inp["w_out"], f32)
    maps = []
    x = np.asarray(inp["x"], f32)
    ctx = np.asarray(inp["ctx"], f32)
    cvec = np.asarray(inp["c"], f32)
    cctx = np.asarray(inp["c_ctx"], f32)
    for b in range(x.shape[0]):
        m = dict(shared)
        m["x"] = np.ascontiguousarray(x[b])
        m["ctx"] = np.ascontiguousarray(ctx[b])
        cc = np.stack([cvec[b].reshape(8, 128).T, cctx.reshape(8, 128).T], axis=-1)
        m["cc"] = np.ascontiguousarray(cc.reshape(128, 16))
        maps.append(m)
    return maps


_PROG = None


def kernel(**inputs):
    global _PROG
    if _PROG is None:
        _PROG = build_program()
    nc, _ = _PROG
    maps = layout_inputs(inputs)
    res = run_bass_kernel_spmd(nc, maps, core_ids=list(range(len(maps))))
    return np.stack([np.asarray(r["out"], np.float32) for r in res.results], axis=0)
```

```python
import contextlib
import numpy as np
import concourse.bass as bass
import concourse.mybir as mybir
from concourse.bass_utils import run_bass_kernel_spmd

F32 = mybir.dt.float32
BF16 = mybir.dt.bfloat16
ACT = mybir.ActivationFunctionType
ALU = mybir.AluOpType
AX = mybir.AxisListType

NDMA_SEMS = 6


class Prog:
    ENGS = ('pe', 'act', 'dve', 'pool', 'sp')

    def __init__(self, nc):
        self.nc = nc
        self.stack = contextlib.ExitStack()
        self.streams = {e: [] for e in self.ENGS}
        self.new_semset()
        self.waited = {e: {} for e in self.ENGS}
        self.lastw = {}
        self.readers = {}
        self.n_inst = 0

    def new_semset(self):
        self._setid = getattr(self, "_setid", -1) + 1
        t = "_%d" % self._setid
        self.sem = {}
        self.cnt = {}
        for e in self.ENGS:
            self.sem[e] = self.stack.enter_context(self.nc.semaphore("s_" + e + t))
            self.cnt[e] = 0
        self.dsem = {}
        self.dcnt = {}
        self.dnext = {}
        for q in ('sp', 'pool', 'act'):
            self.dsem[q] = [self.stack.enter_context(self.nc.semaphore("d_%s%d%s" % (q, i, t))) for i in range(NDMA_SEMS)]
            self.dcnt[q] = [0] * NDMA_SEMS
            self.dnext[q] = 0
        self.waited = {e: {} for e in self.ENGS}

    def begin(self):
        self.pstack = contextlib.ExitStack()

    def sb(self, name, shape, dtype):
        self._uid = getattr(self, "_uid", 0) + 1
        return self.pstack.enter_context(self.nc.sbuf_tensor("%s_%d" % (name, self._uid), list(shape), dtype))

    def ps(self, name, shape, dtype):
        return self.stack.enter_context(self.nc.psum_tensor(name, list(shape), dtype))

    def _need(self, eng, ev):
        sem, val, name = ev
        w = self.waited[eng]
        if w.get(name, 0) >= val:
            return
        w[name] = val
        self.streams[eng].append(lambda e, sem=sem, val=val: e.wait_ge(sem, val))

    def _deps(self, eng, r, w, skip_same=False):
        evs = []
        for k in r:
            if k in self.lastw:
                evs.append(self.lastw[k])
        for k in w:
            if k in self.lastw:
                evs.append(self.lastw[k])
            evs.extend(self.readers.get(k, ()))
        for ev in evs:
            if skip_same and ev[2] == eng:
                continue
            self._need(eng, ev)

    def _record(self, ev, r, w):
        for k in w:
            self.lastw[k] = ev
            self.readers[k] = []
        for k in r:
            if k in w:
                continue
            self.readers.setdefault(k, []).append(ev)

    def op(self, eng, fn, r=(), w=()):
        self._deps(eng, r, w, skip_same=(eng == 'pe'))
        self.cnt[eng] += 1
        sem, val = self.sem[eng], self.cnt[eng]
        self.streams[eng].append(lambda e, fn=fn, sem=sem: fn(e).then_inc(sem, 1))
        self._record((sem, val, eng), r, w)
        self.n_inst += 1

    def dma(self, q, out, in_, r=(), w=(), **kw):
        self._deps(q, r, w)
        i = self.dnext[q]
        self.dnext[q] = (i + 1) % NDMA_SEMS
        sem = self.dsem[q][i]
        name = "d_%s%d" % (q, i)
        if self.dcnt[q][i] > 0:
            self._need(q, (sem, self.dcnt[q][i], name))
        self.dcnt[q][i] += 16
        val = self.dcnt[q][i]
        self.streams[q].append(lambda e, out=out, in_=in_, sem=sem, kw=kw: e.dma_start(out=out, in_=in_, **kw).then_inc(sem, 16))
        self._record((sem, val, name), r, w)
        self.n_inst += 1

    def barrier(self):
        evs = []
        for q in self.dsem:
            for i in range(NDMA_SEMS):
                if self.dcnt[q][i]:
                    evs.append((self.dsem[q][i], self.dcnt[q][i], "d_%s%d" % (q, i)))
        for e in self.ENGS:
            if self.cnt[e]:
                evs.append((self.sem[e], self.cnt[e], e))
        for e in self.ENGS:
            for ev in evs:
                if ev[2] != e:
                    self._need(e, ev)
        self.lastw = {}
        self.readers = {}

    def end(self):
        self.barrier()
        streams = self.streams
        with self.nc.Block() as block:
            @block.tensor
            def _(eng):
                for f in streams['pe']:
                    f(eng)

            @block.scalar
            def _(eng):
                for f in streams['act']:
                    f(eng)

            @block.vector
            def _(eng):
                for f in streams['dve']:
                    f(eng)

            @block.gpsimd
            def _(eng):
                for f in streams['pool']:
                    f(eng)

            @block.sync
            def _(eng):
                for f in streams['sp']:
                    f(eng)
        self.streams = {e: [] for e in self.ENGS}
        self.pstack.close()

    def finish(self):
        self.stack.close()

    def mm(self, out, lhsT, rhs, start=True, stop=True, r=(), w=()):
        self.op('pe', lambda e: e.matmul(out, lhsT, rhs, start=start, stop=stop), r, w)

    def tr(self, out, in_, ident, r=(), w=()):
        self.op('pe', lambda e: e.transpose(out, in_, ident), r, w)

    def act(self, out, in_, func, r=(), w=(), **kw):
        self.op('act', lambda e: e.activation(out, in_, func, **kw), r, w)

    def ts(self, eng, out, in0, s1, s2, op0, op1=None, r=(), w=()):
        if op1 is None:
            self.op(eng, lambda e: e.tensor_scalar(out, in0, s1, None, op0), r, w)
        else:
            self.op(eng, lambda e: e.tensor_scalar(out, in0, s1, s2, op0, op1), r, w)

    def tt(self, eng, out, in0, in1, op, r=(), w=()):
        self.op(eng, lambda e: e.tensor_tensor(out, in0, in1, op), r, w)

    def stt(self, eng, out, in0, sc, in1, op0, op1, r=(), w=()):
        self.op(eng, lambda e: e.scalar_tensor_tensor(out, in0, sc, in1, op0, op1), r, w)

    def cp(self, eng, out, in_, r=(), w=()):
        if eng == 'act':
            self.op(eng, lambda e: e.copy(out, in_), r, w)
        else:
            self.op(eng, lambda e: e.tensor_copy(out, in_), r, w)

    def recip(self, out, in_, r=(), w=()):
        self.op('dve', lambda e: e.reciprocal(out, in_), r, w)

    def memset(self, eng, ap, val, w=()):
        self.op(eng, lambda e: e.memset(ap, val), (), w)


L = 8192
LC = 256
T = L + LC
NT = T // 128
D = 1024
NIN = 4368
DEPTH = 2
NTYPES = 21
NEG = -30000.0
PI = float(np.pi)


def na_tile_list(j):
    if j == 0:
        return [(jp, 5 + jp) for jp in range(4)]
    if j == 1:
        return [(jp, 9 + jp) for jp in range(4)]
    if j == 62:
        return [(60 + i, 13 + i) for i in range(4)]
    if j == 63:
        return [(60 + i, 17 + i) for i in range(4)]
    return [(j + d, d + 2) for d in range(-2, 3)]


def na_type_pairs():
    reps = {}
    for j in [10, 0, 1, 62, 63]:
        for jp, ty in na_tile_list(j):
            reps[ty] = (j, jp)
    return [reps[t] for t in range(NTYPES)]


class Ctx:
    pass


def build_program(n_layers=DEPTH, stop_after=None, debug=()):
    nc = bass.Bass("TRN2", target_bir_lowering=False)
    P = Prog(nc)
    C = Ctx()
    C.nc, C.P = nc, P

    def din(name, shape, dt=F32):
        return nc.dram_tensor(name, list(shape), dt, kind="ExternalInput").ap()

    def scratch(name, shape, dt):
        kind = "ExternalOutput" if name in debug else "Internal"
        return nc.dram_tensor(name, list(shape), dt, kind=kind).ap()

    C.x = din("x", [L, D]); C.ctx = din("ctx", [LC, D]); C.cc = din("cc", [128, 16])
    C.w_ada = din("w_ada", [DEPTH, D, 3 * D]); C.b_ada = din("b_ada", [DEPTH, 1, 3 * D])
    C.g_pre = din("g_pre", [DEPTH, 1, D]); C.g_post = din("g_post", [DEPTH, 1, D])
    C.w_in = din("w_in", [DEPTH, D, NIN]); C.b_if = din("b_if", [DEPTH, 16, 1])
    C.conv_ml = din("conv_ml", [DEPTH, 128, 12]); C.conv_hy = din("conv_hy", [DEPTH, 128, 18])
    C.rpb_g = din("rpb_g", [DEPTH, 8, 128, NTYPES * 128])
    C.hf_w1 = din("hf_w1", [DEPTH, 33, 64]); C.hf_b1 = din("hf_b1", [DEPTH, 64, 1])
    C.hf_w2 = din("hf_w2", [DEPTH, 64, 64]); C.hf_b2 = din("hf_b2", [DEPTH, 64, 1])
    C.hf_w3 = din("hf_w3", [DEPTH, 64, 1024]); C.hf_freq = din("hf_freq", [DEPTH, 64, 2])
    C.hy_bias = din("hy_bias", [DEPTH, 128, 4]); C.w_out = din("w_out", [DEPTH, D, D])
    C.ident_f = din("ident_f", [128, 128]); C.ident_b = din("ident_b", [128, 128], BF16)
    C.na_mask = din("na_mask", [128, NTYPES * 128])
    C.rope_cos = din("rope_cos", [128, L]); C.rope_sin = din("rope_sin", [128, L])
    C.perm_b = din("perm_b", [128, 128], BF16)
    C.ml_mask = din("ml_mask", [128, 2 * 512], BF16)
    C.selC = din("selC", [8, 8 * 128]); C.selcols = din("selcols", [16, 4]); C.selC2 = din("selC2", [40, 8 * 128], BF16)
    C.featT = din("featT", [33, L]); C.decayT = din("decayT", [256, L])
    C.featTc = din("featTc", [33, LC]); C.decayTc = din("decayTc", [256, LC])
    C.dft = din("dft", [128, 12 * 128], BF16)
    C.tw = din("tw", [128, 4 * 1024], BF16)
    C.out = nc.dram_tensor("out", [L, D], F32, kind="ExternalOutput").ap()
    C.x1 = scratch("x1", [T, D], F32)
    C.modrows = scratch("modrows", [2, 3 * D], F32)
    C.qkT = scratch("qkT", [1024, T], BF16)
    C.qkmT = scratch("qkmT", [512, T], BF16)
    C.qkm2 = scratch("qkm2", [512, T], BF16)
    C.hyT = scratch("hyT", [1024, T], BF16)
    C.gmT = scratch("gmT", [16, T], F32)
    C.pT = scratch("pT", [T, 1792], BF16)
    C.gT = scratch("gT", [D, T], BF16)
    C.filtT = scratch("filtT", [1024, L], F32)
    C.filtN = scratch("filtN", [1024, L], BF16)
    C.kspec = scratch("kspec", [2, 128, 256 * 256], BF16)
    C.hyV = scratch("hyV", [256, L], BF16)
    C.hyX = scratch("hyX", [512, L], BF16)
    C.hyY = scratch("hyY", [256, L], BF16)
    C.hyZ = scratch("hyZ", [256, L], BF16)
    C.filtC = scratch("filtC", [1024, LC], F32)
    C.bank = [P.ps("bank%d" % i, [128, 512], F32) for i in range(7)]
    C.psb = P.ps("psb", [128, 1024], BF16)

    phases = []
    for l in range(n_layers):
        phases += [("mods", l), ("inproj", l), ("na", l), ("mlstm", l), ("hyena", l), ("outproj", l)]
    for name, l in phases:
        if name == "mods" and l > 0:
            P.new_semset()
        fn = globals().get("phase_" + name)
        if fn is not None:
            fn(C, l)
        if stop_after == (name, l):
            break
    P.finish()
    C.n_inst = P.n_inst
    return nc, C


def xrows(C, l, tile):
    if l == 0:
        if tile < 64:
            return C.x[tile * 128:(tile + 1) * 128, :]
        return C.ctx[(tile - 64) * 128:(tile - 63) * 128, :]
    return C.x1[tile * 128:(tile + 1) * 128, :]


def phase_mods(C, l):
    P = C.P
    P.begin()
    cc = P.sb("cc", [128, 16], F32)
    sc = P.sb("sc", [128, 16], F32)
    P.dma('sp', cc[:], C.cc, w=['cc'])
    P.act(sc[:], cc[:], ACT.Silu, r=['cc'], w=['sc'])
    wa = [P.sb("wa%d" % i, [128, 3 * D], F32) for i in range(2)]
    for k in range(8):
        t = wa[k % 2]
        P.dma('sp' if k % 2 == 0 else 'act', t[:], C.w_ada[l, k * 128:(k + 1) * 128, :], w=['wa%d' % (k % 2)])
        for nb in range(6):
            P.mm(C.bank[nb][0:2, :], sc[:, 2 * k:2 * k + 2], t[:, nb * 512:(nb + 1) * 512], start=(k == 0), stop=(k == 7),
                 r=['sc', 'wa%d' % (k % 2)], w=['bank%d' % nb])
    mod = P.sb("mod", [2, 3 * D], F32)
    bb = P.sb("bb", [2, 3 * D], F32)
    gp = P.sb("gp", [2, 2 * D], F32)
    res = P.sb("res", [2, 3 * D], F32)
    P.dma('sp', bb[:], C.b_ada[l].partition_broadcast(2), w=['bb'])
    P.dma('sp', gp[:, 0:D], C.g_pre[l].partition_broadcast(2), w=['gp0'])
    P.dma('sp', gp[:, D:2 * D], C.g_post[l].partition_broadcast(2), w=['gp1'])
    for nb in range(6):
        P.tt('dve', mod[:, nb * 512:(nb + 1) * 512], C.bank[nb][0:2, :], bb[:, nb * 512:(nb + 1) * 512], ALU.add,
             r=['bank%d' % nb, 'bb'], w=['mod'])
    P.stt('dve', res[:, 0:D], mod[:, D:2 * D], 1.0, gp[:, 0:D], ALU.add, ALU.mult, r=['mod', 'gp0'], w=['res'])
    P.cp('dve', res[:, D:2 * D], mod[:, 0:D], r=['mod'], w=['res'])
    P.tt('dve', res[:, 2 * D:3 * D], mod[:, 2 * D:3 * D], gp[:, D:2 * D], ALU.mult, r=['mod', 'gp1'], w=['res'])
    P.dma('sp', C.modrows, res[:], r=['res'], w=['modrows'])
    P.end()


def phase_inproj(C, l):
    P = C.P
    P.begin()
    W = P.sb("W", [128, 8, NIN], BF16)
    wst = [P.sb("wst%d" % i, [128, NIN], F32) for i in range(2)]
    for k in range(8):
        P.dma('sp' if k % 2 == 0 else 'act', wst[k % 2][:], C.w_in[l, k * 128:(k + 1) * 128, :], w=['wst%d' % (k % 2)])
        P.cp('dve' if k % 2 == 0 else 'pool', W[:, k, :], wst[k % 2][:], r=['wst%d' % (k % 2)], w=['W'])
    gmod = P.sb("gmod", [128, 2, D], F32)
    shift = P.sb("shift", [128, 2, D], F32)
    for i in range(2):
        P.dma('sp', gmod[:, i, :], C.modrows[i:i + 1, 0:D].partition_broadcast(128), w=['gmod'])
        P.dma('act', shift[:, i, :], C.modrows[i:i + 1, D:2 * D].partition_broadcast(128), w=['shift'])
    idb = P.sb("idb", [128, 128], BF16)
    P.dma('sp', idb[:], C.ident_b, w=['idb'])
    eps = P.sb("eps", [128, 1], F32)
    P.memset('pool', eps[:], 1e-6, w=['eps'])
    xt = [P.sb("xt%d" % i, [128, D], F32) for i in range(3)]
    junk = P.sb("junk", [128, D], BF16)
    hf = [P.sb("hf%d" % i, [128, D], F32) for i in range(2)]
    hb = [P.sb("hb%d" % i, [128, D], BF16) for i in range(2)]
    st3 = [P.sb("st%d" % i, [128, 8], F32) for i in range(3)]
    hT = [P.sb("hT%d" % i, [128, 8, 512], BF16) for i in range(2)]
    stF = [P.sb("stF%d" % i, [128, 512], BF16) for i in range(3)]
    stG = P.sb("stG", [16, 512], F32)
    stT = [P.sb("stT%d" % i, [128, 1792], BF16) for i in range(2)]
    fchunks = []
    for i in range(4):
        fchunks.append((0 + i * 128, C.qkT, i * 128, 0.125))
    for i in range(4):
        fchunks.append((512 + i * 128, C.qkT, 512 + i * 128, 1.0))
    for i in range(4):
        fchunks.append((2048 + i * 128, C.qkmT, i * 128, 1.0))
    for i in range(8):
        fchunks.append((3344 + i * 128, C.hyT, i * 128, 1.0))
    tcols = [(1024, 0, 512), (1536, 512, 512), (2560, 1024, 512), (3072, 1536, 256)]
    blocks = [(b * 4, 4, 0) for b in range(16)] + [(64, 2, 1)]
    nbank = 0
    tcount = 0
    for bi, (t0, ntl, ci) in enumerate(blocks):
        h_ = hT[bi % 2]
        hk = 'hT%d' % (bi % 2)
        ncol = ntl * 128
        for i in range(ntl):
            tile = t0 + i
            xi = tcount % 3
            st = st3[xi]
            sk = 'st%d_' % xi
            tcount += 1
            P.dma('sp' if tile % 2 == 0 else 'act', xt[xi][:], xrows(C, l, tile), w=['xt%d' % xi])
            P.act(junk[:], xt[xi][:], ACT.Square, accum_out=st[:, 0:1], r=['xt%d' % xi], w=[sk + '0'])
            P.act(st[:, 1:2], st[:, 0:1], ACT.Sqrt, scale=1.0 / D, bias=eps[:, 0:1], r=[sk + '0', 'eps'], w=[sk + '1'])
            P.recip(st[:, 2:3], st[:, 1:2], r=[sk + '1'], w=[sk + '2'])
            hi = tile % 2
            P.stt('dve', hf[hi][:], xt[xi][:], st[:, 2:3], gmod[:, ci, :], ALU.mult, ALU.mult,
                  r=['xt%d' % xi, sk + '2', 'gmod'], w=['hf%d' % hi])
            P.tt('pool', hb[hi][:], hf[hi][:], shift[:, ci, :], ALU.add, r=['hf%d' % hi, 'shift'], w=['hb%d' % hi])
            for k in range(8):
                P.tr(C.psb[:, k * 128:(k + 1) * 128], hb[hi][:, k * 128:(k + 1) * 128], idb[:], r=['hb%d' % hi, 'idb'], w=['psb'])
            P.cp('act', h_[:, :, i * 128:(i + 1) * 128], C.psb[:].rearrange("p (k t) -> p k t", k=8), r=['psb'], w=[hk])
        for fi, (wc, dst, drow, scl) in enumerate(fchunks):
            bk = nbank % 4
            nbank += 1
            for k in range(8):
                P.mm(C.bank[bk][:, 0:ncol], W[:, k, wc:wc + 128], h_[:, k, 0:ncol], start=(k == 0), stop=(k == 7),
                     r=['W', hk], w=['bank%d' % bk])
            si = fi % 3
            if fi % 2 == 0:
                P.act(stF[si][:, 0:ncol], C.bank[bk][:, 0:ncol], ACT.Copy, scale=scl, r=['bank%d' % bk], w=['stF%d' % si])
            else:
                P.ts('dve', stF[si][:, 0:ncol], C.bank[bk][:, 0:ncol], scl, None, ALU.mult, r=['bank%d' % bk], w=['stF%d' % si])
            P.dma('pool', dst[drow:drow + 128, t0 * 128:t0 * 128 + ncol], stF[si][:, 0:ncol], r=['stF%d' % si], w=[])
        bk = nbank % 4
        nbank += 1
        for k in range(8):
            P.mm(C.bank[bk][0:16, 0:ncol], W[:, k, 3328:3344], h_[:, k, 0:ncol], start=(k == 0), stop=(k == 7),
                 r=['W', hk], w=['bank%d' % bk])
        P.cp('dve', stG[:, 0:ncol], C.bank[bk][0:16, 0:ncol], r=['bank%d' % bk], w=['stG'])
        P.dma('act', C.gmT[:, t0 * 128:t0 * 128 + ncol], stG[:, 0:ncol], r=['stG'], w=[])
        for i in range(ntl):
            tile = t0 + i
            so = stT[tile % 2]
            sk = 'stT%d' % (tile % 2)
            for ti, (wc, pc, wd) in enumerate(tcols):
                bk = nbank % 4
                nbank += 1
                for k in range(8):
                    P.mm(C.bank[bk][:, 0:wd], h_[:, k, i * 128:(i + 1) * 128], W[:, k, wc:wc + wd], start=(k == 0), stop=(k == 7),
                         r=['W', hk], w=['bank%d' % bk])
                if ti % 2 == 0:
                    P.cp('act', so[:, pc:pc + wd], C.bank[bk][:, 0:wd], r=['bank%d' % bk], w=[sk])
                else:
                    P.cp('dve', so[:, pc:pc + wd], C.bank[bk][:, 0:wd], r=['bank%d' % bk], w=[sk])
            P.dma('pool', C.pT[tile * 128:(tile + 1) * 128, :], so[:], r=[sk], w=[])
    P.end()


def phase_na(C, l):
    P = C.P
    P.begin()
    upd = (l < DEPTH - 1)
    nq = NT if upd else 64
    idb = P.sb("idb", [128, 128], BF16)
    P.dma('sp', idb[:], C.ident_b, w=['idb'])
    maskt = P.sb("maskt", [128, NTYPES * 128], F32)
    P.dma('act', maskt[:], C.na_mask, w=['maskt'])
    ona = P.sb("ona", [128, NT, 512], BF16)
    qT = P.sb("qT", [64, T], BF16)
    kT = P.sb("kT", [64, T], BF16)
    V1 = P.sb("V1", [128, NT, 65], BF16)
    P.memset('pool', V1[:, :, 64:65], 1.0, w=['V1'])
    biasf = P.sb("biasf", [128, NTYPES * 128], F32)
    biasb = P.sb("biasb", [128, NTYPES * 128], BF16)
    E = [P.sb("E%d" % i, [128, 7 * 128], BF16) for i in range(2)]
    rr = P.sb("rr", [128, 4], F32)
    pv = C.pT.rearrange("(t p) c -> p t c", p=128)
    def tiles_of(j):
        if j < 64:
            return na_tile_list(j) + [(64, None), (65, None)]
        return [(64, None), (65, None)]

    def head_loads(h):
        P.dma('sp', qT[:], C.qkT[h * 64:(h + 1) * 64, :], w=['qT'])
        P.dma('act', kT[:], C.qkT[512 + h * 64:512 + (h + 1) * 64, :], w=['kT'])
        P.dma('sp', V1[:, 0:33, 0:64], pv[:, 0:33, h * 64:(h + 1) * 64], w=['V1'])
        P.dma('act', V1[:, 33:NT, 0:64], pv[:, 33:NT, h * 64:(h + 1) * 64], w=['V1'])
        P.dma('sp', biasf[:], C.rpb_g[l, h], w=['biasf'])
        P.tt('dve', biasf[:], biasf[:], maskt[:], ALU.add, r=['biasf', 'maskt'], w=['biasf'])
        P.act(biasb[:], biasf[:], ACT.Exp, r=['biasf'], w=['biasb'])

    def stS(n):
        h, j = n // nq, n % nq
        if j == 0:
            head_loads(h)
        tiles = tiles_of(j)
        par = n % 2
        bA, bB = C.bank[par * 2], C.bank[par * 2 + 1]
        kA, kB = 'bank%d' % (par * 2), 'bank%d' % (par * 2 + 1)
        e_ = E[par]
        ek = 'E%d' % par
        for i, (jp, ty) in enumerate(tiles):
            bk, kk = (bA, kA) if i < 4 else (bB, kB)
            col = (i % 4) * 128
            P.mm(bk[:, col:col + 128], kT[:, jp * 128:(jp + 1) * 128], qT[:, j * 128:(j + 1) * 128],
                 start=True, stop=True, r=['kT', 'qT'], w=[kk])
        nt_ = len(tiles)
        na_ = min(nt_, 4)
        P.act(e_[:, 0:na_ * 128], bA[:, 0:na_ * 128], ACT.Exp, r=[kA], w=[ek])
        if nt_ > 4:
            P.act(e_[:, 512:nt_ * 128], bB[:, 0:(nt_ - 4) * 128], ACT.Exp, r=[kB], w=[ek])
        nl = nt_ - 2
        if nl > 0:
            ty0 = tiles[0][1]
            P.tt('dve', e_[:, 0:nl * 128], e_[:, 0:nl * 128], biasb[:, ty0 * 128:(ty0 + nl) * 128], ALU.mult, r=[ek, 'biasb'], w=[ek])

    def stPV(n):
        h, j = n // nq, n % nq
        tiles = tiles_of(j)
        par = n % 2
        bO, kO = C.bank[4 + par], 'bank%d' % (4 + par)
        e_ = E[par]
        ek = 'E%d' % par
        nt_ = len(tiles)
        for i, (jp, ty) in enumerate(tiles):
            P.mm(bO[:, 0:65], e_[:, i * 128:(i + 1) * 128], V1[:, jp, :], start=(i == 0), stop=(i == nt_ - 1),
                 r=[ek, 'V1'], w=[kO])
        P.recip(rr[:, par:par + 1], bO[:, 64:65], r=[kO], w=['rr%d' % par])
        P.ts('dve', ona[:, j, h * 64:(h + 1) * 64], bO[:, 0:64], rr[:, par:par + 1], None, ALU.mult,
             r=[kO, 'rr%d' % par], w=['ona'])

    for h_ in range(8):
        _run_pipeline([(lambda j, h_=h_: stS(h_ * nq + j), 0), (lambda j, h_=h_: stPV(h_ * nq + j), 1)], nq)
    za = [P.sb("za%d" % i, [128, 512], BF16) for i in range(2)]
    sil = [P.sb("sil%d" % i, [128, 512], F32) for i in range(2)]
    gg = [P.sb("gg%d" % i, [128, 512], BF16) for i in range(2)]
    gst = [P.sb("gst%d" % i, [128, 512], BF16) for i in range(2)]
    for j in range(nq):
        p2 = j % 2
        P.dma('sp', za[p2][:], C.pT[j * 128:(j + 1) * 128, 512:1024], w=['za%d' % p2])
        P.act(sil[p2][:], za[p2][:], ACT.Silu, r=['za%d' % p2], w=['sil%d' % p2])
        P.tt('dve', gg[p2][:], ona[:, j, :], sil[p2][:], ALU.mult, r=['ona', 'sil%d' % p2], w=['gg%d' % p2])
        for c4 in range(4):
            P.tr(C.psb[:, c4 * 128:(c4 + 1) * 128], gg[p2][:, c4 * 128:(c4 + 1) * 128], idb[:], r=['gg%d' % p2, 'idb'], w=['psb'])
        P.cp('act', gst[p2][:], C.psb[:, 0:512], r=['psb'], w=['gst%d' % p2])
        P.dma('act', C.gT[0:512, j * 128:(j + 1) * 128].rearrange("(c p) t -> p c t", p=128),
              gst[p2][:].rearrange("p (c t) -> p c t", c=4), r=['gst%d' % p2], w=[])
    P.end()


def phase_mlstm(C, l):
    P = C.P
    upd = (l < DEPTH - 1)
    P.begin()
    cw = P.sb("cw", [128, 12], F32)
    P.dma('sp', cw[:], C.conv_ml[l], w=['cw'])
    perm = P.sb("perm", [128, 128], BF16)
    P.dma('act', perm[:], C.perm_b, w=['perm'])
    cosb = [P.sb("cosb%d" % i, [128, 512], F32) for i in range(2)]
    sinb = [P.sb("sinb%d" % i, [128, 512], F32) for i in range(2)]
    raw = [P.sb("raw%d" % i, [128, 514], BF16) for i in range(3)]
    a_ = [P.sb("a%d" % i, [128, 512], F32) for i in range(2)]
    s_ = [P.sb("s%d" % i, [128, 512], F32) for i in range(2)]
    sb16 = [P.sb("sb16%d" % i, [128, 512], BF16) for i in range(2)]
    o1 = [P.sb("o1%d" % i, [128, 512], F32) for i in range(2)]
    o2 = [P.sb("o2%d" % i, [128, 512], F32) for i in range(2)]
    ob = [P.sb("ob%d" % i, [128, 512], BF16) for i in range(2)]
    blocks = [(b * 512, 512, 0, L) for b in range(16)] + [(L, 256, L, T)]
    it = 0
    for bi, (t0, n, s0, s1) in enumerate(blocks):
        lat = t0 < L
        cb, sn = cosb[bi % 2], sinb[bi % 2]
        if lat:
            P.dma('sp', cb[:], C.rope_cos[:, t0:t0 + 512], w=['cos%d' % (bi % 2)])
            P.dma('act', sn[:], C.rope_sin[:, t0:t0 + 512], w=['sin%d' % (bi % 2)])
        for c in range(4):
            ri = it % 3
            p2 = it % 2
            it += 1
            rw = raw[ri]
            rk = 'raw%d' % ri
            lo = max(t0 - 1, s0)
            hi = min(t0 + n + 1, s1)
            if lo > t0 - 1:
                P.memset('pool', rw[:, 0:1], 0.0, w=[rk])
            if hi < t0 + n + 1:
                P.memset('pool', rw[:, n + 1:n + 2], 0.0, w=[rk])
            P.dma('sp' if it % 2 == 0 else 'act', rw[:, lo - (t0 - 1):hi - (t0 - 1)], C.qkmT[c * 128:(c + 1) * 128, lo:hi], r=[rk], w=[rk])
            a, sv = a_[p2], s_[p2]
            P.ts('dve', a[:, 0:n], rw[:, 0:n], cw[:, c * 3:c * 3 + 1], None, ALU.mult, r=[rk, 'cw'], w=['a%d' % p2])
            P.stt('dve', a[:, 0:n], rw[:, 1:n + 1], cw[:, c * 3 + 1:c * 3 + 2], a[:, 0:n], ALU.mult, ALU.add, r=[rk, 'cw', 'a%d' % p2], w=['a%d' % p2])
            P.stt('dve', a[:, 0:n], rw[:, 2:n + 2], cw[:, c * 3 + 2:c * 3 + 3], a[:, 0:n], ALU.mult, ALU.add, r=[rk, 'cw', 'a%d' % p2], w=['a%d' % p2])
            P.act(sv[:, 0:n], a[:, 0:n], ACT.Silu, r=['a%d' % p2], w=['s%d' % p2])
            if c >= 2:
                P.act(sv[:, 0:n], sv[:, 0:n], ACT.Copy, scale=0.125, r=['s%d' % p2], w=['s%d' % p2])
            if lat:
                P.cp('act', sb16[p2][:, 0:n], sv[:, 0:n], r=['s%d' % p2], w=['sb16%d' % p2])
                bk = C.bank[p2]
                P.mm(bk[:, 0:n], perm[:], sb16[p2][:, 0:n], r=['perm', 'sb16%d' % p2], w=['bank%d' % p2])
                P.tt('pool', o1[p2][:, 0:n], sv[:, 0:n], cb[:, 0:n], ALU.mult, r=['s%d' % p2, 'cos%d' % (bi % 2)], w=['o1%d' % p2])
                P.tt('dve', o2[p2][:, 0:n], bk[:, 0:n], sn[:, 0:n], ALU.mult, r=['bank%d' % p2, 'sin%d' % (bi % 2)], w=['o2%d' % p2])
                P.tt('pool', ob[p2][:, 0:n], o1[p2][:, 0:n], o2[p2][:, 0:n], ALU.add, r=['o1%d' % p2, 'o2%d' % p2], w=['ob%d' % p2])
            else:
                P.cp('act', ob[p2][:, 0:n], sv[:, 0:n], r=['s%d' % p2], w=['ob%d' % p2])
            P.dma('sp', C.qkm2[c * 128:(c + 1) * 128, t0:t0 + n], ob[p2][:, 0:n], r=['ob%d' % p2], w=[])
    P.end()

    P.begin()
    cum = P.sb("cum", [8, T], F32)
    ict = P.sb("ict", [128, NT, 16], F32)
    wt = P.sb("wt", [128, NT, 16], F32)
    idf = P.sb("idf", [128, 128], F32)
    P.dma('sp', idf[:], C.ident_f, w=['idf'])
    idb = P.sb("idb", [128, 128], BF16)
    P.dma('sp', idb[:], C.ident_b, w=['idb'])
    bif = P.sb("bif", [16, 1], F32)
    P.dma('act', bif[:], C.b_if[l], w=['bif'])
    selc = P.sb("selc", [16, 4], F32)
    P.dma('act', selc[:], C.selcols, w=['selc'])
    selC = P.sb("selC", [8, 8 * 128], F32)
    P.dma('sp', selC[:], C.selC, w=['selC'])
    mlm = P.sb("mlm", [128, 2 * 512], BF16)
    P.dma('sp', mlm[:], C.ml_mask, w=['mlm'])
    NB = 1024
    gmb = P.sb("gmb", [16, NB], F32)
    e1 = P.sb("e1", [16, NB], F32)
    xa = P.sb("xa", [8, NB], F32)
    xb = P.sb("xb", [8, NB], F32)
    x0 = P.sb("x0", [8, NB], F32)
    cF = P.sb("cF", [8, NB], F32)
    A16 = P.sb("A16", [16, NB], F32)
    B16 = P.sb("B16", [16, NB], F32)
    G1 = P.sb("G1", [16, NB], F32)
    gblocks = [(b * NB, NB) for b in range(8)] + [(L, 256)]
    for (t0, n) in gblocks:
        nch = n // 128
        P.dma('sp', gmb[:, 0:n], C.gmT[:, t0:t0 + n], w=['gmb'])
        P.ts('dve', gmb[:, 0:n], gmb[:, 0:n], bif[:, 0:1], None, ALU.add, r=['gmb', 'bif'], w=['gmb'])
        P.act(e1[:, 0:n], gmb[:, 0:n], ACT.Exp, scale=-1.0, r=['gmb'], w=['e1'])
        P.act(e1[:, 0:n], e1[:, 0:n], ACT.Ln, bias=1.0, r=['e1'], w=['e1'])
        P.ts('dve', x0[:, 0:n], e1[0:8, 0:n], -1.0, None, ALU.mult, r=['e1'], w=['x0'])
        src, sk = x0, 'x0'
        pp = [(xa, 'xa'), (xb, 'xb')]
        step = 1
        i = 0
        while step < 128:
            dst, dk = pp[i % 2]
            sv = src[:, 0:n].rearrange("p (c t) -> p c t", t=128)
            dv = dst[:, 0:n].rearrange("p (c t) -> p c t", t=128)
            P.tt('dve', dv[:, :, step:], sv[:, :, step:], sv[:, :, :128 - step], ALU.add, r=[sk], w=[dk])
            P.cp('pool', dv[:, :, :step], sv[:, :, :step], r=[sk], w=[dk])
            src, sk = dst, dk
            step *= 2
            i += 1
        pv_ = src[:, 0:n].rearrange("p (c t) -> p c t", t=128)
        cfv = cF[:, 0:n].rearrange("p (c t) -> p c t", t=128)
        P.tt('dve', cfv, pv_[:, :, 127:128].broadcast_to([8, nch, 128]), pv_, ALU.subtract, r=[sk], w=['cF'])
        P.tt('dve', cF[:, 0:n], cF[:, 0:n], x0[:, 0:n], ALU.add, r=['cF', 'x0'], w=['cF'])
        P.ts('dve', cF[:, 0:n], cF[:, 0:n], selc[0:8, 3:4], None, ALU.mult, r=['cF', 'selc'], w=['cF'])
        P.stt('dve', cum[:, t0:t0 + n], src[:, 0:n], selc[0:8, 2:3], cF[:, 0:n], ALU.mult, ALU.add, r=[sk, 'selc', 'cF'], w=['cum'])
        P.memset('pool', B16[:, 0:n], 0.0, w=['B16'])
        P.dma('sp', B16[8:16, 0:n], cum[:, t0:t0 + n], r=['cum', 'B16'], w=['B16'])
        P.ts('dve', A16[:, 0:n], gmb[:, 0:n], selc[:, 0:1], selc[:, 1:2], ALU.mult, ALU.add, r=['gmb', 'selc'], w=['A16'])
        P.tt('dve', G1[:, 0:n], A16[:, 0:n], B16[:, 0:n], ALU.subtract, r=['A16', 'B16'], w=['G1'])
        for ci in range(nch):
            ch = t0 // 128 + ci
            bk = C.bank[ci % 2]
            P.tr(bk[:, 0:16], G1[:, ci * 128:(ci + 1) * 128], idf[0:16, 0:16], r=['G1', 'idf'], w=['bank%d' % (ci % 2)])
            P.cp('dve', ict[:, ch, :], bk[:, 0:16], r=['bank%d' % (ci % 2)], w=['ict'])
            P.act(wt[:, ch, :], bk[:, 0:16], ACT.Exp, r=['bank%d' % (ci % 2)], w=['wt'])

    hfwd = P.sb("hfwd", [128, NT, 256], BF16)
    Z = P.sb("Z", [64, 4, 65], F32)
    Zb = P.sb("Zb", [64, 4, 65], BF16)
    qc = [P.sb("qc%d" % i, [64, 4, 128], BF16) for i in range(3)]
    kc = [P.sb("kc%d" % i, [64, 4, 128], BF16) for i in range(3)]
    vc = [P.sb("vc%d" % i, [128, 4, 65], BF16) for i in range(3)]
    for i in range(3):
        P.memset('pool', vc[i][:, :, 64:65], 1.0, w=['vc%d' % i])
    oz = [P.sb("oz%d" % i, [128, 512], BF16) for i in range(2)]
    Dt = [P.sb("Dt%d" % i, [128, 512], BF16) for i in range(2)]
    U = [P.sb("U%d" % i, [64, 512], F32) for i in range(2)]
    St = [P.sb("St%d" % i, [128, 512], BF16) for i in range(2)]
    qs = [P.sb("qs%d" % i, [64, 4, 128], BF16) for i in range(2)]
    ks = [P.sb("ks%d" % i, [128, 4, 64], BF16) for i in range(2)]
    dd = P.sb("dd", [128, 16], F32)
    hh = [P.sb("hh%d" % i, [128, 256], F32) for i in range(2)]
    sg = [P.sb("sg%d" % i, [128, 512], F32) for i in range(2)]
    go = [P.sb("go%d" % i, [128, 256], BF16) for i in range(2)]
    gst = [P.sb("gst%d" % i, [128, 256], BF16) for i in range(2)]
    qv = C.qkm2[0:256, :].rearrange("(h d) t -> d h t", d=64)
    kv = C.qkm2[256:512, :].rearrange("(h d) t -> d h t", d=64)
    it = 0
    for dpass in range(2):
        order = ([64, 65] + list(range(64))) if dpass == 0 else ([65, 64] + list(range(63, -1, -1)))
        P.memset('pool', Z[:], 0.0, w=['Z'])
        P.memset('pool', Zb[:], 0.0, w=['Zb'])
        for c in order:
            emit = (c < 64) or upd
            b3 = it % 3
            p2 = it % 2
            it += 1
            q_, k_, v_ = qc[b3], kc[b3], vc[b3]
            qk_, kk_, vk_ = 'qc%d' % b3, 'kc%d' % b3, 'vc%d' % b3
            cs = slice(c * 128, (c + 1) * 128)
            P.dma('sp', q_[:], qv[:, :, cs], w=[qk_])
            P.dma('act', k_[:], kv[:, :, cs], w=[kk_])
            P.dma('sp', v_[:, :, 0:64], C.pT[cs, 1024:1280].rearrange("p (h d) -> p h d", d=64), w=[vk_])
            bD, kD = C.bank[p2], 'bank%d' % p2
            bS, kS = C.bank[2 + p2], 'bank%d' % (2 + p2)
            bU, kU = C.bank[4], 'bank4'
            bN, kN = C.bank[5], 'bank5'
            bZ, kZ = C.bank[6], 'bank6'
            P.mm(bD[:, :], idb[:], mlm[:, dpass * 512:(dpass + 1) * 512], start=True, stop=False, r=['idb', 'mlm'], w=[kD])
            for h in range(4):
                dh = dpass * 4 + h
                P.mm(bD[:, h * 128:(h + 1) * 128], selC[:, dh * 128:(dh + 1) * 128], cum[:, cs], start=False, stop=True,
                     r=['selC', 'cum'], w=[kD])
            for h in range(4):
                dh = dpass * 4 + h
                P.mm(bU[0:64, h * 128:(h + 1) * 128], selC[:, dh * 128:dh * 128 + 64], cum[:, cs], start=True, stop=True,
                     r=['selC', 'cum'], w=[kU])
            for h in range(4):
                P.mm(bS[:, h * 128:(h + 1) * 128], k_[:, h, :], q_[:, h, :], start=True, stop=True, r=[kk_, qk_], w=[kS])
            for h in range(4):
                P.tr(C.psb[:, h * 64:(h + 1) * 64], k_[:, h, :], idb[0:64, 0:64], r=[kk_, 'idb'], w=['psb'])
            d_, u_, s2, q2, k2 = Dt[p2], U[p2], St[p2], qs[p2], ks[p2]
            for h in range(4):
                dh = dpass * 4 + h
                P.act(d_[:, h * 128:(h + 1) * 128], bD[:, h * 128:(h + 1) * 128], ACT.Exp, bias=ict[:, c, 8 + dh:9 + dh],
                      r=[kD, 'ict'], w=['Dt%d' % p2])
            P.act(u_[:], bU[0:64, :], ACT.Exp, r=[kU], w=['U%d' % p2])
            P.tt('dve', s2[:], bS[:, :], d_[:], ALU.mult, r=[kS, 'Dt%d' % p2], w=['St%d' % p2])
            P.tt('pool', q2[:], q_[:], u_[:].rearrange("p (h t) -> p h t", h=4), ALU.mult, r=[qk_, 'U%d' % p2], w=['qs%d' % p2])
            P.tt('dve', k2[:], C.psb[:, 0:256].rearrange("p (h d) -> p h d", d=64),
                 wt[:, c, 8 + dpass * 4:12 + dpass * 4].unsqueeze(2).broadcast_to([128, 4, 64]), ALU.mult,
                 r=['psb', 'wt'], w=['ks%d' % p2])
            if emit:
                for h in range(4):
                    P.mm(bN[:, h * 65:(h + 1) * 65], s2[:, h * 128:(h + 1) * 128], v_[:, h, :], start=True, stop=False,
                         r=['St%d' % p2, vk_], w=[kN])
                    P.mm(bN[:, h * 65:(h + 1) * 65], q2[:, h, :], Zb[:, h, :], start=False, stop=True,
                         r=['qs%d' % p2, 'Zb'], w=[kN])
                nv = bN[:, 0:260].rearrange("p (h e) -> p h e", e=65)
                P.cp('dve', dd[:, 0:4], nv[:, :, 64], r=[kN], w=['dd'])
                P.stt('dve', dd[:, 4:8], dd[:, 0:4], -1.0, dd[:, 0:4], ALU.mult, ALU.max, r=['dd'], w=['dd'])
                P.ts('dve', dd[:, 4:8], dd[:, 4:8], 1.0, None, ALU.max, r=['dd'], w=['dd'])
                P.op('dve', lambda e: e.reciprocal(dd[:, 8:12], dd[:, 4:8]), r=['dd'], w=['dd'])
                hv = hh[p2][:].rearrange("p (h d) -> p h d", d=64)
                P.tt('dve', hv, nv[:, :, 0:64], dd[:, 8:12].unsqueeze(2).broadcast_to([128, 4, 64]), ALU.mult, r=[kN, 'dd'], w=['hh%d' % p2])
                if dpass == 0:
                    P.cp('pool', hfwd[:, c, :], hh[p2][:], r=['hh%d' % p2], w=['hfwd'])
                else:
                    P.dma('act', oz[p2][:], C.pT[cs, 1280:1792], w=['oz%d' % p2])
                    P.act(sg[p2][:, 0:256], oz[p2][:, 0:256], ACT.Sigmoid, r=['oz%d' % p2], w=['sg%d' % p2])
                    P.act(sg[p2][:, 256:512], oz[p2][:, 256:512], ACT.Silu, r=['oz%d' % p2], w=['sg%d' % p2])
                    P.tt('pool', hh[p2][:], hh[p2][:], hfwd[:, c, :], ALU.add, r=['hh%d' % p2, 'hfwd'], w=['hh%d' % p2])
                    P.tt('pool', hh[p2][:], hh[p2][:], sg[p2][:, 0:256], ALU.mult, r=['hh%d' % p2, 'sg%d' % p2], w=['hh%d' % p2])
                    P.tt('pool', go[p2][:], hh[p2][:], sg[p2][:, 256:512], ALU.mult, r=['hh%d' % p2, 'sg%d' % p2], w=['go%d' % p2])
                    for c2 in range(2):
                        P.tr(C.psb[:, 512 + c2 * 128:512 + (c2 + 1) * 128], go[p2][:, c2 * 128:(c2 + 1) * 128], idb[:], r=['go%d' % p2, 'idb'], w=['psb2'])
                    P.cp('act', gst[p2][:], C.psb[:, 512:768], r=['psb2'], w=['gst%d' % p2])
                    P.dma('sp', C.gT[512:768, cs].rearrange("(c p) t -> p c t", p=128), gst[p2][:].rearrange("p (c t) -> p c t", c=2),
                          r=['gst%d' % p2], w=[])
            for h in range(4):
                P.mm(bZ[0:64, h * 65:(h + 1) * 65], k2[:, h, :], v_[:, h, :], start=True, stop=True, r=['ks%d' % p2, vk_], w=[kZ])
            P.tt('dve', Z[:], Z[:], bZ[0:64, 0:260].rearrange("p (h e) -> p h e", e=65), ALU.add, r=['Z', kZ], w=['Z'])
            ecol = 127 if dpass == 0 else 0
            ev = u_[:].rearrange("p (h t) -> p h t", h=4)[:, :, ecol:ecol + 1].broadcast_to([64, 4, 65])
            P.tt('dve', Z[:], Z[:], ev, ALU.mult, r=['Z', 'U%d' % p2], w=['Z'])
            P.cp('act', Zb[:], Z[:], r=['Z'], w=['Zb'])
    P.end()


def _sin_mlp_block(P, C, ps, bcol, fcol, out, n, tag, tmp, pskey):
    pre, t2 = tmp
    P.ts('dve', pre[:, 0:n], ps, bcol, fcol, ALU.add, ALU.mult, r=[pskey, 'hfc'], w=[tag + 'pre'])
    P.ts('pool', t2[:, 0:n], pre[:, 0:n], -1.0, PI, ALU.mult, ALU.add, r=[tag + 'pre'], w=[tag + 't2'])
    P.tt('dve', t2[:, 0:n], pre[:, 0:n], t2[:, 0:n], ALU.min, r=[tag + 'pre', tag + 't2'], w=[tag + 't2'])
    P.ts('dve', pre[:, 0:n], pre[:, 0:n], -1.0, -PI, ALU.mult, ALU.add, r=[tag + 'pre'], w=[tag + 'pre'])
    P.tt('dve', t2[:, 0:n], t2[:, 0:n], pre[:, 0:n], ALU.max, r=[tag + 'pre', tag + 't2'], w=[tag + 't2'])
    P.act(out[:, 0:n], t2[:, 0:n], ACT.Sin, r=[tag + 't2'], w=[tag + 'out'])


def _hy_filters(C, l, Ln, featT, decayT, dstF):
    P = C.P
    w1 = P.sb("w1", [33, 64], F32); w2 = P.sb("w2", [64, 64], F32); w3 = P.sb("w3", [64, 1024], F32)
    hfc = P.sb("hfc", [64, 4], F32)
    P.dma('sp', w1[:], C.hf_w1[l], w=['hfw'])
    P.dma('sp', w2[:], C.hf_w2[l], w=['hfw'])
    P.dma('act', w3[:], C.hf_w3[l], w=['hfw'])
    P.dma('sp', hfc[:, 0:1], C.hf_b1[l], w=['hfc'])
    P.dma('sp', hfc[:, 1:2], C.hf_b2[l], w=['hfc'])
    P.dma('sp', hfc[:, 2:4], C.hf_freq[l], w=['hfc'])
    nb = max(Ln // 512, 1)
    bw = min(Ln, 512)
    ssq = P.sb("ssq", [128, 8, 16], F32)
    P.memset('pool', ssq[:], 0.0, w=['ssq'])
    ft = [P.sb("ft%d" % i, [33, 512], F32) for i in range(3)]
    dec = [P.sb("dec%d" % i, [128, 2, 512], F32) for i in range(5)]
    pre1 = [P.sb("pre1%d" % i, [64, 512], F32) for i in range(3)]
    t21 = [P.sb("t21%d" % i, [64, 512], F32) for i in range(3)]
    a1 = [P.sb("a1%d" % i, [64, 512], F32) for i in range(3)]
    pre2 = [P.sb("pre2%d" % i, [64, 512], F32) for i in range(3)]
    t22 = [P.sb("t22%d" % i, [64, 512], F32) for i in range(3)]
    a2 = [P.sb("a2%d" % i, [64, 512], F32) for i in range(3)]
    fl = [P.sb("fl%d" % i, [128, 512], F32) for i in range(4)]
    junk = P.sb("junkf", [128, 512], BF16)

    def st1(b):
        p3, p2, p5 = b % 3, b % 2, b % 5
        cs = slice(b * bw, (b + 1) * bw)
        P.dma('sp', ft[p3][:, 0:bw], featT[:, cs], w=['ft%d' % p3])
        P.dma('act', dec[p5][:, :, 0:bw], decayT[:, cs].rearrange("(h p) t -> p h t", p=128), w=['dec%d' % p5])
        P.mm(C.bank[0][0:64, 0:bw], w1[:], ft[p3][:, 0:bw], r=['hfw', 'ft%d' % p3], w=['bank0'])
        _sin_mlp_block(P, C, C.bank[0][0:64, 0:bw], hfc[:, 0:1], hfc[:, 2:3], a1[p3], bw, 'm1%d' % p3, (pre1[p3], t21[p3]), 'bank0')

    def st2(b):
        p2, p3 = b % 2, b % 3
        P.mm(C.bank[1][0:64, 0:bw], w2[:], a1[p3][:, 0:bw], r=['hfw', 'm1%dout' % p3], w=['bank1'])
        _sin_mlp_block(P, C, C.bank[1][0:64, 0:bw], hfc[:, 1:2], hfc[:, 3:4], a2[p3], bw, 'm2%d' % p3, (pre2[p3], t22[p3]), 'bank1')

    def st3(b):
        p3, p2, p5 = b % 3, b % 2, b % 5
        cs = slice(b * bw, (b + 1) * bw)
        for cc in range(8):
            bk = C.bank[2 + cc % 4]
            bkk = 'bank%d' % (2 + cc % 4)
            f3 = (b * 8 + cc) % 4
            P.mm(bk[:, 0:bw], w3[:, cc * 128:(cc + 1) * 128], a2[p3][:, 0:bw], r=['hfw', 'm2%dout' % p3], w=[bkk])
            P.tt('dve', fl[f3][:, 0:bw], bk[:, 0:bw], dec[p5][:, cc % 2, 0:bw], ALU.mult, r=[bkk, 'dec%d' % p5], w=['fl%d' % f3])
            if b == 0 and (cc // 2) % 2 == 1:
                P.memset('pool', fl[f3][:, 0:1], 0.0, w=['fl%d' % f3])
            P.act(junk[:, 0:bw], fl[f3][:, 0:bw], ACT.Square, accum_out=ssq[:, cc, b:b + 1], r=['fl%d' % f3], w=['ssq'])
            P.dma('sp', dstF[cc * 128:(cc + 1) * 128, cs], fl[f3][:, 0:bw], r=['fl%d' % f3], w=[])

    _run_pipeline([(st1, 0), (st2, 2), (st3, 4)], nb)
    tot = P.sb("tot", [128, 8], F32)
    nrm = P.sb("nrm", [128, 8], F32)
    P.op('dve', lambda e: e.tensor_reduce(tot[:], ssq[:], AX.X, ALU.add), r=['ssq'], w=['tot'])
    tv = tot[:].rearrange("p (o d h) -> p o d h", o=2, d=2)
    nv = nrm[:, 0:4].rearrange("p (o h) -> p o h", o=2)
    P.tt('dve', nv, tv[:, :, 0, :], tv[:, :, 1, :], ALU.add, r=['tot'], w=['nrm'])
    P.ts('dve', nrm[:, 0:4], nrm[:, 0:4], 1e-6, None, ALU.add, r=['nrm'], w=['nrm'])
    P.act(nrm[:, 0:4], nrm[:, 0:4], ACT.Sqrt, r=['nrm'], w=['nrm'])
    P.op('dve', lambda e: e.reciprocal(nrm[:, 4:8], nrm[:, 0:4]), r=['nrm'], w=['nrm'])
    return nrm


def _fft_S1(P, C, xg, xk, dft, banks):
    for c in range(4):
        bi = banks[c // 2]
        P.mm(C.bank[bi][:, (c % 2) * 256:(c % 2) * 256 + 256], xg[0:64, c, :], dft[0:64, 0:256], r=[xk, 'dft'], w=['bank%d' % bi])


def _fft_S3(P, C, dft, Bt, bkey, banks):
    Bv = Bt[:].rearrange("p (c r k) -> p c r k", c=4, r=2)
    Br, Bi = Bv[:, :, 0, :], Bv[:, :, 1, :]
    r_, i_ = banks
    P.mm(C.bank[r_][:, :], dft[:, 2 * 128:3 * 128], Br, start=True, stop=False, r=['dft', bkey], w=['bank%d' % r_])
    P.mm(C.bank[r_][:, :], dft[:, 3 * 128:4 * 128], Bi, start=False, stop=True, r=['dft', bkey], w=['bank%d' % r_])
    P.mm(C.bank[i_][:, :], dft[:, 4 * 128:5 * 128], Br, start=True, stop=False, r=['dft', bkey], w=['bank%d' % i_])
    P.mm(C.bank[i_][:, :], dft[:, 5 * 128:6 * 128], Bi, start=False, stop=True, r=['dft', bkey], w=['bank%d' % i_])


def _twiddle(P, C, banks, tw, which, Bt, bkey, tmp, tkey):
    TA = tw[:, (2 * which) * 1024:(2 * which) * 1024 + 512].rearrange("p (c r k) -> p c r k", c=2, r=2)
    TB = tw[:, (2 * which + 1) * 1024:(2 * which + 1) * 1024 + 512].rearrange("p (c r k) -> p c r k", c=2, r=2)
    for hb in range(2):
        bi = banks[hb]
        bkk = 'bank%d' % bi
        t1, t2 = tmp[hb]
        k1, k2 = tkey + 'a%d' % hb, tkey + 'b%d' % hb
        A = C.bank[bi][:, :].rearrange("p (c r k) -> p c r k", c=2, r=2)
        T1 = t1[:].rearrange("p (c r k) -> p c r k", c=2, r=2)
        T2 = t2[:].rearrange("p (c r k) -> p c r k", c=2, r=2)
        P.tt('dve', T1, A, TA, ALU.mult, r=[bkk, 'tw'], w=[k1])
        P.tt('dve', T2[:, :, 0, :], A[:, :, 1, :], TB[:, :, 0, :], ALU.mult, r=[bkk, 'tw'], w=[k2])
        P.tt('dve', T2[:, :, 1, :], A[:, :, 0, :], TB[:, :, 1, :], ALU.mult, r=[bkk, 'tw'], w=[k2])
        P.tt('pool', Bt[:, hb * 512:(hb + 1) * 512], t1[:], t2[:], ALU.add, r=[k1, k2], w=[bkey])


def _fft_tables(C):
    P = C.P
    dft = P.sb("dft", [128, 12 * 128], BF16)
    tw = P.sb("tw", [128, 4 * 1024], BF16)
    P.dma('sp', dft[:], C.dft, w=['dft'])
    P.dma('act', tw[:], C.tw, w=['tw'])
    return dft, tw


def _tw_tmp(P, name):
    return [[(P.sb("%st1_%d%d" % (name, p, h), [128, 512], F32), P.sb("%st2_%d%d" % (name, p, h), [128, 512], F32)) for h in range(2)] for p in range(2)]


def _run_pipeline(stages, ng):
    maxlag = max(l for _, l in stages)
    for t in range(ng + maxlag):
        for fn, lag in stages:
            g = t - lag
            if 0 <= g < ng:
                fn(g)


def _fftconv(C, src, order, dst, ng=64, stage=9):
    P = C.P
    P.begin()
    dft, tw = _fft_tables(C)
    xin = [P.sb("xin%d" % i, [64, 16, 128], BF16) for i in range(2)]
    kin = [P.sb("kin%d" % i, [128, 1024], BF16) for i in range(2)]
    Bt = [P.sb("Bt%d" % i, [128, 1024], BF16) for i in range(2)]
    Ht = [P.sb("Ht%d" % i, [128, 1024], BF16) for i in range(2)]
    Yt = [P.sb("Yt%d" % i, [128, 1024], BF16) for i in range(2)]
    pp = [[P.sb("pp%d%d" % (p, i), [128, 512], F32) for i in range(4)] for p in range(2)]
    yst = [P.sb("yst%d" % i, [64, 16, 128], BF16) for i in range(2)]
    tmpB = _tw_tmp(P, "tB")
    tmpF = _tw_tmp(P, "tF")
    sv = src.rearrange("c (a b) -> a c b", b=128)
    dv = dst.rearrange("c (a b) -> a c b", b=128)

    def stA(g):
        ch0, x16, p2 = g * 4, (g // 4) % 2, g % 2
        if g % 4 == 0:
            P.dma('sp', xin[x16][:], sv[:, ch0:ch0 + 16, :], w=['xin%d' % x16])
        P.dma('act', kin[p2][:], C.kspec[order, :, ch0 * 256:(ch0 + 4) * 256], w=['kin%d' % p2])
        xg = xin[x16][:, (g % 4) * 4:(g % 4) * 4 + 4, :]
        _fft_S1(P, C, xg, 'xin%d' % x16, dft, (0, 1))

    def stB(g):
        p2 = g % 2
        _twiddle(P, C, (0, 1), tw, 0, Bt[p2], 'B%d' % p2, tmpB[p2], 'tB%d' % p2)

    def stC(g):
        p2 = g % 2
        _fft_S3(P, C, dft, Bt[p2], 'B%d' % p2, (2, 3))

    def stD(g):
        p2 = g % 2
        Kv = kin[p2][:].rearrange("p (c r k) -> p c r k", c=4, r=2)
        Kr, Ki = Kv[:, :, 0, :], Kv[:, :, 1, :]
        Xr = C.bank[2][:, :].rearrange("p (c k) -> p c k", c=4)
        Xi = C.bank[3][:, :].rearrange("p (c k) -> p c k", c=4)
        ppv = [t[:].rearrange("p (c k) -> p c k", c=4) for t in pp[p2]]
        pk = ['pp%d%d' % (p2, i) for i in range(4)]
        P.tt('dve', ppv[0], Xr, Kr, ALU.mult, r=['bank2', 'kin%d' % p2], w=[pk[0]])
        P.tt('dve', ppv[1], Xi, Ki, ALU.mult, r=['bank3', 'kin%d' % p2], w=[pk[1]])
        P.tt('dve', ppv[2], Xr, Ki, ALU.mult, r=['bank2', 'kin%d' % p2], w=[pk[2]])
        P.tt('dve', ppv[3], Xi, Kr, ALU.mult, r=['bank3', 'kin%d' % p2], w=[pk[3]])
        Y = Yt[p2]
        P.tt('pool', Y[:, 0:512], pp[p2][0][:], pp[p2][1][:], ALU.subtract, r=[pk[0], pk[1]], w=['Y%d' % p2])
        P.tt('pool', Y[:, 512:1024], pp[p2][2][:], pp[p2][3][:], ALU.add, r=[pk[2], pk[3]], w=['Y%d' % p2])

    def stE(g):
        p2 = g % 2
        Y = Yt[p2]
        for c in range(4):
            bi = 4 + c // 2
            o_ = C.bank[bi][:, (c % 2) * 256:(c % 2) * 256 + 256]
            P.mm(o_, Y[:, c * 128:(c + 1) * 128], dft[:, 6 * 128:8 * 128], start=True, stop=False, r=['Y%d' % p2, 'dft'], w=['bank%d' % bi])
            P.mm(o_, Y[:, 512 + c * 128:512 + (c + 1) * 128], dft[:, 8 * 128:10 * 128], start=False, stop=True, r=['Y%d' % p2, 'dft'], w=['bank%d' % bi])

    def stF(g):
        p2 = g % 2
        _twiddle(P, C, (4, 5), tw, 1, Ht[p2], 'H%d' % p2, tmpF[p2], 'tF%d' % p2)

    def stG(g):
        ch0, x16, p2 = g * 4, (g // 4) % 2, g % 2
        Hv = Ht[p2][:].rearrange("p (c r k) -> p c r k", c=4, r=2)
        P.mm(C.bank[6][0:64, :], dft[:, 10 * 128:10 * 128 + 64], Hv[:, :, 0, :], start=True, stop=False, r=['dft', 'H%d' % p2], w=['bank6'])
        P.mm(C.bank[6][0:64, :], dft[:, 11 * 128:11 * 128 + 64], Hv[:, :, 1, :], start=False, stop=True, r=['dft', 'H%d' % p2], w=['bank6'])
        P.cp('act', yst[x16][:, (g % 4) * 4:(g % 4) * 4 + 4, :], C.bank[6][0:64, :].rearrange("p (c k) -> p c k", c=4), r=['bank6'], w=['yst%d' % x16])
        if g % 4 == 3:
            P.dma('sp', dv[:, ch0 - 12:ch0 + 4, :], yst[x16][:], r=['yst%d' % x16], w=[])

    _run_pipeline([(stA, 0), (stC, 1), (stE, 2), (stG, 3), (stB, 0), (stD, 1), (stF, 2)], ng)
    P.end()


def _conv3_fm(P, rw, rk, cw, col, out, ok, n, eng='dve'):
    P.ts(eng, out[:, 0:n], rw[:, 0:n], cw[:, col:col + 1], None, ALU.mult, r=[rk, 'cwh'], w=[ok])
    P.stt(eng, out[:, 0:n], rw[:, 1:n + 1], cw[:, col + 1:col + 2], out[:, 0:n], ALU.mult, ALU.add, r=[rk, 'cwh', ok], w=[ok])
    P.stt(eng, out[:, 0:n], rw[:, 2:n + 2], cw[:, col + 2:col + 3], out[:, 0:n], ALU.mult, ALU.add, r=[rk, 'cwh', ok], w=[ok])


_DBG_STOP = None


def phase_hyena(C, l):
    P = C.P
    upd = (l < DEPTH - 1)
    P.begin()
    nrm = _hy_filters(C, l, L, C.featT, C.decayT, C.filtT)
    P.barrier_dram = True
    fin = [P.sb("fin%d" % i, [128, 2048], F32) for i in range(2)]
    fob = [P.sb("fob%d" % i, [128, 2048], BF16) for i in range(2)]
    P.barrier()
    it = 0
    for cc in range(8):
        o, half = cc // 4, cc % 2
        for b in range(4):
            p2 = it % 2
            it += 1
            cs = slice(b * 2048, (b + 1) * 2048)
            P.dma('sp', fin[p2][:], C.filtT[cc * 128:(cc + 1) * 128, cs], w=['fin%d' % p2])
            P.ts('dve' if it % 2 else 'pool', fob[p2][:], fin[p2][:], nrm[:, 4 + o * 2 + half:5 + o * 2 + half], None, ALU.mult,
                 r=['fin%d' % p2, 'nrm'], w=['fob%d' % p2])
            P.dma('act', C.filtN[cc * 128:(cc + 1) * 128, cs], fob[p2][:], r=['fob%d' % p2], w=[])
    P.end()
    if _DBG_STOP == 'E0':
        return
    P.begin()
    dft, tw = _fft_tables(C)
    xin = [P.sb("xin%d" % i, [64, 16, 128], BF16) for i in range(2)]
    Bt = [P.sb("Bt%d" % i, [128, 1024], BF16) for i in range(3)]
    tb = [[P.sb("tb%d%d" % (p, i), [128, 256], F32) for i in range(2)] for p in range(2)]
    kout = [P.sb("kout%d" % i, [128, 8, 256], BF16) for i in range(2)]
    tmpB = _tw_tmp(P, "tB")
    fv = C.filtN.rearrange("c (a b) -> a c b", b=128)
    for o in range(2):
        def stA(g, o=o):
            ch0, x16 = g * 2, (g // 4) % 2
            if g % 4 == 0:
                for q in range(4):
                    P.dma('sp', xin[x16][:, q * 4:q * 4 + 2, :], fv[:, o * 512 + ch0 + 2 * q:o * 512 + ch0 + 2 * q + 2, :], w=['xin%d' % x16])
                    P.dma('act', xin[x16][:, q * 4 + 2:q * 4 + 4, :], fv[:, o * 512 + 256 + ch0 + 2 * q:o * 512 + 256 + ch0 + 2 * q + 2, :], w=['xin%d' % x16])
            xg = xin[x16][:, (g % 4) * 4:(g % 4) * 4 + 4, :]
            sb = (0, 1) if g % 2 == 0 else (4, 5)
            _fft_S1(P, C, xg, 'xin%d' % x16, dft, sb)

        def stB(g):
            p2 = g % 2
            sb = (0, 1) if g % 2 == 0 else (4, 5)
            _twiddle(P, C, sb, tw, 0, Bt[g % 3], 'B%d' % (g % 3), tmpB[p2], 'tB%d' % p2)

        def stC(g):
            _fft_S3(P, C, dft, Bt[g % 3], 'B%d' % (g % 3), (2, 3))

        def stD(g, o=o):
            ch0, x16, p2 = g * 2, (g // 4) % 2, g % 2
            P.cp('act', tb[p2][0][:], C.bank[2][:, 256:512], r=['bank2'], w=['tb%d0' % p2])
            P.cp('act', tb[p2][1][:], C.bank[3][:, 256:512], r=['bank3'], w=['tb%d1' % p2])
            ko = kout[x16][:, (g % 4) * 2:(g % 4) * 2 + 2, :].rearrange("p c (r k) -> p c r k", r=2)
            P.tt('dve', ko[:, :, 0, :], C.bank[2][:, 0:256].rearrange("p (c k) -> p c k", c=2),
                 tb[p2][0][:].rearrange("p (c k) -> p c k", c=2), ALU.add, r=['bank2', 'tb%d0' % p2], w=['kout%d' % x16])
            P.tt('dve', ko[:, :, 1, :], C.bank[3][:, 0:256].rearrange("p (c k) -> p c k", c=2),
                 tb[p2][1][:].rearrange("p (c k) -> p c k", c=2), ALU.subtract, r=['bank3', 'tb%d1' % p2], w=['kout%d' % x16])
            if g % 4 == 3:
                P.dma('sp', C.kspec[o, :, (ch0 - 6) * 256:(ch0 + 2) * 256], kout[x16][:].rearrange("p c k -> p (c k)"), r=['kout%d' % x16], w=[])

        _run_pipeline([(stA, 0), (stC, 2), (stB, 0), (stD, 2)], 128)
    P.end()
    if _DBG_STOP == 'E0b':
        return
    P.begin()
    cwh = P.sb("cwh", [128, 18], F32)
    P.dma('sp', cwh[:], C.conv_hy[l], w=['cwh'])
    raw = [P.sb("raw%d" % i, [128, 2050], BF16) for i in range(3)]
    cf = [P.sb("cf%d" % i, [128, 2048], F32) for i in range(2)]
    cb = [P.sb("cb%d" % i, [128, 2048], BF16) for i in range(2)]
    it = 0
    for b in range(4):
        t0 = b * 2048
        for c in range(6):
            ri, p2 = it % 3, it % 2
            it += 1
            rw, rk = raw[ri], 'raw%d' % ri
            lo, hi = max(t0 - 1, 0), min(t0 + 2049, L)
            if lo > t0 - 1:
                P.memset('pool', rw[:, 0:1], 0.0, w=[rk])
            if hi < t0 + 2049:
                P.memset('pool', rw[:, 2049:2050], 0.0, w=[rk])
            P.dma('sp' if it % 2 else 'act', rw[:, lo - (t0 - 1):hi - (t0 - 1)], C.hyT[c * 128:(c + 1) * 128, lo:hi], r=[rk], w=[rk])
            _conv3_fm(P, rw, rk, cwh, c * 3, cf[p2], 'cf%d' % p2, 2048)
            P.cp('act', cb[p2][:], cf[p2][:], r=['cf%d' % p2], w=['cb%d' % p2])
            dst = C.hyV[c * 128:(c + 1) * 128, t0:t0 + 2048] if c < 2 else C.hyX[(c - 2) * 128:(c - 1) * 128, t0:t0 + 2048]
            P.dma('sp', dst, cb[p2][:], r=['cb%d' % p2], w=[])
    P.end()
    if _DBG_STOP == 'E1':
        return
    _fftconv(C, C.hyV, 0, C.hyY)
    if _DBG_STOP == 'E2':
        return
    P.begin()
    hbias = P.sb("hbias", [128, 4], F32)
    P.dma('sp', hbias[:], C.hy_bias[l], w=['hbias'])
    ty = [P.sb("ty%d" % i, [128, 2048], BF16) for i in range(2)]
    tv = [P.sb("tv%d" % i, [128, 2048], BF16) for i in range(2)]
    tx = [P.sb("tx%d" % i, [128, 2048], BF16) for i in range(2)]
    tf = [P.sb("tf%d" % i, [128, 2048], F32) for i in range(2)]
    tz = [P.sb("tz%d" % i, [128, 2048], BF16) for i in range(2)]
    it = 0
    for half in range(2):
        rs = slice(half * 128, (half + 1) * 128)
        for b in range(4):
            p2 = it % 2
            it += 1
            cs = slice(b * 2048, (b + 1) * 2048)
            P.dma('sp', ty[p2][:], C.hyY[rs, cs], w=['ty%d' % p2])
            P.dma('act', tv[p2][:], C.hyV[rs, cs], w=['tv%d' % p2])
            P.dma('sp', tx[p2][:], C.hyX[rs, cs], w=['tx%d' % p2])
            P.stt('dve', tf[p2][:], tv[p2][:], hbias[:, half:half + 1], ty[p2][:], ALU.mult, ALU.add, r=['tv%d' % p2, 'ty%d' % p2, 'hbias'], w=['tf%d' % p2])
            P.tt('pool', tz[p2][:], tf[p2][:], tx[p2][:], ALU.mult, r=['tf%d' % p2, 'tx%d' % p2], w=['tz%d' % p2])
            P.dma('act', C.hyZ[rs, cs], tz[p2][:], r=['tz%d' % p2], w=[])
    P.end()
    _fftconv(C, C.hyZ, 1, C.hyY)
    P.begin()
    hbias = P.sb("hbias", [128, 4], F32)
    P.dma('sp', hbias[:], C.hy_bias[l], w=['hbias'])
    ty = [P.sb("ty%d" % i, [128, 2048], BF16) for i in range(2)]
    tv = [P.sb("tv%d" % i, [128, 2048], BF16) for i in range(2)]
    tx = [P.sb("tx%d" % i, [128, 2048], BF16) for i in range(2)]
    tg = [P.sb("tg%d" % i, [128, 2048], BF16) for i in range(2)]
    tf = [P.sb("tf%d" % i, [128, 2048], F32) for i in range(2)]
    ts_ = [P.sb("tsl%d" % i, [128, 2048], F32) for i in range(2)]
    tz = [P.sb("tz%d" % i, [128, 2048], BF16) for i in range(2)]
    it = 0
    for half in range(2):
        rs = slice(half * 128, (half + 1) * 128)
        for b in range(4):
            p2 = it % 2
            it += 1
            cs = slice(b * 2048, (b + 1) * 2048)
            P.dma('sp', ty[p2][:], C.hyY[rs, cs], w=['ty%d' % p2])
            P.dma('act', tv[p2][:], C.hyZ[rs, cs], w=['tv%d' % p2])
            P.dma('sp', tx[p2][:], C.hyX[256 + half * 128:256 + (half + 1) * 128, cs], w=['tx%d' % p2])
            P.dma('act', tg[p2][:], C.hyT[768 + half * 128:768 + (half + 1) * 128, cs], w=['tg%d' % p2])
            P.act(ts_[p2][:], tg[p2][:], ACT.Silu, r=['tg%d' % p2], w=['tsl%d' % p2])
            P.stt('dve', tf[p2][:], tv[p2][:], hbias[:, 2 + half:3 + half], ty[p2][:], ALU.mult, ALU.add, r=['tv%d' % p2, 'ty%d' % p2, 'hbias'], w=['tf%d' % p2])
            P.tt('pool', tf[p2][:], tf[p2][:], tx[p2][:], ALU.mult, r=['tf%d' % p2, 'tx%d' % p2], w=['tf%d' % p2])
            P.tt('dve', tz[p2][:], tf[p2][:], ts_[p2][:], ALU.mult, r=['tf%d' % p2, 'tsl%d' % p2], w=['tz%d' % p2])
            P.dma('sp', C.gT[768 + half * 128:768 + (half + 1) * 128, cs], tz[p2][:], r=['tz%d' % p2], w=[])
    P.end()
    if upd:
        _hyena_ctx(C, l)


def _hyena_ctx(C, l):
    P = C.P
    P.begin()
    nrm = _hy_filters(C, l, LC, C.featTc, C.decayTc, C.filtC)
    P.barrier()
    cwh = P.sb("cwh", [128, 18], F32)
    P.dma('sp', cwh[:], C.conv_hy[l], w=['cwh'])
    hbias = P.sb("hbias", [128, 4], F32)
    P.dma('sp', hbias[:], C.hy_bias[l], w=['hbias'])
    for half in range(2):
        fk = []
        for o in range(2):
            for d in range(2):
                cc = o * 4 + d * 2 + half
                t = P.sb("fk%d%d%d" % (half, o, d), [128, LC], F32)
                P.dma('sp', t[:], C.filtC[cc * 128:(cc + 1) * 128, :], w=['fk%d%d' % (o, d)])
                P.ts('dve', t[:], t[:], nrm[:, 4 + o * 2 + half:5 + o * 2 + half], None, ALU.mult, r=['fk%d%d' % (o, d), 'nrm'], w=['fk%d%d' % (o, d)])
                fk.append(t)
        u = []
        for c3 in range(3):
            rw = P.sb("rwc%d%d" % (half, c3), [128, LC + 2], BF16)
            P.memset('pool', rw[:, 0:1], 0.0, w=['rwc%d' % c3])
            P.memset('pool', rw[:, LC + 1:LC + 2], 0.0, w=['rwc%d' % c3])
            chunk = c3 * 2 + half
            P.dma('sp', rw[:, 1:LC + 1], C.hyT[chunk * 128:(chunk + 1) * 128, L:T], r=['rwc%d' % c3], w=['rwc%d' % c3])
            t = P.sb("uc%d%d" % (half, c3), [128, LC], F32)
            _conv3_fm(P, rw, 'rwc%d' % c3, cwh, chunk * 3, t, 'uc%d' % c3, LC)
            u.append(t)
        zin = u[0]
        zk = 'uc0'
        for o in range(2):
            fw, bw_ = fk[o * 2], fk[o * 2 + 1]
            NA_ = 4
            accs = [P.sb("accd%d%d%d" % (half, o, i), [128, LC], F32) for i in range(2 * NA_)]
            accp = [P.sb("accp%d%d%d" % (half, o, i), [128, LC], F32) for i in range(2)]
            tmpp = [P.sb("tmpp%d%d%d" % (half, o, i), [128, LC], F32) for i in range(2)]
            accf = accs[0]
            P.ts('dve', accs[0][:], zin[:], fw[:, 0:1], None, ALU.mult, r=[zk, 'fk%d0' % o], w=['ad0'])
            for i in range(1, 2 * NA_):
                P.memset('pool', accs[i][:], 0.0, w=['ad%d' % i])
            for i in range(2):
                P.memset('pool', accp[i][:], 0.0, w=['ap%d' % i])
            npool = 0
            for m in range(1, LC):
                ia = m % NA_
                P.stt('dve', accs[ia][:, m:], zin[:, :LC - m], fw[:, m:m + 1], accs[ia][:, m:], ALU.mult, ALU.add, r=[zk, 'fk%d0' % o, 'ad%d' % ia], w=['ad%d' % ia])
                if m % 4 == 0:
                    ip = npool % 2
                    npool += 1
                    P.ts('pool', tmpp[ip][:, :LC - m], zin[:, m:], bw_[:, m:m + 1], None, ALU.mult, r=[zk, 'fk%d1' % o], w=['tp%d' % ip])
                    P.tt('pool', accp[ip][:, :LC - m], accp[ip][:, :LC - m], tmpp[ip][:, :LC - m], ALU.add, r=['tp%d' % ip, 'ap%d' % ip], w=['ap%d' % ip])
                else:
                    ib = NA_ + m % NA_
                    P.stt('dve', accs[ib][:, :LC - m], zin[:, m:], bw_[:, m:m + 1], accs[ib][:, :LC - m], ALU.mult, ALU.add, r=[zk, 'fk%d1' % o, 'ad%d' % ib], w=['ad%d' % ib])
            for i in range(1, 2 * NA_):
                P.tt('dve', accf[:], accf[:], accs[i][:], ALU.add, r=['ad0', 'ad%d' % i], w=['ad0'])
            for i in range(2):
                P.tt('dve', accf[:], accf[:], accp[i][:], ALU.add, r=['ad0', 'ap%d' % i], w=['ad0'])
            P.stt('dve', accf[:], zin[:], hbias[:, o * 2 + half:o * 2 + half + 1], accf[:], ALU.mult, ALU.add, r=[zk, 'hbias', 'ad0'], w=['ad0'])
            znew = P.sb("zn%d%d" % (half, o), [128, LC], F32)
            P.tt('dve', znew[:], accf[:], u[1 + o][:], ALU.mult, r=['ad0', 'uc%d' % (1 + o)], w=['zn%d' % o])
            zin, zk = znew, 'zn%d' % o
        zg = P.sb("zg%d" % half, [128, LC], BF16)
        sl = P.sb("slc%d" % half, [128, LC], F32)
        ob = P.sb("obc%d" % half, [128, LC], BF16)
        P.dma('act', zg[:], C.hyT[768 + half * 128:768 + (half + 1) * 128, L:T], w=['zg'])
        P.act(sl[:], zg[:], ACT.Silu, r=['zg'], w=['slc'])
        P.tt('dve', ob[:], zin[:], sl[:], ALU.mult, r=[zk, 'slc'], w=['obc'])
        P.dma('sp', C.gT[768 + half * 128:768 + (half + 1) * 128, L:T], ob[:], r=['obc'], w=[])
    P.end()


def phase_outproj(C, l):
    P = C.P
    P.begin()
    upd = (l < DEPTH - 1)
    ntl = NT if upd else 64
    W = P.sb("Wo", [128, 8, D], BF16)
    wst = [P.sb("wst%d" % i, [128, D], F32) for i in range(2)]
    for k in range(8):
        P.dma('sp' if k % 2 == 0 else 'act', wst[k % 2][:], C.w_out[l, k * 128:(k + 1) * 128, :], w=['wst%d' % (k % 2)])
        P.cp('dve' if k % 2 == 0 else 'pool', W[:, k, :], wst[k % 2][:], r=['wst%d' % (k % 2)], w=['Wo'])
    ggate = P.sb("ggate", [128, 2, D], F32)
    for i in range(2):
        P.dma('sp', ggate[:, i, :], C.modrows[i:i + 1, 2 * D:3 * D].partition_broadcast(128), w=['ggate'])
    eps = P.sb("eps", [128, 1], F32)
    P.memset('pool', eps[:], 1e-6, w=['eps'])
    gt = [P.sb("gt%d" % i, [128, 8, 128], BF16) for i in range(2)]
    xt = [P.sb("xt%d" % i, [128, D], F32) for i in range(2)]
    yt = [P.sb("yt%d" % i, [128, D], F32) for i in range(2)]
    junk = P.sb("junk", [128, D], BF16)
    st2 = [P.sb("st%d" % i, [128, 8], F32) for i in range(2)]
    gv = C.gT.rearrange("(k p) t -> p k t", p=128)
    for tile in range(ntl):
        p2 = tile % 2
        st = st2[p2]
        sk = 'st%d_' % p2
        ci = 0 if tile < 64 else 1
        P.dma('sp', gt[p2][:], gv[:, :, tile * 128:(tile + 1) * 128], w=['gt%d' % p2])
        P.dma('act', xt[p2][:], xrows(C, l, tile), w=['xt%d' % p2])
        b0, b1 = C.bank[p2 * 2], C.bank[p2 * 2 + 1]
        k0, k1 = 'bank%d' % (p2 * 2), 'bank%d' % (p2 * 2 + 1)
        for half, (bk, kk) in enumerate(((b0, k0), (b1, k1))):
            for k in range(8):
                P.mm(bk[:, :], gt[p2][:, k, :], W[:, k, half * 512:(half + 1) * 512], start=(k == 0), stop=(k == 7),
                     r=['gt%d' % p2, 'Wo'], w=[kk])
        P.act(junk[:, 0:512], b0[:, :], ACT.Square, accum_out=st[:, 0:1], r=[k0], w=[sk + '0'])
        P.act(junk[:, 512:1024], b1[:, :], ACT.Square, accum_out=st[:, 1:2], r=[k1], w=[sk + '1'])
        P.tt('dve', st[:, 2:3], st[:, 0:1], st[:, 1:2], ALU.add, r=[sk + '0', sk + '1'], w=[sk + '2'])
        P.act(st[:, 3:4], st[:, 2:3], ACT.Sqrt, scale=1.0 / D, bias=eps[:, 0:1], r=[sk + '2', 'eps'], w=[sk + '3'])
        P.recip(st[:, 4:5], st[:, 3:4], r=[sk + '3'], w=[sk + '4'])
        P.stt('dve', yt[p2][:, 0:512], b0[:, :], st[:, 4:5], ggate[:, ci, 0:512], ALU.mult, ALU.mult, r=[k0, sk + '4', 'ggate'], w=['yt%d' % p2])
        P.stt('dve', yt[p2][:, 512:1024], b1[:, :], st[:, 4:5], ggate[:, ci, 512:1024], ALU.mult, ALU.mult, r=[k1, sk + '4', 'ggate'], w=['yt%d' % p2])
        P.tt('pool', yt[p2][:], yt[p2][:], xt[p2][:], ALU.add, r=['yt%d' % p2, 'xt%d' % p2], w=['yt%d' % p2])
        if upd:
            dst = C.x1[tile * 128:(tile + 1) * 128, :]
        else:
            dst = C.out[tile * 128:(tile + 1) * 128, :]
        P.dma('pool', dst, yt[p2][:], r=['yt%d' % p2], w=[])
    P.end()


def _bf16(a):
    import ml_dtypes
    return np.ascontiguousarray(a).astype(ml_dtypes.bfloat16)


_CONSTS = None


def host_consts():
    global _CONSTS
    if _CONSTS is not None:
        return _CONSTS
    f32 = np.float32
    c = {}
    c["ident_f"] = np.eye(128, dtype=f32)
    c["ident_b"] = _bf16(np.eye(128, dtype=f32))
    pairs = na_type_pairs()
    idx_r = np.zeros((NTYPES, 128, 128), np.int64)
    idx_c = np.zeros((NTYPES, 128, 128), np.int64)
    mask = np.zeros((NTYPES, 128, 128), f32)
    a = np.arange(128)
    krow_in, kcol = a // 64, a % 64
    for ty, (j, jp) in enumerate(pairs):
        qr = 2 * j + krow_in
        kr = 2 * jp + krow_in
        rs = np.clip(qr - 4, 0, 120)
        cs = np.clip(kcol - 8, 0, 48)
        dr = kr[:, None] - qr[None, :]
        okr = (kr[:, None] >= rs[None, :]) & (kr[:, None] < rs[None, :] + 8)
        okc = (kcol[:, None] >= cs[None, :]) & (kcol[:, None] < cs[None, :] + 16)
        ok = okr & okc
        idx_r[ty] = np.clip(dr + 7, 0, 14)
        idx_c[ty] = np.clip(kcol[:, None] - kcol[None, :], -15, 15) + 15
        mask[ty] = np.where(ok, 0.0, NEG)
    c["_na_idx_r"], c["_na_idx_c"] = idx_r, idx_c
    c["na_mask"] = np.ascontiguousarray(mask.transpose(1, 0, 2).reshape(128, NTYPES * 128))
    f = np.arange(128)
    jj = f % 64
    ax, half, n = jj // 32, (jj % 32) // 16, jj % 16
    inv = (10000.0 ** (-(np.arange(16, dtype=f32)) / 16)).astype(f32)
    t = np.arange(L)
    pos = np.stack([t // 64, t % 64], 0).astype(f32)
    ang = pos[ax, :] * inv[n][:, None]
    c["rope_cos"] = np.cos(ang).astype(f32)
    c["rope_sin"] = (np.where(half == 0, -1.0, 1.0)[:, None] * np.sin(ang)).astype(f32)
    partner = np.where(half == 0, f + 16, f - 16)
    perm = np.zeros((128, 128), f32)
    perm[partner, f] = 1.0
    c["perm_b"] = _bf16(perm)
    s_, t_ = np.meshgrid(np.arange(128), np.arange(128), indexing="ij")
    mf = np.where(s_ <= t_, 0.0, NEG).astype(f32)
    mb = np.where(s_ >= t_, 0.0, NEG).astype(f32)
    c["ml_mask"] = _bf16(np.concatenate([np.tile(mf, (1, 4)), np.tile(mb, (1, 4))], axis=1))
    selC = np.zeros((8, 8, 128), f32)
    for dh in range(8):
        selC[dh, dh, :] = 1.0
    c["selC"] = selC.reshape(8, 8 * 128)
    selC2 = np.zeros((40, 8, 128), f32)
    for dh in range(8):
        selC2[dh, dh, :] = 1.0
        selC2[32 + dh, dh, :] = 1.0
    c["selC2"] = _bf16(selC2.reshape(40, 8 * 128))
    sc = np.zeros((16, 4), f32)
    sc[8:, 0] = 1.0
    sc[:8, 1] = 1.0
    sc[0:4, 2] = 1.0
    sc[4:8, 3] = 1.0
    c["selcols"] = sc
    def feats(Ln):
        tt = np.arange(Ln, dtype=f32)
        tn = tt / f32(Ln - 1)
        fr = np.linspace(1e-4, 15.0, 16, dtype=f32)
        an = (f32(2.0 * np.pi / Ln) * tt[:, None] * fr[None, :]).astype(f32)
        feat = np.concatenate([tn[:, None], np.cos(an), -np.sin(an)], axis=-1).astype(f32)
        deltas = np.abs(np.linspace(np.log(1e-2) / 1.5, np.log(1e-2) / 0.3, 256, dtype=f32))
        dec = np.exp(-tn[:, None] * deltas[None, :]).astype(f32)
        return np.ascontiguousarray(feat.T), np.ascontiguousarray(dec.T)
    c["featT"], c["decayT"] = feats(L)
    c["featTc"], c["decayTc"] = feats(LC)
    k = np.arange(128)
    th = 2.0 * np.pi * np.outer(k, k) / 128.0
    Cm, Sm = np.cos(th), np.sin(th)
    N2 = 2 * L
    dft = np.zeros((128, 12, 128), np.float64)
    dft[:, 0], dft[:, 1] = Cm, -Sm
    dft[:, 2], dft[:, 3] = Cm, Sm
    dft[:, 4], dft[:, 5] = -Sm, Cm
    dft[:, 6], dft[:, 7] = Cm, Sm
    dft[:, 8], dft[:, 9] = -Sm, Cm
    dft[:, 10], dft[:, 11] = Cm / N2, -Sm / N2
    c["dft"] = _bf16(dft.reshape(128, 12 * 128).astype(f32))
    tht = 2.0 * np.pi * np.outer(k, k) / N2
    twr, tws = np.cos(tht), np.sin(tht)
    def rep(a0, a1):
        return np.tile(np.stack([a0, a1], 1)[:, None], (1, 4, 1, 1)).reshape(128, 1024)
    tw = np.concatenate([rep(twr, twr), rep(tws, -tws), rep(twr, twr), rep(-tws, tws)], axis=1)
    c["tw"] = _bf16(tw.astype(f32))
    _CONSTS = c
    return c


CONST_KEYS = ["ident_f", "ident_b", "na_mask", "rope_cos", "rope_sin", "perm_b", "ml_mask", "selC", "selC2", "selcols",
              "featT", "decayT", "featTc", "decayTc", "dft", "tw"]


def layout_inputs(inp):
    c = host_consts()
    f32 = np.float32
    shared = {k: c[k] for k in CONST_KEYS}
    shared["w_ada"] = np.ascontiguousarray(inp["w_ada"], f32)
    shared["b_ada"] = np.ascontiguousarray(inp["b_ada"], f32).reshape(DEPTH, 1, 3 * D)
    shared["g_pre"] = np.ascontiguousarray(inp["g_pre"], f32).reshape(DEPTH, 1, D)
    shared["g_post"] = np.ascontiguousarray(inp["g_post"], f32).reshape(DEPTH, 1, D)
    w_in = np.array(inp["w_in"], f32, copy=True)
    gm = w_in[:, :, 3328:3344].reshape(DEPTH, D, 2, 2, 4).copy()
    w_in[:, :, 3328:3336] = gm[:, :, :, 1, :].reshape(DEPTH, D, 8)
    w_in[:, :, 3336:3344] = gm[:, :, :, 0, :].reshape(DEPTH, D, 8)
    shared["w_in"] = w_in
    b_if = np.asarray(inp["b_if"], f32)
    shared["b_if"] = np.concatenate([b_if[:, :, 1, :].reshape(DEPTH, 8), b_if[:, :, 0, :].reshape(DEPTH, 8)], 1).reshape(DEPTH, 16, 1).copy()
    cm = np.asarray(inp["conv_ml"], f32)
    shared["conv_ml"] = np.ascontiguousarray(cm.reshape(DEPTH, 3, 4, 128).transpose(0, 3, 2, 1)).reshape(DEPTH, 128, 12)
    ch = np.asarray(inp["conv_hy"], f32)
    shared["conv_hy"] = np.ascontiguousarray(ch.reshape(DEPTH, 3, 6, 128).transpose(0, 3, 2, 1)).reshape(DEPTH, 128, 18)
    rpb = np.asarray(inp["rpb"], f32)
    g = rpb[:, :, c["_na_idx_r"], c["_na_idx_c"]]
    shared["rpb_g"] = np.ascontiguousarray(g.transpose(0, 1, 3, 2, 4)).reshape(DEPTH, 8, 128, NTYPES * 128)
    shared["hf_w1"] = np.ascontiguousarray(inp["hf_w1"], f32)
    shared["hf_b1"] = np.ascontiguousarray(inp["hf_b1"], f32).reshape(DEPTH, 64, 1)
    shared["hf_w2"] = np.ascontiguousarray(inp["hf_w2"], f32)
    shared["hf_b2"] = np.ascontiguousarray(inp["hf_b2"], f32).reshape(DEPTH, 64, 1)
    shared["hf_w3"] = np.ascontiguousarray(inp["hf_w3"], f32)
    shared["hf_freq"] = np.ascontiguousarray(np.asarray(inp["hf_freq"], f32).transpose(0, 2, 1))
    hb = np.asarray(inp["hy_bias"], f32)
    shared["hy_bias"] = np.ascontiguousarray(hb.reshape(DEPTH, 2, 2, 128).transpose(0, 3, 1, 2)).reshape(DEPTH, 128, 4)
    shared["w_out"] = np.ascontiguousarray(inp["w_out"], f32)
    maps = []
    x = np.asarray(inp["x"], f32)
    ctx = np.asarray(inp["ctx"], f32)
    cvec = np.asarray(inp["c"], f32)
    cctx = np.asarray(inp["c_ctx"], f32)
    for b in range(x.shape[0]):
        m = dict(shared)
        m["x"] = np.ascontiguousarray(x[b])
        m["ctx"] = np.ascontiguousarray(ctx[b])
        cc = np.stack([cvec[b].reshape(8, 128).T, cctx.reshape(8, 128).T], axis=-1)
        m["cc"] = np.ascontiguousarray(cc.reshape(128, 16))
        maps.append(m)
    return maps


_PROG = None


def kernel(**inputs):
    global _PROG
    if _PROG is None:
        _PROG = build_program()
    nc, _ = _PROG
    maps = layout_inputs(inputs)
    res = run_bass_kernel_spmd(nc, maps, core_ids=list(range(len(maps))))
    return np.stack([np.asarray(r["out"], np.float32) for r in res.results], axis=0)
```

```python
import contextlib
import numpy as np
import concourse.bass as bass
import concourse.mybir as mybir
from concourse.bass_utils import run_bass_kernel_spmd

F32 = mybir.dt.float32
BF16 = mybir.dt.bfloat16
ACT = mybir.ActivationFunctionType
ALU = mybir.AluOpType
AX = mybir.AxisListType

NDMA_SEMS = 6


class Prog:
    ENGS = ('pe', 'act', 'dve', 'pool', 'sp')

    def __init__(self, nc):
        self.nc = nc
        self.stack = contextlib.ExitStack()
        self.streams = {e: [] for e in self.ENGS}
        self.new_semset()
        self.waited = {e: {} for e in self.ENGS}
        self.lastw = {}
        self.readers = {}
        self.n_inst = 0

    def new_semset(self):
        self._setid = getattr(self, "_setid", -1) + 1
        t = "_%d" % self._setid
        self.sem = {}
        self.cnt = {}
        for e in self.ENGS:
            self.sem[e] = self.stack.enter_context(self.nc.semaphore("s_" + e + t))
            self.cnt[e] = 0
        self.dsem = {}
        self.dcnt = {}
        self.dnext = {}
        for q in ('sp', 'pool', 'act'):
            self.dsem[q] = [self.stack.enter_context(self.nc.semaphore("d_%s%d%s" % (q, i, t))) for i in range(NDMA_SEMS)]
            self.dcnt[q] = [0] * NDMA_SEMS
            self.dnext[q] = 0
        self.waited = {e: {} for e in self.ENGS}

    def begin(self):
        self.pstack = contextlib.ExitStack()

    def sb(self, name, shape, dtype):
        self._uid = getattr(self, "_uid", 0) + 1
        return self.pstack.enter_context(self.nc.sbuf_tensor("%s_%d" % (name, self._uid), list(shape), dtype))

    def ps(self, name, shape, dtype):
        return self.stack.enter_context(self.nc.psum_tensor(name, list(shape), dtype))

    def _need(self, eng, ev):
        sem, val, name = ev
        w = self.waited[eng]
        if w.get(name, 0) >= val:
            return
        w[name] = val
        self.streams[eng].append(lambda e, sem=sem, val=val: e.wait_ge(sem, val))

    def _deps(self, eng, r, w, skip_same=False):
        evs = []
        for k in r:
            if k in self.lastw:
                evs.append(self.lastw[k])
        for k in w:
            if k in self.lastw:
                evs.append(self.lastw[k])
            evs.extend(self.readers.get(k, ()))
        for ev in evs:
            if skip_same and ev[2] == eng:
                continue
            self._need(eng, ev)

    def _record(self, ev, r, w):
        for k in w:
            self.lastw[k] = ev
            self.readers[k] = []
        for k in r:
            if k in w:
                continue
            self.readers.setdefault(k, []).append(ev)

    def op(self, eng, fn, r=(), w=()):
        self._deps(eng, r, w, skip_same=(eng == 'pe'))
        self.cnt[eng] += 1
        sem, val = self.sem[eng], self.cnt[eng]
        self.streams[eng].append(lambda e, fn=fn, sem=sem: fn(e).then_inc(sem, 1))
        self._record((sem, val, eng), r, w)
        self.n_inst += 1

    def dma(self, q, out, in_, r=(), w=(), **kw):
        self._deps(q, r, w)
        i = self.dnext[q]
        self.dnext[q] = (i + 1) % NDMA_SEMS
        sem = self.dsem[q][i]
        name = "d_%s%d" % (q, i)
        if self.dcnt[q][i] > 0:
            self._need(q, (sem, self.dcnt[q][i], name))
        self.dcnt[q][i] += 16
        val = self.dcnt[q][i]
        self.streams[q].append(lambda e, out=out, in_=in_, sem=sem, kw=kw: e.dma_start(out=out, in_=in_, **kw).then_inc(sem, 16))
        self._record((sem, val, name), r, w)
        self.n_inst += 1

    def barrier(self):
        evs = []
        for q in self.dsem:
            for i in range(NDMA_SEMS):
                if self.dcnt[q][i]:
                    evs.append((self.dsem[q][i], self.dcnt[q][i], "d_%s%d" % (q, i)))
        for e in self.ENGS:
            if self.cnt[e]:
                evs.append((self.sem[e], self.cnt[e], e))
        for e in self.ENGS:
            for ev in evs:
                if ev[2] != e:
                    self._need(e, ev)
        self.lastw = {}
        self.readers = {}

    def end(self):
        self.barrier()
        streams = self.streams
        with self.nc.Block() as block:
            @block.tensor
            def _(eng):
                for f in streams['pe']:
                    f(eng)

            @block.scalar
            def _(eng):
                for f in streams['act']:
                    f(eng)

            @block.vector
            def _(eng):
                for f in streams['dve']:
                    f(eng)

            @block.gpsimd
            def _(eng):
                for f in streams['pool']:
                    f(eng)

            @block.sync
            def _(eng):
                for f in streams['sp']:
                    f(eng)
        self.streams = {e: [] for e in self.ENGS}
        self.pstack.close()

    def finish(self):
        self.stack.close()

    def mm(self, out, lhsT, rhs, start=True, stop=True, r=(), w=()):
        self.op('pe', lambda e: e.matmul(out, lhsT, rhs, start=start, stop=stop), r, w)

    def tr(self, out, in_, ident, r=(), w=()):
        self.op('pe', lambda e: e.transpose(out, in_, ident), r, w)

    def act(self, out, in_, func, r=(), w=(), **kw):
        self.op('act', lambda e: e.activation(out, in_, func, **kw), r, w)

    def ts(self, eng, out, in0, s1, s2, op0, op1=None, r=(), w=()):
        if op1 is None:
            self.op(eng, lambda e: e.tensor_scalar(out, in0, s1, None, op0), r, w)
        else:
            self.op(eng, lambda e: e.tensor_scalar(out, in0, s1, s2, op0, op1), r, w)

    def tt(self, eng, out, in0, in1, op, r=(), w=()):
        self.op(eng, lambda e: e.tensor_tensor(out, in0, in1, op), r, w)

    def stt(self, eng, out, in0, sc, in1, op0, op1, r=(), w=()):
        self.op(eng, lambda e: e.scalar_tensor_tensor(out, in0, sc, in1, op0, op1), r, w)

    def cp(self, eng, out, in_, r=(), w=()):
        if eng == 'act':
            self.op(eng, lambda e: e.copy(out, in_), r, w)
        else:
            self.op(eng, lambda e: e.tensor_copy(out, in_), r, w)

    def recip(self, out, in_, r=(), w=()):
        self.op('dve', lambda e: e.reciprocal(out, in_), r, w)

    def memset(self, eng, ap, val, w=()):
        self.op(eng, lambda e: e.memset(ap, val), (), w)


L = 8192
LC = 256
T = L + LC
NT = T // 128
D = 1024
NIN = 4368
DEPTH = 2
NTYPES = 21
NEG = -30000.0
PI = float(np.pi)


def na_tile_list(j):
    if j == 0:
        return [(jp, 5 + jp) for jp in range(4)]
    if j == 1:
        return [(jp, 9 + jp) for jp in range(4)]
    if j == 62:
        return [(60 + i, 13 + i) for i in range(4)]
    if j == 63:
        return [(60 + i, 17 + i) for i in range(4)]
    return [(j + d, d + 2) for d in range(-2, 3)]


def na_type_pairs():
    reps = {}
    for j in [10, 0, 1, 62, 63]:
        for jp, ty in na_tile_list(j):
            reps[ty] = (j, jp)
    return [reps[t] for t in range(NTYPES)]


class Ctx:
    pass


def build_program(n_layers=DEPTH, stop_after=None, debug=()):
    nc = bass.Bass("TRN2", target_bir_lowering=False)
    P = Prog(nc)
    C = Ctx()
    C.nc, C.P = nc, P

    def din(name, shape, dt=F32):
        return nc.dram_tensor(name, list(shape), dt, kind="ExternalInput").ap()

    def scratch(name, shape, dt):
        kind = "ExternalOutput" if name in debug else "Internal"
        return nc.dram_tensor(name, list(shape), dt, kind=kind).ap()

    C.x = din("x", [L, D]); C.ctx = din("ctx", [LC, D]); C.cc = din("cc", [128, 16])
    C.w_ada = din("w_ada", [DEPTH, D, 3 * D]); C.b_ada = din("b_ada", [DEPTH, 1, 3 * D])
    C.g_pre = din("g_pre", [DEPTH, 1, D]); C.g_post = din("g_post", [DEPTH, 1, D])
    C.w_in = din("w_in", [DEPTH, D, NIN]); C.b_if = din("b_if", [DEPTH, 16, 1])
    C.conv_ml = din("conv_ml", [DEPTH, 128, 12]); C.conv_hy = din("conv_hy", [DEPTH, 128, 18])
    C.rpb_g = din("rpb_g", [DEPTH, 8, 128, NTYPES * 128])
    C.hf_w1 = din("hf_w1", [DEPTH, 33, 64]); C.hf_b1 = din("hf_b1", [DEPTH, 64, 1])
    C.hf_w2 = din("hf_w2", [DEPTH, 64, 64]); C.hf_b2 = din("hf_b2", [DEPTH, 64, 1])
    C.hf_w3 = din("hf_w3", [DEPTH, 64, 1024]); C.hf_freq = din("hf_freq", [DEPTH, 64, 2])
    C.hy_bias = din("hy_bias", [DEPTH, 128, 4]); C.w_out = din("w_out", [DEPTH, D, D])
    C.ident_f = din("ident_f", [128, 128]); C.ident_b = din("ident_b", [128, 128], BF16)
    C.na_mask = din("na_mask", [128, NTYPES * 128])
    C.rope_cos = din("rope_cos", [128, L]); C.rope_sin = din("rope_sin", [128, L])
    C.perm_b = din("perm_b", [128, 128], BF16)
    C.ml_mask = din("ml_mask", [128, 2 * 512], BF16)
    C.selC = din("selC", [8, 8 * 128]); C.selcols = din("selcols", [16, 4]); C.selC2 = din("selC2", [64, 8 * 128], BF16)
    C.featT = din("featT", [33, L]); C.decayT = din("decayT", [256, L])
    C.featTc = din("featTc", [33, LC]); C.decayTc = din("decayTc", [256, LC])
    C.dft = din("dft", [128, 12 * 128], BF16)
    C.tw = din("tw", [128, 4 * 1024], BF16)
    C.out = nc.dram_tensor("out", [L, D], F32, kind="ExternalOutput").ap()
    C.x1 = scratch("x1", [T, D], F32)
    C.modrows = scratch("modrows", [2, 3 * D], F32)
    C.qkT = scratch("qkT", [1024, T], BF16)
    C.qkmT = scratch("qkmT", [512, T], BF16)
    C.qkm2 = scratch("qkm2", [512, T], BF16)
    C.hyT = scratch("hyT", [1024, T], BF16)
    C.gmT = scratch("gmT", [16, T], F32)
    C.pT = scratch("pT", [T, 1792], BF16)
    C.gT = scratch("gT", [D, T], BF16)
    C.filtT = scratch("filtT", [1024, L], F32)
    C.filtN = scratch("filtN", [1024, L], BF16)
    C.kspec = scratch("kspec", [2, 128, 256 * 256], BF16)
    C.hyV = scratch("hyV", [256, L], BF16)
    C.hyX = scratch("hyX", [512, L], BF16)
    C.hyY = scratch("hyY", [256, L], BF16)
    C.hyZ = scratch("hyZ", [256, L], BF16)
    C.filtC = scratch("filtC", [1024, LC], F32)
    C.bank = [P.ps("bank%d" % i, [128, 512], F32) for i in range(7)]
    C.psb = P.ps("psb", [128, 1024], BF16)

    phases = []
    for l in range(n_layers):
        phases += [("mods", l), ("inproj", l), ("na", l), ("mlstm", l), ("hyena", l), ("outproj", l)]
    for name, l in phases:
        if name == "mods" and l > 0:
            P.new_semset()
        fn = globals().get("phase_" + name)
        if fn is not None:
            fn(C, l)
        if stop_after == (name, l):
            break
    P.finish()
    C.n_inst = P.n_inst
    return nc, C


def xrows(C, l, tile):
    if l == 0:
        if tile < 64:
            return C.x[tile * 128:(tile + 1) * 128, :]
        return C.ctx[(tile - 64) * 128:(tile - 63) * 128, :]
    return C.x1[tile * 128:(tile + 1) * 128, :]


def phase_mods(C, l):
    P = C.P
    P.begin()
    cc = P.sb("cc", [128, 16], F32)
    sc = P.sb("sc", [128, 16], F32)
    P.dma('sp', cc[:], C.cc, w=['cc'])
    P.act(sc[:], cc[:], ACT.Silu, r=['cc'], w=['sc'])
    wa = [P.sb("wa%d" % i, [128, 3 * D], F32) for i in range(2)]
    for k in range(8):
        t = wa[k % 2]
        P.dma('sp' if k % 2 == 0 else 'act', t[:], C.w_ada[l, k * 128:(k + 1) * 128, :], w=['wa%d' % (k % 2)])
        for nb in range(6):
            P.mm(C.bank[nb][0:2, :], sc[:, 2 * k:2 * k + 2], t[:, nb * 512:(nb + 1) * 512], start=(k == 0), stop=(k == 7),
                 r=['sc', 'wa%d' % (k % 2)], w=['bank%d' % nb])
    mod = P.sb("mod", [2, 3 * D], F32)
    bb = P.sb("bb", [2, 3 * D], F32)
    gp = P.sb("gp", [2, 2 * D], F32)
    res = P.sb("res", [2, 3 * D], F32)
    P.dma('sp', bb[:], C.b_ada[l].partition_broadcast(2), w=['bb'])
    P.dma('sp', gp[:, 0:D], C.g_pre[l].partition_broadcast(2), w=['gp0'])
    P.dma('sp', gp[:, D:2 * D], C.g_post[l].partition_broadcast(2), w=['gp1'])
    for nb in range(6):
        P.tt('dve', mod[:, nb * 512:(nb + 1) * 512], C.bank[nb][0:2, :], bb[:, nb * 512:(nb + 1) * 512], ALU.add,
             r=['bank%d' % nb, 'bb'], w=['mod'])
    P.stt('dve', res[:, 0:D], mod[:, D:2 * D], 1.0, gp[:, 0:D], ALU.add, ALU.mult, r=['mod', 'gp0'], w=['res'])
    P.cp('dve', res[:, D:2 * D], mod[:, 0:D], r=['mod'], w=['res'])
    P.tt('dve', res[:, 2 * D:3 * D], mod[:, 2 * D:3 * D], gp[:, D:2 * D], ALU.mult, r=['mod', 'gp1'], w=['res'])
    P.dma('sp', C.modrows, res[:], r=['res'], w=['modrows'])
    P.end()


def phase_inproj(C, l):
    P = C.P
    P.begin()
    W = P.sb("W", [128, 8, NIN], BF16)
    wst = [P.sb("wst%d" % i, [128, NIN], F32) for i in range(2)]
    for k in range(8):
        P.dma('sp' if k % 2 == 0 else 'act', wst[k % 2][:], C.w_in[l, k * 128:(k + 1) * 128, :], w=['wst%d' % (k % 2)])
        P.cp('dve' if k % 2 == 0 else 'pool', W[:, k, :], wst[k % 2][:], r=['wst%d' % (k % 2)], w=['W'])
    gmod = P.sb("gmod", [128, 2, D], F32)
    shift = P.sb("shift", [128, 2, D], F32)
    for i in range(2):
        P.dma('sp', gmod[:, i, :], C.modrows[i:i + 1, 0:D].partition_broadcast(128), w=['gmod'])
        P.dma('act', shift[:, i, :], C.modrows[i:i + 1, D:2 * D].partition_broadcast(128), w=['shift'])
    idb = P.sb("idb", [128, 128], BF16)
    P.dma('sp', idb[:], C.ident_b, w=['idb'])
    eps = P.sb("eps", [128, 1], F32)
    P.memset('pool', eps[:], 1e-6, w=['eps'])
    xt = [P.sb("xt%d" % i, [128, D], F32) for i in range(3)]
    junk = P.sb("junk", [128, D], BF16)
    hf = [P.sb("hf%d" % i, [128, D], F32) for i in range(2)]
    hb = [P.sb("hb%d" % i, [128, D], BF16) for i in range(2)]
    st3 = [P.sb("st%d" % i, [128, 8], F32) for i in range(3)]
    hT = [P.sb("hT%d" % i, [128, 8, 512], BF16) for i in range(2)]
    stF = [P.sb("stF%d" % i, [128, 512], BF16) for i in range(3)]
    stG = P.sb("stG", [16, 512], F32)
    stT = [P.sb("stT%d" % i, [128, 1792], BF16) for i in range(2)]
    fchunks = []
    for i in range(4):
        fchunks.append((0 + i * 128, C.qkT, i * 128, 0.125))
    for i in range(4):
        fchunks.append((512 + i * 128, C.qkT, 512 + i * 128, 1.0))
    for i in range(4):
        fchunks.append((2048 + i * 128, C.qkmT, i * 128, 1.0))
    for i in range(8):
        fchunks.append((3344 + i * 128, C.hyT, i * 128, 1.0))
    tcols = [(1024, 0, 512), (1536, 512, 512), (2560, 1024, 512), (3072, 1536, 256)]
    blocks = [(b * 4, 4, 0) for b in range(16)] + [(64, 2, 1)]
    nbank = 0
    tcount = 0
    for bi, (t0, ntl, ci) in enumerate(blocks):
        h_ = hT[bi % 2]
        hk = 'hT%d' % (bi % 2)
        ncol = ntl * 128
        for i in range(ntl):
            tile = t0 + i
            xi = tcount % 3
            st = st3[xi]
            sk = 'st%d_' % xi
            tcount += 1
            P.dma('sp' if tile % 2 == 0 else 'act', xt[xi][:], xrows(C, l, tile), w=['xt%d' % xi])
            P.act(junk[:], xt[xi][:], ACT.Square, accum_out=st[:, 0:1], r=['xt%d' % xi], w=[sk + '0'])
            P.act(st[:, 1:2], st[:, 0:1], ACT.Sqrt, scale=1.0 / D, bias=eps[:, 0:1], r=[sk + '0', 'eps'], w=[sk + '1'])
            P.recip(st[:, 2:3], st[:, 1:2], r=[sk + '1'], w=[sk + '2'])
            hi = tile % 2
            P.stt('dve', hf[hi][:], xt[xi][:], st[:, 2:3], gmod[:, ci, :], ALU.mult, ALU.mult,
                  r=['xt%d' % xi, sk + '2', 'gmod'], w=['hf%d' % hi])
            P.tt('pool', hb[hi][:], hf[hi][:], shift[:, ci, :], ALU.add, r=['hf%d' % hi, 'shift'], w=['hb%d' % hi])
            for k in range(8):
                P.tr(C.psb[:, k * 128:(k + 1) * 128], hb[hi][:, k * 128:(k + 1) * 128], idb[:], r=['hb%d' % hi, 'idb'], w=['psb'])
            P.cp('act', h_[:, :, i * 128:(i + 1) * 128], C.psb[:].rearrange("p (k t) -> p k t", k=8), r=['psb'], w=[hk])
        for fi, (wc, dst, drow, scl) in enumerate(fchunks):
            bk = nbank % 4
            nbank += 1
            for k in range(8):
                P.mm(C.bank[bk][:, 0:ncol], W[:, k, wc:wc + 128], h_[:, k, 0:ncol], start=(k == 0), stop=(k == 7),
                     r=['W', hk], w=['bank%d' % bk])
            si = fi % 3
            if fi % 2 == 0:
                P.act(stF[si][:, 0:ncol], C.bank[bk][:, 0:ncol], ACT.Copy, scale=scl, r=['bank%d' % bk], w=['stF%d' % si])
            else:
                P.ts('dve', stF[si][:, 0:ncol], C.bank[bk][:, 0:ncol], scl, None, ALU.mult, r=['bank%d' % bk], w=['stF%d' % si])
            P.dma('pool', dst[drow:drow + 128, t0 * 128:t0 * 128 + ncol], stF[si][:, 0:ncol], r=['stF%d' % si], w=[])
        bk = nbank % 4
        nbank += 1
        for k in range(8):
            P.mm(C.bank[bk][0:16, 0:ncol], W[:, k, 3328:3344], h_[:, k, 0:ncol], start=(k == 0), stop=(k == 7),
                 r=['W', hk], w=['bank%d' % bk])
        P.cp('dve', stG[:, 0:ncol], C.bank[bk][0:16, 0:ncol], r=['bank%d' % bk], w=['stG'])
        P.dma('act', C.gmT[:, t0 * 128:t0 * 128 + ncol], stG[:, 0:ncol], r=['stG'], w=[])
        for i in range(ntl):
            tile = t0 + i
            so = stT[tile % 2]
            sk = 'stT%d' % (tile % 2)
            for ti, (wc, pc, wd) in enumerate(tcols):
                bk = nbank % 4
                nbank += 1
                for k in range(8):
                    P.mm(C.bank[bk][:, 0:wd], h_[:, k, i * 128:(i + 1) * 128], W[:, k, wc:wc + wd], start=(k == 0), stop=(k == 7),
                         r=['W', hk], w=['bank%d' % bk])
                if ti % 2 == 0:
                    P.cp('act', so[:, pc:pc + wd], C.bank[bk][:, 0:wd], r=['bank%d' % bk], w=[sk])
                else:
                    P.cp('dve', so[:, pc:pc + wd], C.bank[bk][:, 0:wd], r=['bank%d' % bk], w=[sk])
            P.dma('pool', C.pT[tile * 128:(tile + 1) * 128, :], so[:], r=[sk], w=[])
    P.end()


def phase_na(C, l):
    P = C.P
    P.begin()
    upd = (l < DEPTH - 1)
    nq = NT if upd else 64
    idb = P.sb("idb", [128, 128], BF16)
    P.dma('sp', idb[:], C.ident_b, w=['idb'])
    maskt = P.sb("maskt", [128, NTYPES * 128], F32)
    P.dma('act', maskt[:], C.na_mask, w=['maskt'])
    ona = P.sb("ona", [128, NT, 512], BF16)
    qT = P.sb("qT", [64, T], BF16)
    kT = P.sb("kT", [64, T], BF16)
    V1 = P.sb("V1", [128, NT, 65], BF16)
    P.memset('pool', V1[:, :, 64:65], 1.0, w=['V1'])
    biasf = P.sb("biasf", [128, NTYPES * 128], F32)
    biasb = P.sb("biasb", [128, NTYPES * 128], BF16)
    E = [P.sb("E%d" % i, [128, 7 * 128], BF16) for i in range(2)]
    rr = P.sb("rr", [128, 4], F32)
    pv = C.pT.rearrange("(t p) c -> p t c", p=128)
    def tiles_of(j):
        if j < 64:
            return na_tile_list(j) + [(64, None), (65, None)]
        return [(64, None), (65, None)]

    def head_loads(h):
        P.dma('sp', qT[:], C.qkT[h * 64:(h + 1) * 64, :], w=['qT'])
        P.dma('act', kT[:], C.qkT[512 + h * 64:512 + (h + 1) * 64, :], w=['kT'])
        P.dma('sp', V1[:, 0:33, 0:64], pv[:, 0:33, h * 64:(h + 1) * 64], w=['V1'])
        P.dma('act', V1[:, 33:NT, 0:64], pv[:, 33:NT, h * 64:(h + 1) * 64], w=['V1'])
        P.dma('sp', biasf[:], C.rpb_g[l, h], w=['biasf'])
        P.tt('dve', biasf[:], biasf[:], maskt[:], ALU.add, r=['biasf', 'maskt'], w=['biasf'])
        P.act(biasb[:], biasf[:], ACT.Exp, r=['biasf'], w=['biasb'])

    def stS(n):
        h, j = n // nq, n % nq
        if j == 0:
            head_loads(h)
        tiles = tiles_of(j)
        par = n % 2
        bA, bB = C.bank[par * 2], C.bank[par * 2 + 1]
        kA, kB = 'bank%d' % (par * 2), 'bank%d' % (par * 2 + 1)
        e_ = E[par]
        ek = 'E%d' % par
        for i, (jp, ty) in enumerate(tiles):
            bk, kk = (bA, kA) if i < 4 else (bB, kB)
            col = (i % 4) * 128
            P.mm(bk[:, col:col + 128], kT[:, jp * 128:(jp + 1) * 128], qT[:, j * 128:(j + 1) * 128],
                 start=True, stop=True, r=['kT', 'qT'], w=[kk])
        nt_ = len(tiles)
        na_ = min(nt_, 4)
        P.act(e_[:, 0:na_ * 128], bA[:, 0:na_ * 128], ACT.Exp, r=[kA], w=[ek])
        if nt_ > 4:
            P.act(e_[:, 512:nt_ * 128], bB[:, 0:(nt_ - 4) * 128], ACT.Exp, r=[kB], w=[ek])
        nl = nt_ - 2
        if nl > 0:
            ty0 = tiles[0][1]
            P.tt('dve', e_[:, 0:nl * 128], e_[:, 0:nl * 128], biasb[:, ty0 * 128:(ty0 + nl) * 128], ALU.mult, r=[ek, 'biasb'], w=[ek])

    def stPV(n):
        h, j = n // nq, n % nq
        tiles = tiles_of(j)
        par = n % 2
        bO, kO = C.bank[4 + par], 'bank%d' % (4 + par)
        e_ = E[par]
        ek = 'E%d' % par
        nt_ = len(tiles)
        for i, (jp, ty) in enumerate(tiles):
            P.mm(bO[:, 0:65], e_[:, i * 128:(i + 1) * 128], V1[:, jp, :], start=(i == 0), stop=(i == nt_ - 1),
                 r=[ek, 'V1'], w=[kO])
        P.recip(rr[:, par:par + 1], bO[:, 64:65], r=[kO], w=['rr%d' % par])
        P.ts('dve', ona[:, j, h * 64:(h + 1) * 64], bO[:, 0:64], rr[:, par:par + 1], None, ALU.mult,
             r=[kO, 'rr%d' % par], w=['ona'])

    for h_ in range(8):
        _run_pipeline([(lambda j, h_=h_: stS(h_ * nq + j), 0), (lambda j, h_=h_: stPV(h_ * nq + j), 1)], nq)
    za = [P.sb("za%d" % i, [128, 512], BF16) for i in range(2)]
    sil = [P.sb("sil%d" % i, [128, 512], F32) for i in range(2)]
    gg = [P.sb("gg%d" % i, [128, 512], BF16) for i in range(2)]
    gst = [P.sb("gst%d" % i, [128, 512], BF16) for i in range(2)]
    for j in range(nq):
        p2 = j % 2
        P.dma('sp', za[p2][:], C.pT[j * 128:(j + 1) * 128, 512:1024], w=['za%d' % p2])
        P.act(sil[p2][:], za[p2][:], ACT.Silu, r=['za%d' % p2], w=['sil%d' % p2])
        P.tt('dve', gg[p2][:], ona[:, j, :], sil[p2][:], ALU.mult, r=['ona', 'sil%d' % p2], w=['gg%d' % p2])
        for c4 in range(4):
            P.tr(C.psb[:, c4 * 128:(c4 + 1) * 128], gg[p2][:, c4 * 128:(c4 + 1) * 128], idb[:], r=['gg%d' % p2, 'idb'], w=['psb'])
        P.cp('act', gst[p2][:], C.psb[:, 0:512], r=['psb'], w=['gst%d' % p2])
        P.dma('act', C.gT[0:512, j * 128:(j + 1) * 128].rearrange("(c p) t -> p c t", p=128),
              gst[p2][:].rearrange("p (c t) -> p c t", c=4), r=['gst%d' % p2], w=[])
    P.end()


def phase_mlstm(C, l):
    P = C.P
    upd = (l < DEPTH - 1)
    P.begin()
    cw = P.sb("cw", [128, 12], F32)
    P.dma('sp', cw[:], C.conv_ml[l], w=['cw'])
    perm = P.sb("perm", [128, 128], BF16)
    P.dma('act', perm[:], C.perm_b, w=['perm'])
    cosb = [P.sb("cosb%d" % i, [128, 512], F32) for i in range(2)]
    sinb = [P.sb("sinb%d" % i, [128, 512], F32) for i in range(2)]
    raw = [P.sb("raw%d" % i, [128, 514], BF16) for i in range(3)]
    a_ = [P.sb("a%d" % i, [128, 512], F32) for i in range(2)]
    s_ = [P.sb("s%d" % i, [128, 512], F32) for i in range(2)]
    sb16 = [P.sb("sb16%d" % i, [128, 512], BF16) for i in range(2)]
    o1 = [P.sb("o1%d" % i, [128, 512], F32) for i in range(2)]
    o2 = [P.sb("o2%d" % i, [128, 512], F32) for i in range(2)]
    ob = [P.sb("ob%d" % i, [128, 512], BF16) for i in range(2)]
    blocks = [(b * 512, 512, 0, L) for b in range(16)] + [(L, 256, L, T)]
    it = 0
    for bi, (t0, n, s0, s1) in enumerate(blocks):
        lat = t0 < L
        cb, sn = cosb[bi % 2], sinb[bi % 2]
        if lat:
            P.dma('sp', cb[:], C.rope_cos[:, t0:t0 + 512], w=['cos%d' % (bi % 2)])
            P.dma('act', sn[:], C.rope_sin[:, t0:t0 + 512], w=['sin%d' % (bi % 2)])
        for c in range(4):
            ri = it % 3
            p2 = it % 2
            it += 1
            rw = raw[ri]
            rk = 'raw%d' % ri
            lo = max(t0 - 1, s0)
            hi = min(t0 + n + 1, s1)
            if lo > t0 - 1:
                P.memset('pool', rw[:, 0:1], 0.0, w=[rk])
            if hi < t0 + n + 1:
                P.memset('pool', rw[:, n + 1:n + 2], 0.0, w=[rk])
            P.dma('sp' if it % 2 == 0 else 'act', rw[:, lo - (t0 - 1):hi - (t0 - 1)], C.qkmT[c * 128:(c + 1) * 128, lo:hi], r=[rk], w=[rk])
            a, sv = a_[p2], s_[p2]
            P.ts('dve', a[:, 0:n], rw[:, 0:n], cw[:, c * 3:c * 3 + 1], None, ALU.mult, r=[rk, 'cw'], w=['a%d' % p2])
            P.stt('dve', a[:, 0:n], rw[:, 1:n + 1], cw[:, c * 3 + 1:c * 3 + 2], a[:, 0:n], ALU.mult, ALU.add, r=[rk, 'cw', 'a%d' % p2], w=['a%d' % p2])
            P.stt('dve', a[:, 0:n], rw[:, 2:n + 2], cw[:, c * 3 + 2:c * 3 + 3], a[:, 0:n], ALU.mult, ALU.add, r=[rk, 'cw', 'a%d' % p2], w=['a%d' % p2])
            P.act(sv[:, 0:n], a[:, 0:n], ACT.Silu, r=['a%d' % p2], w=['s%d' % p2])
            if c >= 2:
                P.act(sv[:, 0:n], sv[:, 0:n], ACT.Copy, scale=0.125, r=['s%d' % p2], w=['s%d' % p2])
            if lat:
                P.cp('act', sb16[p2][:, 0:n], sv[:, 0:n], r=['s%d' % p2], w=['sb16%d' % p2])
                bk = C.bank[p2]
                P.mm(bk[:, 0:n], perm[:], sb16[p2][:, 0:n], r=['perm', 'sb16%d' % p2], w=['bank%d' % p2])
                P.tt('pool', o1[p2][:, 0:n], sv[:, 0:n], cb[:, 0:n], ALU.mult, r=['s%d' % p2, 'cos%d' % (bi % 2)], w=['o1%d' % p2])
                P.tt('dve', o2[p2][:, 0:n], bk[:, 0:n], sn[:, 0:n], ALU.mult, r=['bank%d' % p2, 'sin%d' % (bi % 2)], w=['o2%d' % p2])
                P.tt('pool', ob[p2][:, 0:n], o1[p2][:, 0:n], o2[p2][:, 0:n], ALU.add, r=['o1%d' % p2, 'o2%d' % p2], w=['ob%d' % p2])
            else:
                P.cp('act', ob[p2][:, 0:n], sv[:, 0:n], r=['s%d' % p2], w=['ob%d' % p2])
            P.dma('sp', C.qkm2[c * 128:(c + 1) * 128, t0:t0 + n], ob[p2][:, 0:n], r=['ob%d' % p2], w=[])
    P.end()

    P.begin()
    cum = P.sb("cum", [8, T], F32)
    ict = P.sb("ict", [128, NT, 16], F32)
    wt = P.sb("wt", [128, NT, 16], F32)
    idf = P.sb("idf", [128, 128], F32)
    P.dma('sp', idf[:], C.ident_f, w=['idf'])
    idb = P.sb("idb", [128, 128], BF16)
    P.dma('sp', idb[:], C.ident_b, w=['idb'])
    bif = P.sb("bif", [16, 1], F32)
    P.dma('act', bif[:], C.b_if[l], w=['bif'])
    selc = P.sb("selc", [16, 4], F32)
    P.dma('act', selc[:], C.selcols, w=['selc'])
    selC = P.sb("selC2", [64, 8 * 128], BF16)
    P.dma('sp', selC[:], C.selC2, w=['selC'])
    cumhl = P.sb("cumhl", [64, T], BF16)
    P.memset('pool', cumhl[:], 0.0, w=['cumhl'])
    hif = P.sb("hif", [8, 1024], F32)
    lob = P.sb("lob", [8, 1024], BF16)
    mlm = P.sb("mlm", [128, 2 * 512], BF16)
    P.dma('sp', mlm[:], C.ml_mask, w=['mlm'])
    NB = 1024
    gmb = P.sb("gmb", [16, NB], F32)
    e1 = P.sb("e1", [16, NB], F32)
    xa = P.sb("xa", [8, NB], F32)
    xb = P.sb("xb", [8, NB], F32)
    x0 = P.sb("x0", [8, NB], F32)
    cF = P.sb("cF", [8, NB], F32)
    A16 = P.sb("A16", [16, NB], F32)
    B16 = P.sb("B16", [16, NB], F32)
    G1 = P.sb("G1", [16, NB], F32)
    gblocks = [(b * NB, NB) for b in range(8)] + [(L, 256)]
    for (t0, n) in gblocks:
        nch = n // 128
        P.dma('sp', gmb[:, 0:n], C.gmT[:, t0:t0 + n], w=['gmb'])
        P.ts('dve', gmb[:, 0:n], gmb[:, 0:n], bif[:, 0:1], None, ALU.add, r=['gmb', 'bif'], w=['gmb'])
        P.act(e1[:, 0:n], gmb[:, 0:n], ACT.Exp, scale=-1.0, r=['gmb'], w=['e1'])
        P.act(e1[:, 0:n], e1[:, 0:n], ACT.Ln, bias=1.0, r=['e1'], w=['e1'])
        P.ts('dve', x0[:, 0:n], e1[0:8, 0:n], -1.0, None, ALU.mult, r=['e1'], w=['x0'])
        src, sk = x0, 'x0'
        pp = [(xa, 'xa'), (xb, 'xb')]
        step = 1
        i = 0
        while step < 128:
            dst, dk = pp[i % 2]
            sv = src[:, 0:n].rearrange("p (c t) -> p c t", t=128)
            dv = dst[:, 0:n].rearrange("p (c t) -> p c t", t=128)
            P.tt('dve', dv[:, :, step:], sv[:, :, step:], sv[:, :, :128 - step], ALU.add, r=[sk], w=[dk])
            P.cp('pool', dv[:, :, :step], sv[:, :, :step], r=[sk], w=[dk])
            src, sk = dst, dk
            step *= 2
            i += 1
        pv_ = src[:, 0:n].rearrange("p (c t) -> p c t", t=128)
        cfv = cF[:, 0:n].rearrange("p (c t) -> p c t", t=128)
        P.tt('dve', cfv, pv_[:, :, 127:128].broadcast_to([8, nch, 128]), pv_, ALU.subtract, r=[sk], w=['cF'])
        P.tt('dve', cF[:, 0:n], cF[:, 0:n], x0[:, 0:n], ALU.add, r=['cF', 'x0'], w=['cF'])
        P.ts('dve', cF[:, 0:n], cF[:, 0:n], selc[0:8, 3:4], None, ALU.mult, r=['cF', 'selc'], w=['cF'])
        P.stt('dve', cum[:, t0:t0 + n], src[:, 0:n], selc[0:8, 2:3], cF[:, 0:n], ALU.mult, ALU.add, r=[sk, 'selc', 'cF'], w=['cum'])
        P.cp('dve', cumhl[0:8, t0:t0 + n], cum[:, t0:t0 + n], r=['cum'], w=['cumhl'])
        P.cp('dve', hif[:, 0:n], cumhl[0:8, t0:t0 + n], r=['cumhl'], w=['hif'])
        P.tt('dve', lob[:, 0:n], cum[:, t0:t0 + n], hif[:, 0:n], ALU.subtract, r=['cum', 'hif'], w=['lob'])
        P.dma('sp', cumhl[32:40, t0:t0 + n], lob[:, 0:n], r=['lob', 'cumhl'], w=['cumhl'])
        P.memset('pool', B16[:, 0:n], 0.0, w=['B16'])
        P.dma('sp', B16[8:16, 0:n], cum[:, t0:t0 + n], r=['cum', 'B16'], w=['B16'])
        P.ts('dve', A16[:, 0:n], gmb[:, 0:n], selc[:, 0:1], selc[:, 1:2], ALU.mult, ALU.add, r=['gmb', 'selc'], w=['A16'])
        P.tt('dve', G1[:, 0:n], A16[:, 0:n], B16[:, 0:n], ALU.subtract, r=['A16', 'B16'], w=['G1'])
        for ci in range(nch):
            ch = t0 // 128 + ci
            bk = C.bank[ci % 2]
            P.tr(bk[:, 0:16], G1[:, ci * 128:(ci + 1) * 128], idf[0:16, 0:16], r=['G1', 'idf'], w=['bank%d' % (ci % 2)])
            P.cp('dve', ict[:, ch, :], bk[:, 0:16], r=['bank%d' % (ci % 2)], w=['ict'])
            P.act(wt[:, ch, :], bk[:, 0:16], ACT.Exp, r=['bank%d' % (ci % 2)], w=['wt'])

    hfwd = P.sb("hfwd", [128, NT, 256], BF16)
    Z = P.sb("Z", [64, 4, 65], F32)
    Zb = P.sb("Zb", [64, 4, 65], BF16)
    qc = [P.sb("qc%d" % i, [64, 4, 128], BF16) for i in range(3)]
    kc = [P.sb("kc%d" % i, [64, 4, 128], BF16) for i in range(3)]
    vc = [P.sb("vc%d" % i, [128, 4, 65], BF16) for i in range(3)]
    for i in range(3):
        P.memset('pool', vc[i][:, :, 64:65], 1.0, w=['vc%d' % i])
    oz = [P.sb("oz%d" % i, [128, 512], BF16) for i in range(2)]
    Dt = [P.sb("Dt%d" % i, [128, 512], BF16) for i in range(2)]
    U = [P.sb("U%d" % i, [64, 512], F32) for i in range(2)]
    St = [P.sb("St%d" % i, [128, 512], BF16) for i in range(2)]
    qs = [P.sb("qs%d" % i, [64, 4, 128], BF16) for i in range(2)]
    ks = [P.sb("ks%d" % i, [128, 4, 64], BF16) for i in range(2)]
    dd = P.sb("dd", [128, 16], F32)
    hh = [P.sb("hh%d" % i, [128, 256], F32) for i in range(2)]
    sg = [P.sb("sg%d" % i, [128, 512], F32) for i in range(2)]
    go = [P.sb("go%d" % i, [128, 256], BF16) for i in range(2)]
    gst = [P.sb("gst%d" % i, [128, 256], BF16) for i in range(2)]
    qv = C.qkm2[0:256, :].rearrange("(h d) t -> d h t", d=64)
    kv = C.qkm2[256:512, :].rearrange("(h d) t -> d h t", d=64)
    it = 0
    for dpass in range(2):
        order = ([64, 65] + list(range(64))) if dpass == 0 else ([65, 64] + list(range(63, -1, -1)))
        P.memset('pool', Z[:], 0.0, w=['Z'])
        P.memset('pool', Zb[:], 0.0, w=['Zb'])
        for c in order:
            emit = (c < 64) or upd
            b3 = it % 3
            p2 = it % 2
            it += 1
            q_, k_, v_ = qc[b3], kc[b3], vc[b3]
            qk_, kk_, vk_ = 'qc%d' % b3, 'kc%d' % b3, 'vc%d' % b3
            cs = slice(c * 128, (c + 1) * 128)
            P.dma('sp', q_[:], qv[:, :, cs], w=[qk_])
            P.dma('act', k_[:], kv[:, :, cs], w=[kk_])
            P.dma('sp', v_[:, :, 0:64], C.pT[cs, 1024:1280].rearrange("p (h d) -> p h d", d=64), w=[vk_])
            bD, kD = C.bank[p2], 'bank%d' % p2
            bS, kS = C.bank[2 + p2], 'bank%d' % (2 + p2)
            bU, kU = C.bank[4], 'bank4'
            bN, kN = C.bank[5], 'bank5'
            bZ, kZ = C.bank[6], 'bank6'
            P.mm(bD[:, :], idb[:], mlm[:, dpass * 512:(dpass + 1) * 512], start=True, stop=False, r=['idb', 'mlm'], w=[kD])
            for h in range(4):
                dh = dpass * 4 + h
                P.mm(bD[:, h * 128:(h + 1) * 128], selC[:, dh * 128:(dh + 1) * 128], cumhl[:, cs], start=False, stop=True,
                     r=['selC', 'cumhl'], w=[kD])
            for h in range(4):
                dh = dpass * 4 + h
                P.mm(bU[0:64, h * 128:(h + 1) * 128], selC[:, dh * 128:dh * 128 + 64], cumhl[:, cs], start=True, stop=True,
                     r=['selC', 'cumhl'], w=[kU])
            for h in range(4):
                P.mm(bS[:, h * 128:(h + 1) * 128], k_[:, h, :], q_[:, h, :], start=True, stop=True, r=[kk_, qk_], w=[kS])
            for h in range(4):
                P.tr(C.psb[:, h * 64:(h + 1) * 64], k_[:, h, :], idb[0:64, 0:64], r=[kk_, 'idb'], w=['psb'])
            d_, u_, s2, q2, k2 = Dt[p2], U[p2], St[p2], qs[p2], ks[p2]
            for h in range(4):
                dh = dpass * 4 + h
                P.act(d_[:, h * 128:(h + 1) * 128], bD[:, h * 128:(h + 1) * 128], ACT.Exp, bias=ict[:, c, 8 + dh:9 + dh],
                      r=[kD, 'ict'], w=['Dt%d' % p2])
            P.act(u_[:], bU[0:64, :], ACT.Exp, r=[kU], w=['U%d' % p2])
            P.tt('dve', s2[:], bS[:, :], d_[:], ALU.mult, r=[kS, 'Dt%d' % p2], w=['St%d' % p2])
            P.tt('pool', q2[:], q_[:], u_[:].rearrange("p (h t) -> p h t", h=4), ALU.mult, r=[qk_, 'U%d' % p2], w=['qs%d' % p2])
            P.tt('dve', k2[:], C.psb[:, 0:256].rearrange("p (h d) -> p h d", d=64),
                 wt[:, c, 8 + dpass * 4:12 + dpass * 4].unsqueeze(2).broadcast_to([128, 4, 64]), ALU.mult,
                 r=['psb', 'wt'], w=['ks%d' % p2])
            if emit:
                for h in range(4):
                    P.mm(bN[:, h * 65:(h + 1) * 65], s2[:, h * 128:(h + 1) * 128], v_[:, h, :], start=True, stop=False,
                         r=['St%d' % p2, vk_], w=[kN])
                    P.mm(bN[:, h * 65:(h + 1) * 65], q2[:, h, :], Zb[:, h, :], start=False, stop=True,
                         r=['qs%d' % p2, 'Zb'], w=[kN])
                nv = bN[:, 0:260].rearrange("p (h e) -> p h e", e=65)
                P.cp('dve', dd[:, 0:4], nv[:, :, 64], r=[kN], w=['dd'])
                P.stt('dve', dd[:, 4:8], dd[:, 0:4], -1.0, dd[:, 0:4], ALU.mult, ALU.max, r=['dd'], w=['dd'])
                P.ts('dve', dd[:, 4:8], dd[:, 4:8], 1.0, None, ALU.max, r=['dd'], w=['dd'])
                P.op('dve', lambda e: e.reciprocal(dd[:, 8:12], dd[:, 4:8]), r=['dd'], w=['dd'])
                hv = hh[p2][:].rearrange("p (h d) -> p h d", d=64)
                P.tt('dve', hv, nv[:, :, 0:64], dd[:, 8:12].unsqueeze(2).broadcast_to([128, 4, 64]), ALU.mult, r=[kN, 'dd'], w=['hh%d' % p2])
                if dpass == 0:
                    P.cp('pool', hfwd[:, c, :], hh[p2][:], r=['hh%d' % p2], w=['hfwd'])
                else:
                    P.dma('act', oz[p2][:], C.pT[cs, 1280:1792], w=['oz%d' % p2])
                    P.act(sg[p2][:, 0:256], oz[p2][:, 0:256], ACT.Sigmoid, r=['oz%d' % p2], w=['sg%d' % p2])
                    P.act(sg[p2][:, 256:512], oz[p2][:, 256:512], ACT.Silu, r=['oz%d' % p2], w=['sg%d' % p2])
                    P.tt('pool', hh[p2][:], hh[p2][:], hfwd[:, c, :], ALU.add, r=['hh%d' % p2, 'hfwd'], w=['hh%d' % p2])
                    P.tt('pool', hh[p2][:], hh[p2][:], sg[p2][:, 0:256], ALU.mult, r=['hh%d' % p2, 'sg%d' % p2], w=['hh%d' % p2])
                    P.tt('pool', go[p2][:], hh[p2][:], sg[p2][:, 256:512], ALU.mult, r=['hh%d' % p2, 'sg%d' % p2], w=['go%d' % p2])
                    for c2 in range(2):
                        P.tr(C.psb[:, 512 + c2 * 128:512 + (c2 + 1) * 128], go[p2][:, c2 * 128:(c2 + 1) * 128], idb[:], r=['go%d' % p2, 'idb'], w=['psb2'])
                    P.cp('act', gst[p2][:], C.psb[:, 512:768], r=['psb2'], w=['gst%d' % p2])
                    P.dma('sp', C.gT[512:768, cs].rearrange("(c p) t -> p c t", p=128), gst[p2][:].rearrange("p (c t) -> p c t", c=2),
                          r=['gst%d' % p2], w=[])
            for h in range(4):
                P.mm(bZ[0:64, h * 65:(h + 1) * 65], k2[:, h, :], v_[:, h, :], start=True, stop=True, r=['ks%d' % p2, vk_], w=[kZ])
            P.tt('dve', Z[:], Z[:], bZ[0:64, 0:260].rearrange("p (h e) -> p h e", e=65), ALU.add, r=['Z', kZ], w=['Z'])
            ecol = 127 if dpass == 0 else 0
            ev = u_[:].rearrange("p (h t) -> p h t", h=4)[:, :, ecol:ecol + 1].broadcast_to([64, 4, 65])
            P.tt('dve', Z[:], Z[:], ev, ALU.mult, r=['Z', 'U%d' % p2], w=['Z'])
            P.cp('act', Zb[:], Z[:], r=['Z'], w=['Zb'])
    P.end()


def _sin_mlp_block(P, C, ps, bcol, fcol, out, n, tag, tmp, pskey):
    pre, t2 = tmp
    P.ts('dve', pre[:, 0:n], ps, bcol, fcol, ALU.add, ALU.mult, r=[pskey, 'hfc'], w=[tag + 'pre'])
    P.ts('pool', t2[:, 0:n], pre[:, 0:n], -1.0, PI, ALU.mult, ALU.add, r=[tag + 'pre'], w=[tag + 't2'])
    P.tt('dve', t2[:, 0:n], pre[:, 0:n], t2[:, 0:n], ALU.min, r=[tag + 'pre', tag + 't2'], w=[tag + 't2'])
    P.ts('dve', pre[:, 0:n], pre[:, 0:n], -1.0, -PI, ALU.mult, ALU.add, r=[tag + 'pre'], w=[tag + 'pre'])
    P.tt('dve', t2[:, 0:n], t2[:, 0:n], pre[:, 0:n], ALU.max, r=[tag + 'pre', tag + 't2'], w=[tag + 't2'])
    P.act(out[:, 0:n], t2[:, 0:n], ACT.Sin, r=[tag + 't2'], w=[tag + 'out'])


def _hy_filters(C, l, Ln, featT, decayT, dstF):
    P = C.P
    w1 = P.sb("w1", [33, 64], F32); w2 = P.sb("w2", [64, 64], F32); w3 = P.sb("w3", [64, 1024], F32)
    hfc = P.sb("hfc", [64, 4], F32)
    P.dma('sp', w1[:], C.hf_w1[l], w=['hfw'])
    P.dma('sp', w2[:], C.hf_w2[l], w=['hfw'])
    P.dma('act', w3[:], C.hf_w3[l], w=['hfw'])
    P.dma('sp', hfc[:, 0:1], C.hf_b1[l], w=['hfc'])
    P.dma('sp', hfc[:, 1:2], C.hf_b2[l], w=['hfc'])
    P.dma('sp', hfc[:, 2:4], C.hf_freq[l], w=['hfc'])
    nb = max(Ln // 512, 1)
    bw = min(Ln, 512)
    ssq = P.sb("ssq", [128, 8, 16], F32)
    P.memset('pool', ssq[:], 0.0, w=['ssq'])
    ft = [P.sb("ft%d" % i, [33, 512], F32) for i in range(3)]
    dec = [P.sb("dec%d" % i, [128, 2, 512], F32) for i in range(5)]
    pre1 = [P.sb("pre1%d" % i, [64, 512], F32) for i in range(3)]
    t21 = [P.sb("t21%d" % i, [64, 512], F32) for i in range(3)]
    a1 = [P.sb("a1%d" % i, [64, 512], F32) for i in range(3)]
    pre2 = [P.sb("pre2%d" % i, [64, 512], F32) for i in range(3)]
    t22 = [P.sb("t22%d" % i, [64, 512], F32) for i in range(3)]
    a2 = [P.sb("a2%d" % i, [64, 512], F32) for i in range(3)]
    fl = [P.sb("fl%d" % i, [128, 512], F32) for i in range(4)]
    junk = P.sb("junkf", [128, 512], BF16)

    def st1(b):
        p3, p2, p5 = b % 3, b % 2, b % 5
        cs = slice(b * bw, (b + 1) * bw)
        P.dma('sp', ft[p3][:, 0:bw], featT[:, cs], w=['ft%d' % p3])
        P.dma('act', dec[p5][:, :, 0:bw], decayT[:, cs].rearrange("(h p) t -> p h t", p=128), w=['dec%d' % p5])
        P.mm(C.bank[0][0:64, 0:bw], w1[:], ft[p3][:, 0:bw], r=['hfw', 'ft%d' % p3], w=['bank0'])
        _sin_mlp_block(P, C, C.bank[0][0:64, 0:bw], hfc[:, 0:1], hfc[:, 2:3], a1[p3], bw, 'm1%d' % p3, (pre1[p3], t21[p3]), 'bank0')

    def st2(b):
        p2, p3 = b % 2, b % 3
        P.mm(C.bank[1][0:64, 0:bw], w2[:], a1[p3][:, 0:bw], r=['hfw', 'm1%dout' % p3], w=['bank1'])
        _sin_mlp_block(P, C, C.bank[1][0:64, 0:bw], hfc[:, 1:2], hfc[:, 3:4], a2[p3], bw, 'm2%d' % p3, (pre2[p3], t22[p3]), 'bank1')

    def st3(b):
        p3, p2, p5 = b % 3, b % 2, b % 5
        cs = slice(b * bw, (b + 1) * bw)
        for cc in range(8):
            bk = C.bank[2 + cc % 4]
            bkk = 'bank%d' % (2 + cc % 4)
            f3 = (b * 8 + cc) % 4
            P.mm(bk[:, 0:bw], w3[:, cc * 128:(cc + 1) * 128], a2[p3][:, 0:bw], r=['hfw', 'm2%dout' % p3], w=[bkk])
            P.tt('dve', fl[f3][:, 0:bw], bk[:, 0:bw], dec[p5][:, cc % 2, 0:bw], ALU.mult, r=[bkk, 'dec%d' % p5], w=['fl%d' % f3])
            if b == 0 and (cc // 2) % 2 == 1:
                P.memset('pool', fl[f3][:, 0:1], 0.0, w=['fl%d' % f3])
            P.act(junk[:, 0:bw], fl[f3][:, 0:bw], ACT.Square, accum_out=ssq[:, cc, b:b + 1], r=['fl%d' % f3], w=['ssq'])
            P.dma('sp', dstF[cc * 128:(cc + 1) * 128, cs], fl[f3][:, 0:bw], r=['fl%d' % f3], w=[])

    _run_pipeline([(st1, 0), (st2, 2), (st3, 4)], nb)
    tot = P.sb("tot", [128, 8], F32)
    nrm = P.sb("nrm", [128, 8], F32)
    P.op('dve', lambda e: e.tensor_reduce(tot[:], ssq[:], AX.X, ALU.add), r=['ssq'], w=['tot'])
    tv = tot[:].rearrange("p (o d h) -> p o d h", o=2, d=2)
    nv = nrm[:, 0:4].rearrange("p (o h) -> p o h", o=2)
    P.tt('dve', nv, tv[:, :, 0, :], tv[:, :, 1, :], ALU.add, r=['tot'], w=['nrm'])
    P.ts('dve', nrm[:, 0:4], nrm[:, 0:4], 1e-6, None, ALU.add, r=['nrm'], w=['nrm'])
    P.act(nrm[:, 0:4], nrm[:, 0:4], ACT.Sqrt, r=['nrm'], w=['nrm'])
    P.op('dve', lambda e: e.reciprocal(nrm[:, 4:8], nrm[:, 0:4]), r=['nrm'], w=['nrm'])
    return nrm


def _fft_S1(P, C, xg, xk, dft, banks):
    for c in range(4):
        bi = banks[c // 2]
        P.mm(C.bank[bi][:, (c % 2) * 256:(c % 2) * 256 + 256], xg[0:64, c, :], dft[0:64, 0:256], r=[xk, 'dft'], w=['bank%d' % bi])


def _fft_S3(P, C, dft, Bt, bkey, banks):
    Bv = Bt[:].rearrange("p (c r k) -> p c r k", c=4, r=2)
    Br, Bi = Bv[:, :, 0, :], Bv[:, :, 1, :]
    r_, i_ = banks
    P.mm(C.bank[r_][:, :], dft[:, 2 * 128:3 * 128], Br, start=True, stop=False, r=['dft', bkey], w=['bank%d' % r_])
    P.mm(C.bank[r_][:, :], dft[:, 3 * 128:4 * 128], Bi, start=False, stop=True, r=['dft', bkey], w=['bank%d' % r_])
    P.mm(C.bank[i_][:, :], dft[:, 4 * 128:5 * 128], Br, start=True, stop=False, r=['dft', bkey], w=['bank%d' % i_])
    P.mm(C.bank[i_][:, :], dft[:, 5 * 128:6 * 128], Bi, start=False, stop=True, r=['dft', bkey], w=['bank%d' % i_])


def _twiddle(P, C, banks, tw, which, Bt, bkey, tmp, tkey):
    TA = tw[:, (2 * which) * 1024:(2 * which) * 1024 + 512].rearrange("p (c r k) -> p c r k", c=2, r=2)
    TB = tw[:, (2 * which + 1) * 1024:(2 * which + 1) * 1024 + 512].rearrange("p (c r k) -> p c r k", c=2, r=2)
    for hb in range(2):
        bi = banks[hb]
        bkk = 'bank%d' % bi
        t1, t2 = tmp[hb]
        k1, k2 = tkey + 'a%d' % hb, tkey + 'b%d' % hb
        A = C.bank[bi][:, :].rearrange("p (c r k) -> p c r k", c=2, r=2)
        T1 = t1[:].rearrange("p (c r k) -> p c r k", c=2, r=2)
        T2 = t2[:].rearrange("p (c r k) -> p c r k", c=2, r=2)
        P.tt('dve', T1, A, TA, ALU.mult, r=[bkk, 'tw'], w=[k1])
        P.tt('dve', T2[:, :, 0, :], A[:, :, 1, :], TB[:, :, 0, :], ALU.mult, r=[bkk, 'tw'], w=[k2])
        P.tt('dve', T2[:, :, 1, :], A[:, :, 0, :], TB[:, :, 1, :], ALU.mult, r=[bkk, 'tw'], w=[k2])
        P.tt('pool', Bt[:, hb * 512:(hb + 1) * 512], t1[:], t2[:], ALU.add, r=[k1, k2], w=[bkey])


def _fft_tables(C):
    P = C.P
    dft = P.sb("dft", [128, 12 * 128], BF16)
    tw = P.sb("tw", [128, 4 * 1024], BF16)
    P.dma('sp', dft[:], C.dft, w=['dft'])
    P.dma('act', tw[:], C.tw, w=['tw'])
    return dft, tw


def _tw_tmp(P, name):
    return [[(P.sb("%st1_%d%d" % (name, p, h), [128, 512], F32), P.sb("%st2_%d%d" % (name, p, h), [128, 512], F32)) for h in range(2)] for p in range(2)]


def _run_pipeline(stages, ng):
    maxlag = max(l for _, l in stages)
    for t in range(ng + maxlag):
        for fn, lag in stages:
            g = t - lag
            if 0 <= g < ng:
                fn(g)


def _fftconv(C, src, order, dst, ng=64, stage=9):
    P = C.P
    P.begin()
    dft, tw = _fft_tables(C)
    xin = [P.sb("xin%d" % i, [64, 16, 128], BF16) for i in range(2)]
    kin = [P.sb("kin%d" % i, [128, 1024], BF16) for i in range(2)]
    Bt = [P.sb("Bt%d" % i, [128, 1024], BF16) for i in range(2)]
    Ht = [P.sb("Ht%d" % i, [128, 1024], BF16) for i in range(2)]
    Yt = [P.sb("Yt%d" % i, [128, 1024], BF16) for i in range(2)]
    pp = [[P.sb("pp%d%d" % (p, i), [128, 512], F32) for i in range(4)] for p in range(2)]
    yst = [P.sb("yst%d" % i, [64, 16, 128], BF16) for i in range(2)]
    tmpB = _tw_tmp(P, "tB")
    tmpF = _tw_tmp(P, "tF")
    sv = src.rearrange("c (a b) -> a c b", b=128)
    dv = dst.rearrange("c (a b) -> a c b", b=128)

    def stA(g):
        ch0, x16, p2 = g * 4, (g // 4) % 2, g % 2
        if g % 4 == 0:
            P.dma('sp', xin[x16][:], sv[:, ch0:ch0 + 16, :], w=['xin%d' % x16])
        P.dma('act', kin[p2][:], C.kspec[order, :, ch0 * 256:(ch0 + 4) * 256], w=['kin%d' % p2])
        xg = xin[x16][:, (g % 4) * 4:(g % 4) * 4 + 4, :]
        _fft_S1(P, C, xg, 'xin%d' % x16, dft, (0, 1))

    def stB(g):
        p2 = g % 2
        _twiddle(P, C, (0, 1), tw, 0, Bt[p2], 'B%d' % p2, tmpB[p2], 'tB%d' % p2)

    def stC(g):
        p2 = g % 2
        _fft_S3(P, C, dft, Bt[p2], 'B%d' % p2, (2, 3))

    def stD(g):
        p2 = g % 2
        Kv = kin[p2][:].rearrange("p (c r k) -> p c r k", c=4, r=2)
        Kr, Ki = Kv[:, :, 0, :], Kv[:, :, 1, :]
        Xr = C.bank[2][:, :].rearrange("p (c k) -> p c k", c=4)
        Xi = C.bank[3][:, :].rearrange("p (c k) -> p c k", c=4)
        ppv = [t[:].rearrange("p (c k) -> p c k", c=4) for t in pp[p2]]
        pk = ['pp%d%d' % (p2, i) for i in range(4)]
        P.tt('dve', ppv[0], Xr, Kr, ALU.mult, r=['bank2', 'kin%d' % p2], w=[pk[0]])
        P.tt('dve', ppv[1], Xi, Ki, ALU.mult, r=['bank3', 'kin%d' % p2], w=[pk[1]])
        P.tt('dve', ppv[2], Xr, Ki, ALU.mult, r=['bank2', 'kin%d' % p2], w=[pk[2]])
        P.tt('dve', ppv[3], Xi, Kr, ALU.mult, r=['bank3', 'kin%d' % p2], w=[pk[3]])
        Y = Yt[p2]
        P.tt('pool', Y[:, 0:512], pp[p2][0][:], pp[p2][1][:], ALU.subtract, r=[pk[0], pk[1]], w=['Y%d' % p2])
        P.tt('pool', Y[:, 512:1024], pp[p2][2][:], pp[p2][3][:], ALU.add, r=[pk[2], pk[3]], w=['Y%d' % p2])

    def stE(g):
        p2 = g % 2
        Y = Yt[p2]
        for c in range(4):
            bi = 4 + c // 2
            o_ = C.bank[bi][:, (c % 2) * 256:(c % 2) * 256 + 256]
            P.mm(o_, Y[:, c * 128:(c + 1) * 128], dft[:, 6 * 128:8 * 128], start=True, stop=False, r=['Y%d' % p2, 'dft'], w=['bank%d' % bi])
            P.mm(o_, Y[:, 512 + c * 128:512 + (c + 1) * 128], dft[:, 8 * 128:10 * 128], start=False, stop=True, r=['Y%d' % p2, 'dft'], w=['bank%d' % bi])

    def stF(g):
        p2 = g % 2
        _twiddle(P, C, (4, 5), tw, 1, Ht[p2], 'H%d' % p2, tmpF[p2], 'tF%d' % p2)

    def stG(g):
        ch0, x16, p2 = g * 4, (g // 4) % 2, g % 2
        Hv = Ht[p2][:].rearrange("p (c r k) -> p c r k", c=4, r=2)
        P.mm(C.bank[6][0:64, :], dft[:, 10 * 128:10 * 128 + 64], Hv[:, :, 0, :], start=True, stop=False, r=['dft', 'H%d' % p2], w=['bank6'])
        P.mm(C.bank[6][0:64, :], dft[:, 11 * 128:11 * 128 + 64], Hv[:, :, 1, :], start=False, stop=True, r=['dft', 'H%d' % p2], w=['bank6'])
        P.cp('act', yst[x16][:, (g % 4) * 4:(g % 4) * 4 + 4, :], C.bank[6][0:64, :].rearrange("p (c k) -> p c k", c=4), r=['bank6'], w=['yst%d' % x16])
        if g % 4 == 3:
            P.dma('sp', dv[:, ch0 - 12:ch0 + 4, :], yst[x16][:], r=['yst%d' % x16], w=[])

    _run_pipeline([(stA, 0), (stC, 1), (stE, 2), (stG, 3), (stB, 0), (stD, 1), (stF, 2)], ng)
    P.end()


def _conv3_fm(P, rw, rk, cw, col, out, ok, n, eng='dve'):
    P.ts(eng, out[:, 0:n], rw[:, 0:n], cw[:, col:col + 1], None, ALU.mult, r=[rk, 'cwh'], w=[ok])
    P.stt(eng, out[:, 0:n], rw[:, 1:n + 1], cw[:, col + 1:col + 2], out[:, 0:n], ALU.mult, ALU.add, r=[rk, 'cwh', ok], w=[ok])
    P.stt(eng, out[:, 0:n], rw[:, 2:n + 2], cw[:, col + 2:col + 3], out[:, 0:n], ALU.mult, ALU.add, r=[rk, 'cwh', ok], w=[ok])


_DBG_STOP = None


def phase_hyena(C, l):
    P = C.P
    upd = (l < DEPTH - 1)
    P.begin()
    nrm = _hy_filters(C, l, L, C.featT, C.decayT, C.filtT)
    P.barrier_dram = True
    fin = [P.sb("fin%d" % i, [128, 2048], F32) for i in range(2)]
    fob = [P.sb("fob%d" % i, [128, 2048], BF16) for i in range(2)]
    P.barrier()
    it = 0
    for cc in range(8):
        o, half = cc // 4, cc % 2
        for b in range(4):
            p2 = it % 2
            it += 1
            cs = slice(b * 2048, (b + 1) * 2048)
            P.dma('sp', fin[p2][:], C.filtT[cc * 128:(cc + 1) * 128, cs], w=['fin%d' % p2])
            P.ts('dve' if it % 2 else 'pool', fob[p2][:], fin[p2][:], nrm[:, 4 + o * 2 + half:5 + o * 2 + half], None, ALU.mult,
                 r=['fin%d' % p2, 'nrm'], w=['fob%d' % p2])
            P.dma('act', C.filtN[cc * 128:(cc + 1) * 128, cs], fob[p2][:], r=['fob%d' % p2], w=[])
    P.end()
    if _DBG_STOP == 'E0':
        return
    P.begin()
    dft, tw = _fft_tables(C)
    xin = [P.sb("xin%d" % i, [64, 16, 128], BF16) for i in range(2)]
    Bt = [P.sb("Bt%d" % i, [128, 1024], BF16) for i in range(3)]
    tb = [[P.sb("tb%d%d" % (p, i), [128, 256], F32) for i in range(2)] for p in range(2)]
    kout = [P.sb("kout%d" % i, [128, 8, 256], BF16) for i in range(2)]
    tmpB = _tw_tmp(P, "tB")
    fv = C.filtN.rearrange("c (a b) -> a c b", b=128)
    for o in range(2):
        def stA(g, o=o):
            ch0, x16 = g * 2, (g // 4) % 2
            if g % 4 == 0:
                for q in range(4):
                    P.dma('sp', xin[x16][:, q * 4:q * 4 + 2, :], fv[:, o * 512 + ch0 + 2 * q:o * 512 + ch0 + 2 * q + 2, :], w=['xin%d' % x16])
                    P.dma('act', xin[x16][:, q * 4 + 2:q * 4 + 4, :], fv[:, o * 512 + 256 + ch0 + 2 * q:o * 512 + 256 + ch0 + 2 * q + 2, :], w=['xin%d' % x16])
            xg = xin[x16][:, (g % 4) * 4:(g % 4) * 4 + 4, :]
            sb = (0, 1) if g % 2 == 0 else (4, 5)
            _fft_S1(P, C, xg, 'xin%d' % x16, dft, sb)

        def stB(g):
            p2 = g % 2
            sb = (0, 1) if g % 2 == 0 else (4, 5)
            _twiddle(P, C, sb, tw, 0, Bt[g % 3], 'B%d' % (g % 3), tmpB[p2], 'tB%d' % p2)

        def stC(g):
            _fft_S3(P, C, dft, Bt[g % 3], 'B%d' % (g % 3), (2, 3))

        def stD(g, o=o):
            ch0, x16, p2 = g * 2, (g // 4) % 2, g % 2
            P.cp('act', tb[p2][0][:], C.bank[2][:, 256:512], r=['bank2'], w=['tb%d0' % p2])
            P.cp('act', tb[p2][1][:], C.bank[3][:, 256:512], r=['bank3'], w=['tb%d1' % p2])
            ko = kout[x16][:, (g % 4) * 2:(g % 4) * 2 + 2, :].rearrange("p c (r k) -> p c r k", r=2)
            P.tt('dve', ko[:, :, 0, :], C.bank[2][:, 0:256].rearrange("p (c k) -> p c k", c=2),
                 tb[p2][0][:].rearrange("p (c k) -> p c k", c=2), ALU.add, r=['bank2', 'tb%d0' % p2], w=['kout%d' % x16])
            P.tt('dve', ko[:, :, 1, :], C.bank[3][:, 0:256].rearrange("p (c k) -> p c k", c=2),
                 tb[p2][1][:].rearrange("p (c k) -> p c k", c=2), ALU.subtract, r=['bank3', 'tb%d1' % p2], w=['kout%d' % x16])
            if g % 4 == 3:
                P.dma('sp', C.kspec[o, :, (ch0 - 6) * 256:(ch0 + 2) * 256], kout[x16][:].rearrange("p c k -> p (c k)"), r=['kout%d' % x16], w=[])

        _run_pipeline([(stA, 0), (stC, 2), (stB, 0), (stD, 2)], 128)
    P.end()
    if _DBG_STOP == 'E0b':
        return
    P.begin()
    cwh = P.sb("cwh", [128, 18], F32)
    P.dma('sp', cwh[:], C.conv_hy[l], w=['cwh'])
    raw = [P.sb("raw%d" % i, [128, 2050], BF16) for i in range(3)]
    cf = [P.sb("cf%d" % i, [128, 2048], F32) for i in range(2)]
    cb = [P.sb("cb%d" % i, [128, 2048], BF16) for i in range(2)]
    it = 0
    for b in range(4):
        t0 = b * 2048
        for c in range(6):
            ri, p2 = it % 3, it % 2
            it += 1
            rw, rk = raw[ri], 'raw%d' % ri
            lo, hi = max(t0 - 1, 0), min(t0 + 2049, L)
            if lo > t0 - 1:
                P.memset('pool', rw[:, 0:1], 0.0, w=[rk])
            if hi < t0 + 2049:
                P.memset('pool', rw[:, 2049:2050], 0.0, w=[rk])
            P.dma('sp' if it % 2 else 'act', rw[:, lo - (t0 - 1):hi - (t0 - 1)], C.hyT[c * 128:(c + 1) * 128, lo:hi], r=[rk], w=[rk])
            _conv3_fm(P, rw, rk, cwh, c * 3, cf[p2], 'cf%d' % p2, 2048)
            P.cp('act', cb[p2][:], cf[p2][:], r=['cf%d' % p2], w=['cb%d' % p2])
            dst = C.hyV[c * 128:(c + 1) * 128, t0:t0 + 2048] if c < 2 else C.hyX[(c - 2) * 128:(c - 1) * 128, t0:t0 + 2048]
            P.dma('sp', dst, cb[p2][:], r=['cb%d' % p2], w=[])
    P.end()
    if _DBG_STOP == 'E1':
        return
    _fftconv(C, C.hyV, 0, C.hyY)
    if _DBG_STOP == 'E2':
        return
    P.begin()
    hbias = P.sb("hbias", [128, 4], F32)
    P.dma('sp', hbias[:], C.hy_bias[l], w=['hbias'])
    ty = [P.sb("ty%d" % i, [128, 2048], BF16) for i in range(2)]
    tv = [P.sb("tv%d" % i, [128, 2048], BF16) for i in range(2)]
    tx = [P.sb("tx%d" % i, [128, 2048], BF16) for i in range(2)]
    tf = [P.sb("tf%d" % i, [128, 2048], F32) for i in range(2)]
    tz = [P.sb("tz%d" % i, [128, 2048], BF16) for i in range(2)]
    it = 0
    for half in range(2):
        rs = slice(half * 128, (half + 1) * 128)
        for b in range(4):
            p2 = it % 2
            it += 1
            cs = slice(b * 2048, (b + 1) * 2048)
            P.dma('sp', ty[p2][:], C.hyY[rs, cs], w=['ty%d' % p2])
            P.dma('act', tv[p2][:], C.hyV[rs, cs], w=['tv%d' % p2])
            P.dma('sp', tx[p2][:], C.hyX[rs, cs], w=['tx%d' % p2])
            P.stt('dve', tf[p2][:], tv[p2][:], hbias[:, half:half + 1], ty[p2][:], ALU.mult, ALU.add, r=['tv%d' % p2, 'ty%d' % p2, 'hbias'], w=['tf%d' % p2])
            P.tt('pool', tz[p2][:], tf[p2][:], tx[p2][:], ALU.mult, r=['tf%d' % p2, 'tx%d' % p2], w=['tz%d' % p2])
            P.dma('act', C.hyZ[rs, cs], tz[p2][:], r=['tz%d' % p2], w=[])
    P.end()
    _fftconv(C, C.hyZ, 1, C.hyY)
    P.begin()
    hbias = P.sb("hbias", [128, 4], F32)
    P.dma('sp', hbias[:], C.hy_bias[l], w=['hbias'])
    ty = [P.sb("ty%d" % i, [128, 2048], BF16) for i in range(2)]
    tv = [P.sb("tv%d" % i, [128, 2048], BF16) for i in range(2)]
    tx = [P.sb("tx%d" % i, [128, 2048], BF16) for i in range(2)]
    tg = [P.sb("tg%d" % i, [128, 2048], BF16) for i in range(2)]
    tf = [P.sb("tf%d" % i, [128, 2048], F32) for i in range(2)]
    ts_ = [P.sb("tsl%d" % i, [128, 2048], F32) for i in range(2)]
    tz = [P.sb("tz%d" % i, [128, 2048], BF16) for i in range(2)]
    it = 0
    for half in range(2):
        rs = slice(half * 128, (half + 1) * 128)
        for b in range(4):
            p2 = it % 2
            it += 1
            cs = slice(b * 2048, (b + 1) * 2048)
            P.dma('sp', ty[p2][:], C.hyY[rs, cs], w=['ty%d' % p2])
            P.dma('act', tv[p2][:], C.hyZ[rs, cs], w=['tv%d' % p2])
            P.dma('sp', tx[p2][:], C.hyX[256 + half * 128:256 + (half + 1) * 128, cs], w=['tx%d' % p2])
            P.dma('act', tg[p2][:], C.hyT[768 + half * 128:768 + (half + 1) * 128, cs], w=['tg%d' % p2])
            P.act(ts_[p2][:], tg[p2][:], ACT.Silu, r=['tg%d' % p2], w=['tsl%d' % p2])
            P.stt('dve', tf[p2][:], tv[p2][:], hbias[:, 2 + half:3 + half], ty[p2][:], ALU.mult, ALU.add, r=['tv%d' % p2, 'ty%d' % p2, 'hbias'], w=['tf%d' % p2])
            P.tt('pool', tf[p2][:], tf[p2][:], tx[p2][:], ALU.mult, r=['tf%d' % p2, 'tx%d' % p2], w=['tf%d' % p2])
            P.tt('dve', tz[p2][:], tf[p2][:], ts_[p2][:], ALU.mult, r=['tf%d' % p2, 'tsl%d' % p2], w=['tz%d' % p2])
            P.dma('sp', C.gT[768 + half * 128:768 + (half + 1) * 128, cs], tz[p2][:], r=['tz%d' % p2], w=[])
    P.end()
    if upd:
        _hyena_ctx(C, l)


def _hyena_ctx(C, l):
    P = C.P
    P.begin()
    nrm = _hy_filters(C, l, LC, C.featTc, C.decayTc, C.filtC)
    P.barrier()
    cwh = P.sb("cwh", [128, 18], F32)
    P.dma('sp', cwh[:], C.conv_hy[l], w=['cwh'])
    hbias = P.sb("hbias", [128, 4], F32)
    P.dma('sp', hbias[:], C.hy_bias[l], w=['hbias'])
    for half in range(2):
        fk = []
        for o in range(2):
            for d in range(2):
                cc = o * 4 + d * 2 + half
                t = P.sb("fk%d%d%d" % (half, o, d), [128, LC], F32)
                P.dma('sp', t[:], C.filtC[cc * 128:(cc + 1) * 128, :], w=['fk%d%d' % (o, d)])
                P.ts('dve', t[:], t[:], nrm[:, 4 + o * 2 + half:5 + o * 2 + half], None, ALU.mult, r=['fk%d%d' % (o, d), 'nrm'], w=['fk%d%d' % (o, d)])
                fk.append(t)
        u = []
        for c3 in range(3):
            rw = P.sb("rwc%d%d" % (half, c3), [128, LC + 2], BF16)
            P.memset('pool', rw[:, 0:1], 0.0, w=['rwc%d' % c3])
            P.memset('pool', rw[:, LC + 1:LC + 2], 0.0, w=['rwc%d' % c3])
            chunk = c3 * 2 + half
            P.dma('sp', rw[:, 1:LC + 1], C.hyT[chunk * 128:(chunk + 1) * 128, L:T], r=['rwc%d' % c3], w=['rwc%d' % c3])
            t = P.sb("uc%d%d" % (half, c3), [128, LC], F32)
            _conv3_fm(P, rw, 'rwc%d' % c3, cwh, chunk * 3, t, 'uc%d' % c3, LC)
            u.append(t)
        zin = u[0]
        zk = 'uc0'
        for o in range(2):
            fw, bw_ = fk[o * 2], fk[o * 2 + 1]
            NA_ = 4
            accs = [P.sb("accd%d%d%d" % (half, o, i), [128, LC], F32) for i in range(2 * NA_)]
            accp = [P.sb("accp%d%d%d" % (half, o, i), [128, LC], F32) for i in range(2)]
            tmpp = [P.sb("tmpp%d%d%d" % (half, o, i), [128, LC], F32) for i in range(2)]
            accf = accs[0]
            P.ts('dve', accs[0][:], zin[:], fw[:, 0:1], None, ALU.mult, r=[zk, 'fk%d0' % o], w=['ad0'])
            for i in range(1, 2 * NA_):
                P.memset('pool', accs[i][:], 0.0, w=['ad%d' % i])
            for i in range(2):
                P.memset('pool', accp[i][:], 0.0, w=['ap%d' % i])
            npool = 0
            for m in range(1, LC):
                ia = m % NA_
                P.stt('dve', accs[ia][:, m:], zin[:, :LC - m], fw[:, m:m + 1], accs[ia][:, m:], ALU.mult, ALU.add, r=[zk, 'fk%d0' % o, 'ad%d' % ia], w=['ad%d' % ia])
                if m % 4 == 0:
                    ip = npool % 2
                    npool += 1
                    P.ts('pool', tmpp[ip][:, :LC - m], zin[:, m:], bw_[:, m:m + 1], None, ALU.mult, r=[zk, 'fk%d1' % o], w=['tp%d' % ip])
                    P.tt('pool', accp[ip][:, :LC - m], accp[ip][:, :LC - m], tmpp[ip][:, :LC - m], ALU.add, r=['tp%d' % ip, 'ap%d' % ip], w=['ap%d' % ip])
                else:
                    ib = NA_ + m % NA_
                    P.stt('dve', accs[ib][:, :LC - m], zin[:, m:], bw_[:, m:m + 1], accs[ib][:, :LC - m], ALU.mult, ALU.add, r=[zk, 'fk%d1' % o, 'ad%d' % ib], w=['ad%d' % ib])
            for i in range(1, 2 * NA_):
                P.tt('dve', accf[:], accf[:], accs[i][:], ALU.add, r=['ad0', 'ad%d' % i], w=['ad0'])
            for i in range(2):
                P.tt('dve', accf[:], accf[:], accp[i][:], ALU.add, r=['ad0', 'ap%d' % i], w=['ad0'])
            P.stt('dve', accf[:], zin[:], hbias[:, o * 2 + half:o * 2 + half + 1], accf[:], ALU.mult, ALU.add, r=[zk, 'hbias', 'ad0'], w=['ad0'])
            znew = P.sb("zn%d%d" % (half, o), [128, LC], F32)
            P.tt('dve', znew[:], accf[:], u[1 + o][:], ALU.mult, r=['ad0', 'uc%d' % (1 + o)], w=['zn%d' % o])
            zin, zk = znew, 'zn%d' % o
        zg = P.sb("zg%d" % half, [128, LC], BF16)
        sl = P.sb("slc%d" % half, [128, LC], F32)
        ob = P.sb("obc%d" % half, [128, LC], BF16)
        P.dma('act', zg[:], C.hyT[768 + half * 128:768 + (half + 1) * 128, L:T], w=['zg'])
        P.act(sl[:], zg[:], ACT.Silu, r=['zg'], w=['slc'])
        P.tt('dve', ob[:], zin[:], sl[:], ALU.mult, r=[zk, 'slc'], w=['obc'])
        P.dma('sp', C.gT[768 + half * 128:768 + (half + 1) * 128, L:T], ob[:], r=['obc'], w=[])
    P.end()


def phase_outproj(C, l):
    P = C.P
    P.begin()
    upd = (l < DEPTH - 1)
    ntl = NT if upd else 64
    W = P.sb("Wo", [128, 8, D], BF16)
    wst = [P.sb("wst%d" % i, [128, D], F32) for i in range(2)]
    for k in range(8):
        P.dma('sp' if k % 2 == 0 else 'act', wst[k % 2][:], C.w_out[l, k * 128:(k + 1) * 128, :], w=['wst%d' % (k % 2)])
        P.cp('dve' if k % 2 == 0 else 'pool', W[:, k, :], wst[k % 2][:], r=['wst%d' % (k % 2)], w=['Wo'])
    ggate = P.sb("ggate", [128, 2, D], F32)
    for i in range(2):
        P.dma('sp', ggate[:, i, :], C.modrows[i:i + 1, 2 * D:3 * D].partition_broadcast(128), w=['ggate'])
    eps = P.sb("eps", [128, 1], F32)
    P.memset('pool', eps[:], 1e-6, w=['eps'])
    gt = [P.sb("gt%d" % i, [128, 8, 128], BF16) for i in range(2)]
    xt = [P.sb("xt%d" % i, [128, D], F32) for i in range(2)]
    yt = [P.sb("yt%d" % i, [128, D], F32) for i in range(2)]
    junk = P.sb("junk", [128, D], BF16)
    st2 = [P.sb("st%d" % i, [128, 8], F32) for i in range(2)]
    gv = C.gT.rearrange("(k p) t -> p k t", p=128)
    for tile in range(ntl):
        p2 = tile % 2
        st = st2[p2]
        sk = 'st%d_' % p2
        ci = 0 if tile < 64 else 1
        P.dma('sp', gt[p2][:], gv[:, :, tile * 128:(tile + 1) * 128], w=['gt%d' % p2])
        P.dma('act', xt[p2][:], xrows(C, l, tile), w=['xt%d' % p2])
        b0, b1 = C.bank[p2 * 2], C.bank[p2 * 2 + 1]
        k0, k1 = 'bank%d' % (p2 * 2), 'bank%d' % (p2 * 2 + 1)
        for half, (bk, kk) in enumerate(((b0, k0), (b1, k1))):
            for k in range(8):
                P.mm(bk[:, :], gt[p2][:, k, :], W[:, k, half * 512:(half + 1) * 512], start=(k == 0), stop=(k == 7),
                     r=['gt%d' % p2, 'Wo'], w=[kk])
        P.act(junk[:, 0:512], b0[:, :], ACT.Square, accum_out=st[:, 0:1], r=[k0], w=[sk + '0'])
        P.act(junk[:, 512:1024], b1[:, :], ACT.Square, accum_out=st[:, 1:2], r=[k1], w=[sk + '1'])
        P.tt('dve', st[:, 2:3], st[:, 0:1], st[:, 1:2], ALU.add, r=[sk + '0', sk + '1'], w=[sk + '2'])
        P.act(st[:, 3:4], st[:, 2:3], ACT.Sqrt, scale=1.0 / D, bias=eps[:, 0:1], r=[sk + '2', 'eps'], w=[sk + '3'])
        P.recip(st[:, 4:5], st[:, 3:4], r=[sk + '3'], w=[sk + '4'])
        P.stt('dve', yt[p2][:, 0:512], b0[:, :], st[:, 4:5], ggate[:, ci, 0:512], ALU.mult, ALU.mult, r=[k0, sk + '4', 'ggate'], w=['yt%d' % p2])
        P.stt('dve', yt[p2][:, 512:1024], b1[:, :], st[:, 4:5], ggate[:, ci, 512:1024], ALU.mult, ALU.mult, r=[k1, sk + '4', 'ggate'], w=['yt%d' % p2])
        P.tt('pool', yt[p2][:], yt[p2][:], xt[p2][:], ALU.add, r=['yt%d' % p2, 'xt%d' % p2], w=['yt%d' % p2])
        if upd:
            dst = C.x1[tile * 128:(tile + 1) * 128, :]
        else:
            dst = C.out[tile * 128:(tile + 1) * 128, :]
        P.dma('pool', dst, yt[p2][:], r=['yt%d' % p2], w=[])
    P.end()


def _bf16(a):
    import ml_dtypes
    return np.ascontiguousarray(a).astype(ml_dtypes.bfloat16)


_CONSTS = None


def host_consts():
    global _CONSTS
    if _CONSTS is not None:
        return _CONSTS
    f32 = np.float32
    c = {}
    c["ident_f"] = np.eye(128, dtype=f32)
    c["ident_b"] = _bf16(np.eye(128, dtype=f32))
    pairs = na_type_pairs()
    idx_r = np.zeros((NTYPES, 128, 128), np.int64)
    idx_c = np.zeros((NTYPES, 128, 128), np.int64)
    mask = np.zeros((NTYPES, 128, 128), f32)
    a = np.arange(128)
    krow_in, kcol = a // 64, a % 64
    for ty, (j, jp) in enumerate(pairs):
        qr = 2 * j + krow_in
        kr = 2 * jp + krow_in
        rs = np.clip(qr - 4, 0, 120)
        cs = np.clip(kcol - 8, 0, 48)
        dr = kr[:, None] - qr[None, :]
        okr = (kr[:, None] >= rs[None, :]) & (kr[:, None] < rs[None, :] + 8)
        okc = (kcol[:, None] >= cs[None, :]) & (kcol[:, None] < cs[None, :] + 16)
        ok = okr & okc
        idx_r[ty] = np.clip(dr + 7, 0, 14)
        idx_c[ty] = np.clip(kcol[:, None] - kcol[None, :], -15, 15) + 15
        mask[ty] = np.where(ok, 0.0, NEG)
    c["_na_idx_r"], c["_na_idx_c"] = idx_r, idx_c
    c["na_mask"] = np.ascontiguousarray(mask.transpose(1, 0, 2).reshape(128, NTYPES * 128))
    f = np.arange(128)
    jj = f % 64
    ax, half, n = jj // 32, (jj % 32) // 16, jj % 16
    inv = (10000.0 ** (-(np.arange(16, dtype=f32)) / 16)).astype(f32)
    t = np.arange(L)
    pos = np.stack([t // 64, t % 64], 0).astype(f32)
    ang = pos[ax, :] * inv[n][:, None]
    c["rope_cos"] = np.cos(ang).astype(f32)
    c["rope_sin"] = (np.where(half == 0, -1.0, 1.0)[:, None] * np.sin(ang)).astype(f32)
    partner = np.where(half == 0, f + 16, f - 16)
    perm = np.zeros((128, 128), f32)
    perm[partner, f] = 1.0
    c["perm_b"] = _bf16(perm)
    s_, t_ = np.meshgrid(np.arange(128), np.arange(128), indexing="ij")
    mf = np.where(s_ <= t_, 0.0, NEG).astype(f32)
    mb = np.where(s_ >= t_, 0.0, NEG).astype(f32)
    c["ml_mask"] = _bf16(np.concatenate([np.tile(mf, (1, 4)), np.tile(mb, (1, 4))], axis=1))
    selC = np.zeros((8, 8, 128), f32)
    for dh in range(8):
        selC[dh, dh, :] = 1.0
    c["selC"] = selC.reshape(8, 8 * 128)
    selC2 = np.zeros((64, 8, 128), f32)
    for dh in range(8):
        selC2[dh, dh, :] = 1.0
        selC2[32 + dh, dh, :] = 1.0
    c["selC2"] = _bf16(selC2.reshape(64, 8 * 128))
    sc = np.zeros((16, 4), f32)
    sc[8:, 0] = 1.0
    sc[:8, 1] = 1.0
    sc[0:4, 2] = 1.0
    sc[4:8, 3] = 1.0
    c["selcols"] = sc
    def feats(Ln):
        tt = np.arange(Ln, dtype=f32)
        tn = tt / f32(Ln - 1)
        fr = np.linspace(1e-4, 15.0, 16, dtype=f32)
        an = (f32(2.0 * np.pi / Ln) * tt[:, None] * fr[None, :]).astype(f32)
        feat = np.concatenate([tn[:, None], np.cos(an), -np.sin(an)], axis=-1).astype(f32)
        deltas = np.abs(np.linspace(np.log(1e-2) / 1.5, np.log(1e-2) / 0.3, 256, dtype=f32))
        dec = np.exp(-tn[:, None] * deltas[None, :]).astype(f32)
        return np.ascontiguousarray(feat.T), np.ascontiguousarray(dec.T)
    c["featT"], c["decayT"] = feats(L)
    c["featTc"], c["decayTc"] = feats(LC)
    k = np.arange(128)
    th = 2.0 * np.pi * np.outer(k, k) / 128.0
    Cm, Sm = np.cos(th), np.sin(th)
    N2 = 2 * L
    dft = np.zeros((128, 12, 128), np.float64)
    dft[:, 0], dft[:, 1] = Cm, -Sm
    dft[:, 2], dft[:, 3] = Cm, Sm
    dft[:, 4], dft[:, 5] = -Sm, Cm
    dft[:, 6], dft[:, 7] = Cm, Sm
    dft[:, 8], dft[:, 9] = -Sm, Cm
    dft[:, 10], dft[:, 11] = Cm / N2, -Sm / N2
    c["dft"] = _bf16(dft.reshape(128, 12 * 128).astype(f32))
    tht = 2.0 * np.pi * np.outer(k, k) / N2
    twr, tws = np.cos(tht), np.sin(tht)
    def rep(a0, a1):
        return np.tile(np.stack([a0, a1], 1)[:, None], (1, 4, 1, 1)).reshape(128, 1024)
    tw = np.concatenate([rep(twr, twr), rep(tws, -tws), rep(twr, twr), rep(-tws, tws)], axis=1)
    c["tw"] = _bf16(tw.astype(f32))
    _CONSTS = c
    return c


CONST_KEYS = ["ident_f", "ident_b", "na_mask", "rope_cos", "rope_sin", "perm_b", "ml_mask", "selC", "selC2", "selcols",
              "featT", "decayT", "featTc", "decayTc", "dft", "tw"]


def layout_inputs(inp):
    c = host_consts()
    f32 = np.float32
    shared = {k: c[k] for k in CONST_KEYS}
    shared["w_ada"] = np.ascontiguousarray(inp["w_ada"], f32)
    shared["b_ada"] = np.ascontiguousarray(inp["b_ada"], f32).reshape(DEPTH, 1, 3 * D)
    shared["g_pre"] = np.ascontiguousarray(inp["g_pre"], f32).reshape(DEPTH, 1, D)
    shared["g_post"] = np.ascontiguousarray(inp["g_post"], f32).reshape(DEPTH, 1, D)
    w_in = np.array(inp["w_in"], f32, copy=True)
    gm = w_in[:, :, 3328:3344].reshape(DEPTH, D, 2, 2, 4).copy()
    w_in[:, :, 3328:3336] = gm[:, :, :, 1, :].reshape(DEPTH, D, 8)
    w_in[:, :, 3336:3344] = gm[:, :, :, 0, :].reshape(DEPTH, D, 8)
    shared["w_in"] = w_in
    b_if = np.asarray(inp["b_if"], f32)
    shared["b_if"] = np.concatenate([b_if[:, :, 1, :].reshape(DEPTH, 8), b_if[:, :, 0, :].reshape(DEPTH, 8)], 1).reshape(DEPTH, 16, 1).copy()
    cm = np.asarray(inp["conv_ml"], f32)
    shared["conv_ml"] = np.ascontiguousarray(cm.reshape(DEPTH, 3, 4, 128).transpose(0, 3, 2, 1)).reshape(DEPTH, 128, 12)
    ch = np.asarray(inp["conv_hy"], f32)
    shared["conv_hy"] = np.ascontiguousarray(ch.reshape(DEPTH, 3, 6, 128).transpose(0, 3, 2, 1)).reshape(DEPTH, 128, 18)
    rpb = np.asarray(inp["rpb"], f32)
    g = rpb[:, :, c["_na_idx_r"], c["_na_idx_c"]]
    shared["rpb_g"] = np.ascontiguousarray(g.transpose(0, 1, 3, 2, 4)).reshape(DEPTH, 8, 128, NTYPES * 128)
    shared["hf_w1"] = np.ascontiguousarray(inp["hf_w1"], f32)
    shared["hf_b1"] = np.ascontiguousarray(inp["hf_b1"], f32).reshape(DEPTH, 64, 1)
    shared["hf_w2"] = np.ascontiguousarray(inp["hf_w2"], f32)
    shared["hf_b2"] = np.ascontiguousarray(inp["hf_b2"], f32).reshape(DEPTH, 64, 1)
    shared["hf_w3"] = np.ascontiguousarray(inp["hf_w3"], f32)
    shared["hf_freq"] = np.ascontiguousarray(np.asarray(inp["hf_freq"], f32).transpose(0, 2, 1))
    hb = np.asarray(inp["hy_bias"], f32)
    shared["hy_bias"] = np.ascontiguousarray(hb.reshape(DEPTH, 2, 2, 128).transpose(0, 3, 1, 2)).reshape(DEPTH, 128, 4)
    shared["w_out"] = np.ascontiguousarray(inp["w_out"], f32)
    maps = []
    x = np.asarray(inp["x"], f32)
    ctx = np.asarray(inp["ctx"], f32)
    cvec = np.asarray(inp["c"], f32)
    cctx = np.asarray(inp["c_ctx"], f32)
    for b in range(x.shape[0]):
        m = dict(shared)
        m["x"] = np.ascontiguousarray(x[b])
        m["ctx"] = np.ascontiguousarray(ctx[b])
        cc = np.stack([cvec[b].reshape(8, 128).T, cctx.reshape(8, 128).T], axis=-1)
        m["cc"] = np.ascontiguousarray(cc.reshape(128, 16))
        maps.append(m)
    return maps


_PROG = None


def kernel(**inputs):
    global _PROG
    if _PROG is None:
        _PROG = build_program()
    nc, _ = _PROG
    maps = layout_inputs(inputs)
    res = run_bass_kernel_spmd(nc, maps, core_ids=list(range(len(maps))))
    return np.stack([np.asarray(r["out"], np.float32) for r in res.results], axis=0)
```
